# Optimizing a Trainium2 kernel written in Bass

```python
import jax, jax.numpy as jnp
from jax import lax
import numpy as np

D_MODEL = 2048
BATCH = 4
SEQ = 4096
DEPTH = 2

GRID_W = 64
CTX_LEN = 256
N_HEADS = 16
HEAD_DIM = 64
ATTN_WIDTH = N_HEADS * HEAD_DIM
WIN_ROWS = 8
WIN_COLS = 16
COL_BLOCK = 16
KEY_SPAN = 32
ROPE_BASE = 10000.0
CONV_WIDTH = 512
CONV_K = 31
SC_WIDTH = 512
SC_K = 3
N_BRANCH = 3
N_EXPERTS = 16
EXPERT_FF = 1024
CAP_FACTOR = 2
EPS = 1e-6
NEG_INF = -1e30
PROJ_SPLITS = (ATTN_WIDTH, ATTN_WIDTH, ATTN_WIDTH, CONV_WIDTH, CONV_WIDTH, SC_WIDTH, SC_WIDTH, SC_WIDTH, N_BRANCH * D_MODEL)
PROJ_WIDTH = 3 * ATTN_WIDTH + 2 * CONV_WIDTH + 3 * SC_WIDTH + N_BRANCH * D_MODEL

kernel_name = "hybrid_natten_conformer_shortconv_ec_moe_dit"


def rmsnorm(x, g):
    x32 = x.astype(jnp.float32)
    y = x32 * lax.rsqrt(jnp.mean(x32 * x32, axis=-1, keepdims=True) + EPS)
    return (y * g.astype(jnp.float32)).astype(x.dtype)


def layernorm(x, g, b):
    x32 = x.astype(jnp.float32)
    mu = jnp.mean(x32, axis=-1, keepdims=True)
    xc = x32 - mu
    y = xc * lax.rsqrt(jnp.mean(xc * xc, axis=-1, keepdims=True) + EPS)
    return (y * g.astype(jnp.float32) + b.astype(jnp.float32)).astype(x.dtype)


def modulate(h, shift, scale):
    return h * (1 + scale) + shift


def heads(t):
    return t.reshape(*t.shape[:-1], N_HEADS, HEAD_DIM)


def split_proj(p):
    idx = np.cumsum(PROJ_SPLITS)[:-1].tolist()
    return jnp.split(p, idx, axis=-1)


def depthwise_conv(u, w):
    k = w.shape[0]
    kern = w.astype(u.dtype)[:, None, :]
    return lax.conv_general_dilated(u, kern, window_strides=(1,), padding=[(k // 2, k // 2)],
                                    dimension_numbers=('NWC', 'WIO', 'NWC'),
                                    feature_group_count=u.shape[-1])


def axial_rope(x, row_pos, col_pos):
    half = HEAD_DIM // 2
    quarter = half // 2
    freqs = 1.0 / (ROPE_BASE ** (jnp.arange(quarter, dtype=jnp.float32) / quarter))

    def rot(xp, pos):
        ang = pos.astype(jnp.float32)[:, None] * freqs[None, :]
        cos = jnp.cos(ang)[None, :, None, :]
        sin = jnp.sin(ang)[None, :, None, :]
        x1 = xp[..., :quarter].astype(jnp.float32)
        x2 = xp[..., quarter:].astype(jnp.float32)
        return jnp.concatenate([x1 * cos - x2 * sin, x2 * cos + x1 * sin], axis=-1)

    out = jnp.concatenate([rot(x[..., :half], row_pos), rot(x[..., half:], col_pos)], axis=-1)
    return out.astype(x.dtype)


def context_attention(qc, kc, vc):
    s = jnp.einsum('bqhd,bkhd->bhqk', qc, kc).astype(jnp.float32) * (HEAD_DIM ** -0.5)
    p = jax.nn.softmax(s, axis=-1).astype(vc.dtype)
    o = jnp.einsum('bhqk,bkhd->bqhd', p, vc)
    return o.reshape(*o.shape[:2], ATTN_WIDTH)


def neighbourhood_attention(q, k, v, q_plain, kc, vc, rpb):
    B, S, H, Dh = q.shape
    rows = S // GRID_W
    kr = min(WIN_ROWS, rows)
    n_cb = GRID_W // COL_BLOCK
    scale = Dh ** -0.5
    qg = q.reshape(B, rows, n_cb, COL_BLOCK, H, Dh)
    qpg = q_plain.reshape(B, rows, n_cb, COL_BLOCK, H, Dh)
    kg = k.reshape(B, rows, GRID_W, H, Dh)
    vg = v.reshape(B, rows, GRID_W, H, Dh)
    q_col = np.arange(GRID_W).reshape(n_cb, COL_BLOCK)
    q_cstart = np.clip(q_col - WIN_COLS // 2, 0, GRID_W - WIN_COLS)
    blk_start = np.clip(np.arange(n_cb) * COL_BLOCK - WIN_COLS // 2, 0, GRID_W - KEY_SPAN)
    key_col = blk_start[:, None] + np.arange(KEY_SPAN)
    col_valid = ((key_col[:, None, :] >= q_cstart[..., None]) &
                 (key_col[:, None, :] < q_cstart[..., None] + WIN_COLS))
    col_off = np.clip(key_col[:, None, :] - q_col[..., None] + WIN_COLS - 1, 0, 2 * WIN_COLS - 2)
    rpb_cols = rpb[:, :, col_off]
    n_loc = kr * KEY_SPAN

    def row_step(r):
        rs = jnp.clip(r - kr // 2, 0, rows - kr)
        kb = lax.dynamic_slice_in_dim(kg, rs, kr, axis=1)[:, :, key_col]
        vb = lax.dynamic_slice_in_dim(vg, rs, kr, axis=1)[:, :, key_col]
        qr = lax.dynamic_index_in_dim(qg, r, axis=1, keepdims=False)
        qpr = lax.dynamic_index_in_dim(qpg, r, axis=1, keepdims=False)
        row_off = rs + jnp.arange(kr) - r + WIN_ROWS - 1
        bias = jnp.take(rpb_cols, row_off, axis=1).transpose(0, 2, 3, 1, 4)
        s_loc = (jnp.einsum('bjqhd,brjkhd->bhjqrk', qr, kb).astype(jnp.float32) * scale
                 + bias[None].astype(jnp.float32))
        s_loc = jnp.where(col_valid[None, None, :, :, None, :], s_loc, NEG_INF)
        s_ctx = jnp.einsum('bjqhd,bkhd->bhjqk', qpr, kc).astype(jnp.float32) * scale
        s = jnp.concatenate([s_loc.reshape(B, H, n_cb, COL_BLOCK, n_loc), s_ctx], axis=-1)
        p = jax.nn.softmax(s, axis=-1).astype(v.dtype)
        p_loc = p[..., :n_loc].reshape(B, H, n_cb, COL_BLOCK, kr, KEY_SPAN)
        p_ctx = p[..., n_loc:]
        o = (jnp.einsum('bhjqrk,brjkhd->bjqhd', p_loc, vb)
             + jnp.einsum('bhjqk,bkhd->bjqhd', p_ctx, vc))
        return o.reshape(B, GRID_W, H * Dh)

    out = lax.map(row_step, jnp.arange(rows))
    return out.transpose(1, 0, 2, 3).reshape(B, S, H * Dh)


def conformer_conv(a, g, dw_w, dw_b, ln_g, ln_b, w_out):
    u = depthwise_conv(a * jax.nn.sigmoid(g), dw_w) + dw_b
    u = jax.nn.silu(layernorm(u, ln_g, ln_b))
    return u @ w_out


def short_conv(b_gate, c_gate, u, w, w_out):
    return (b_gate * depthwise_conv(c_gate * u, w)) @ w_out


def merge_branches(attn_out, pieces, w_attn_o, conv_dw_w, conv_dw_b, conv_ln_g, conv_ln_b,
                   w_conv_o, sc_w, w_sc_o, w_o):
    _, _, _, glu_a, glu_g, sc_b, sc_c, sc_x, gates = pieces
    y_a = attn_out @ w_attn_o
    y_b = conformer_conv(glu_a, glu_g, conv_dw_w, conv_dw_b, conv_ln_g, conv_ln_b, w_conv_o)
    y_c = short_conv(sc_b, sc_c, sc_x, sc_w, w_sc_o)
    g_a, g_b, g_c = jnp.split(jax.nn.sigmoid(gates), N_BRANCH, axis=-1)
    return (g_a * y_a + g_b * y_b + g_c * y_c) @ w_o


def expert_choice_ffn(h, w_router, w1, w3, w2):
    B, n, D = h.shape
    cap = max(1, CAP_FACTOR * n // N_EXPERTS)
    aff = jax.nn.softmax(jnp.einsum('bnd,de->bne', h, w_router).astype(jnp.float32), axis=-1)
    g, idx = lax.top_k(aff.transpose(0, 2, 1), cap)
    xs = jax.vmap(lambda hb, ib: hb[ib])(h, idx)
    a = jnp.einsum('becd,edf->becf', xs, w1)
    b3 = jnp.einsum('becd,edf->becf', xs, w3)
    y = jnp.einsum('becf,efd->becd', jax.nn.silu(a) * b3, w2) * g[..., None].astype(h.dtype)
    return jax.vmap(lambda yb, ib: jnp.zeros((n, D), yb.dtype).at[ib.reshape(-1)].add(yb.reshape(-1, D)))(y, idx)


def setup_inputs(seed: int = 0) -> dict:
    key = jax.random.key(seed)
    ks = jax.random.split(key, 32)
    L, D = DEPTH, D_MODEL

    def nrm(k, shape, scale):
        return jax.random.normal(k, shape, jnp.float32) * scale

    return {
        "x": nrm(ks[0], (BATCH, SEQ, D), 1.0),
        "c": nrm(ks[1], (BATCH, D), 1.0),
        "ctx": nrm(ks[2], (BATCH, CTX_LEN, D), 1.0),
        "c_ctx": nrm(ks[3], (D,), 1.0),
        "w_ada": nrm(ks[4], (L, D, 6 * D), 0.25 * D ** -0.5),
        "b_ada": nrm(ks[5], (L, 6 * D), 0.02),
        "g_norm1": 1.0 + nrm(ks[6], (L, D), 0.1),
        "g_norm2": 1.0 + nrm(ks[7], (L, D), 0.1),
        "w_in": nrm(ks[8], (L, D, PROJ_WIDTH), D ** -0.5),
        "b_in": nrm(ks[9], (L, PROJ_WIDTH), 0.02),
        "g_q": 1.0 + nrm(ks[10], (L, HEAD_DIM), 0.1),
        "g_k": 1.0 + nrm(ks[11], (L, HEAD_DIM), 0.1),
        "rpb": nrm(ks[12], (L, N_HEADS, 2 * WIN_ROWS - 1, 2 * WIN_COLS - 1), 0.5),
        "w_attn_o": nrm(ks[13], (L, ATTN_WIDTH, D), ATTN_WIDTH ** -0.5),
        "conv_dw_w": nrm(ks[14], (L, CONV_K, CONV_WIDTH), CONV_K ** -0.5),
        "conv_dw_b": nrm(ks[15], (L, CONV_WIDTH), 0.02),
        "conv_ln_g": 1.0 + nrm(ks[16], (L, CONV_WIDTH), 0.1),
        "conv_ln_b": nrm(ks[17], (L, CONV_WIDTH), 0.02),
        "w_conv_o": nrm(ks[18], (L, CONV_WIDTH, D), CONV_WIDTH ** -0.5),
        "sc_w": nrm(ks[19], (L, SC_K, SC_WIDTH), SC_K ** -0.5),
        "w_sc_o": nrm(ks[20], (L, SC_WIDTH, D), SC_WIDTH ** -0.5),
        "w_o": nrm(ks[21], (L, D, D), D ** -0.5),
        "w_router": nrm(ks[22], (L, D, N_EXPERTS), D ** -0.5),
        "w_e1": nrm(ks[23], (L, N_EXPERTS, D, EXPERT_FF), D ** -0.5),
        "w_e3": nrm(ks[24], (L, N_EXPERTS, D, EXPERT_FF), D ** -0.5),
        "w_e2": nrm(ks[25], (L, N_EXPERTS, EXPERT_FF, D), EXPERT_FF ** -0.5),
    }


def reference(x, c, ctx, c_ctx, w_ada, b_ada, g_norm1, g_norm2, w_in, b_in, g_q, g_k, rpb, w_attn_o,
              conv_dw_w, conv_dw_b, conv_ln_g, conv_ln_b, w_conv_o, sc_w, w_sc_o, w_o, w_router,
              w_e1, w_e3, w_e2):
    B, S, D = x.shape
    t = jnp.arange(S)
    row_pos, col_pos = t // GRID_W, t % GRID_W
    xc = ctx
    for l in range(DEPTH):
        last = l == DEPTH - 1
        mod = jnp.split(jax.nn.silu(c) @ w_ada[l] + b_ada[l], 6, axis=-1)
        n_mod = 2 if last else 6
        mod_c = jnp.split(jax.nn.silu(c_ctx) @ w_ada[l][:, :n_mod * D] + b_ada[l][:n_mod * D], n_mod, axis=-1)
        branch_params = (w_attn_o[l], conv_dw_w[l], conv_dw_b[l], conv_ln_g[l], conv_ln_b[l],
                         w_conv_o[l], sc_w[l], w_sc_o[l], w_o[l])

        hc = modulate(rmsnorm(xc, g_norm1[l]), mod_c[0], mod_c[1])
        if last:
            kv = hc @ w_in[l][:, ATTN_WIDTH:3 * ATTN_WIDTH] + b_in[l][ATTN_WIDTH:3 * ATTN_WIDTH]
            kc_raw, vc_raw = jnp.split(kv, 2, axis=-1)
            kc = rmsnorm(heads(kc_raw), g_k[l])
            vc = heads(vc_raw)
        else:
            pc = split_proj(hc @ w_in[l] + b_in[l])
            qc = rmsnorm(heads(pc[0]), g_q[l])
            kc = rmsnorm(heads(pc[1]), g_k[l])
            vc = heads(pc[2])
            mix_c = merge_branches(context_attention(qc, kc, vc), pc, *branch_params)
            xc_mid = xc + mod_c[2] * mix_c
            hc2 = modulate(rmsnorm(xc_mid, g_norm2[l]), mod_c[3], mod_c[4])
            xc_next = xc_mid + mod_c[5] * expert_choice_ffn(hc2, w_router[l], w_e1[l], w_e3[l], w_e2[l])

        h = modulate(rmsnorm(x, g_norm1[l]), mod[0][:, None], mod[1][:, None])
        p = split_proj(h @ w_in[l] + b_in[l])
        q = rmsnorm(heads(p[0]), g_q[l])
        k = rmsnorm(heads(p[1]), g_k[l])
        v = heads(p[2])
        q_rot = axial_rope(q, row_pos, col_pos)
        k_rot = axial_rope(k, row_pos, col_pos)
        attn = neighbourhood_attention(q_rot, k_rot, v, q, kc, vc, rpb[l])
        x = x + mod[2][:, None] * merge_branches(attn, p, *branch_params)
        h2 = modulate(rmsnorm(x, g_norm2[l]), mod[3][:, None], mod[4][:, None])
        x = x + mod[5][:, None] * expert_choice_ffn(h2, w_router[l], w_e1[l], w_e3[l], w_e2[l])

        if not last:
            xc = xc_next
    return x
```

```python
import numpy as np
import concourse.bass as bass
import concourse.mybir as mybir
from concourse.bass_utils import run_bass_kernel_spmd
from contextlib import ExitStack

F32 = mybir.dt.float32
BF16 = mybir.dt.bfloat16
I32 = mybir.dt.int32
ALU = mybir.AluOpType
AF = mybir.ActivationFunctionType

N = 4096
NCX = 256
D = 2048
NH = 16
DH = 64
AW = 1024
CW = 512
PW = 11776
NE = 16
FF = 1024
GW = 64
EPS = 1e-6
NEG = -30000.0
MW = 20


def I(name, **kw):
    return (name, kw)


class Buf:
    __slots__ = ("name", "w", "r")

    def __init__(self, name):
        self.name = name
        self.w = None
        self.r = {}


class Sched:
    ENG = ("pe", "act", "dve", "pool", "sp")
    DQ = ("sp", "pool", "act")

    def __init__(self, nc, es, ndma=8):
        self.nc = nc
        self.es = es
        self.prog = {e: [] for e in self.ENG}
        self.sems = {}
        self.cnt = {}
        for e in self.ENG:
            self.sems[e] = es.enter_context(nc.semaphore("s_" + e))
            self.cnt[e] = 0
        self.ndma = ndma
        self.dcnt = {}
        for q in self.DQ:
            self.dcnt[q] = 0
            for i in range(ndma):
                self.sems[("d", q, i)] = es.enter_context(nc.semaphore("d_%s_%d" % (q, i)))
        self.seen = {e: {} for e in self.ENG}
        self.nbuf = 0
        self.ninst = 0

    def buf(self, name=None):
        self.nbuf += 1
        return Buf(name or "b%d" % self.nbuf)

    def _wait(self, e, ev):
        key, val = ev
        if self.seen[e].get(key, 0) >= val:
            return
        self.seen[e][key] = val
        sem = self.sems[key]
        self.prog[e].append(lambda eng, sem=sem, val=val: eng.wait_ge(sem, val))

    def _deps(self, e, reads, writes):
        for b in reads:
            if b.w is not None:
                if not (b.w[0] == e and e == "pe"):
                    self._wait(e, b.w)
        for b in writes:
            if b.w is not None and b.w[0] != e:
                self._wait(e, b.w)
            for k, v in b.r.items():
                if k != e:
                    self._wait(e, (k, v))

    def _mark(self, ev, reads, writes):
        k, v = ev
        for b in reads:
            if b.r.get(k, 0) < v:
                b.r[k] = v
        for b in writes:
            b.w = ev
            b.r = {}

    def op(self, e, insts, reads=(), writes=()):
        if isinstance(insts, tuple):
            insts = [insts]
        self._deps(e, reads, writes)
        self.cnt[e] += 1
        val = self.cnt[e]
        sem = self.sems[e]
        self.ninst += len(insts)

        def run(eng, insts=insts, sem=sem):
            r = None
            for name, kw in insts:
                r = getattr(eng, name)(**kw)
            r.then_inc(sem, 1)
        self.prog[e].append(run)
        self._mark((e, val), reads, writes)

    def dma(self, q, out, in_, reads=(), writes=(), **kw):
        self.dma_fn(q, I("dma_start", out=out, in_=in_, **kw), reads, writes)

    def dma_fn(self, q, inst, reads=(), writes=()):
        self._deps(q, reads, writes)
        i = self.dcnt[q]
        self.dcnt[q] += 1
        slot = i % self.ndma
        val = 16 * (i // self.ndma + 1)
        key = ("d", q, slot)
        if val > 16:
            self._wait(q, (key, val - 16))
        sem = self.sems[key]
        self.ninst += 1
        self.prog[q].append(lambda eng, inst=inst, sem=sem: getattr(eng, inst[0])(**inst[1]).then_inc(sem, 16))
        self._mark((key, val), reads, writes)

    def all_events(self):
        evs = [(e, self.cnt[e]) for e in self.ENG if self.cnt[e] > 0]
        for q in self.DQ:
            for s in range(self.ndma):
                n = (self.dcnt[q] - 1 - s) // self.ndma + 1 if self.dcnt[q] > s else 0
                if n > 0:
                    evs.append((("d", q, s), 16 * n))
        return evs

    def barrier(self, engines=None):
        evs = self.all_events()
        for e in (engines or self.ENG):
            for ev in evs:
                if ev[0] != e:
                    self._wait(e, ev)

    def flush(self):
        nc = self.nc
        prog = self.prog
        if not any(prog[e] for e in self.ENG):
            return
        with nc.Block() as block:
            @block.tensor
            def _(eng):
                for f in prog["pe"]:
                    f(eng)

            @block.scalar
            def _(eng):
                for f in prog["act"]:
                    f(eng)

            @block.vector
            def _(eng):
                for f in prog["dve"]:
                    f(eng)

            @block.gpsimd
            def _(eng):
                for f in prog["pool"]:
                    f(eng)

            @block.sync
            def _(eng):
                for f in prog["sp"]:
                    f(eng)
        self.prog = {e: [] for e in self.ENG}

    def finish(self):
        for ev in self.all_events():
            if ev[0] != "sp":
                self._wait("sp", ev)
        self.flush()


class Stage:
    _uid = [0]

    def __init__(self, S, name):
        self.S = S
        Stage._uid[0] += 1
        self.name = "%s%d" % (name, Stage._uid[0])
        self.k = 0

    def __enter__(self):
        self.S.barrier()
        self.st = ExitStack()
        self.st.__enter__()
        return self

    def __exit__(self, *a):
        if a[0] is None:
            self.S.barrier()
            self.S.flush()
        return self.st.__exit__(*a)

    def sb(self, shape, dt, name=None):
        self.k += 1
        t = self.st.enter_context(self.S.nc.sbuf_tensor("%s_%s%d" % (self.name, name or "t", self.k), list(shape), dt))
        return t, self.S.buf()

    def ps(self, shape, dt, name=None):
        self.k += 1
        t = self.st.enter_context(self.S.nc.psum_tensor("%s_%s%d" % (self.name, name or "p", self.k), list(shape), dt))
        return t, self.S.buf()


def _rope_tables():
    quarter = 16
    freqs = 1.0 / (10000.0 ** (np.arange(quarter, dtype=np.float32) / quarter))
    t = np.arange(N)
    row, col = t // GW, t % GW
    cos = np.zeros((128, N), np.float32)
    sin = np.zeros((128, N), np.float32)
    for p in range(128):
        d = p % 64
        hh, i = d // 32, d % 32
        pos = (row if hh == 0 else col).astype(np.float32)
        ang = pos * freqs[i % 16]
        cos[p] = np.cos(ang)
        sin[p] = np.sin(ang)
    rperm = np.zeros((128, 128), np.float32)
    for m in range(128):
        i = (m % 64) % 32
        if i < 16:
            rperm[m + 16, m] = -1.0
        else:
            rperm[m - 16, m] = 1.0
    return cos, sin, rperm


ACASE_B = [0, 1, 2, 30, 31]


def _acase(b):
    return 0 if b == 0 else 1 if b == 1 else 3 if b == 30 else 4 if b == 31 else 2


def _attn_geometry():
    ro = np.zeros((5, 128, 5, 128), np.int64)
    co = np.zeros((5, 128, 5, 128), np.int64)
    va = np.zeros((5, 128, 5, 128), bool)
    kcol = np.arange(64)
    qcol = np.arange(64)
    qcs = np.clip(qcol - 8, 0, 48)
    cv = (kcol[:, None] >= qcs[None, :]) & (kcol[:, None] < qcs[None, :] + 16)
    cof = np.clip(kcol[:, None] - qcol[None, :] + 15, 0, 30)
    for ci, b in enumerate(ACASE_B):
        kstart = int(np.clip(2 * b - 4, 0, 54))
        for c in range(5):
            for kk in range(2):
                krow = kstart + 2 * c + kk
                for qr in range(2):
                    r = 2 * b + qr
                    rs = int(np.clip(r - 4, 0, 56))
                    rv = rs <= krow < rs + 8
                    ps = slice(kk * 64, kk * 64 + 64)
                    qs = slice(qr * 64, qr * 64 + 64)
                    ro[ci, ps, c, qs] = int(np.clip(krow - r + 7, 0, 14))
                    co[ci, ps, c, qs] = cof
                    va[ci, ps, c, qs] = cv & rv
    return ro, co, va


def _metafill():
    m = np.zeros((NE * 544, MW), np.float32)
    row = np.arange(NE * 544)
    slot = row % 544
    m[:, 0] = np.where(slot < 512, N, NCX) + (row % 128)
    return m.reshape(128, 68 * MW)


_CONST = {}


def _consts():
    if _CONST:
        return _CONST
    cos, sin, rperm = _rope_tables()
    ro, co, va = _attn_geometry()
    blk = np.zeros((128, 128), np.float32)
    blk[:64, :64] = 1.0 / 64
    blk[64:, 64:] = 1.0 / 64
    tri = np.triu(np.ones((128, 128), np.float32), 1)
    _CONST.update(dict(
        cosT=cos, sinT=sin, rperm=rperm, blk64=blk, ident=np.eye(128, dtype=np.float32),
        ones=np.ones((128, 128), np.float32), tri=tri,
        tokid=(np.arange(32)[None, :] * 128 + np.arange(128)[:, None]).astype(np.float32),
        ebase=np.tile((np.arange(16) * 544).astype(np.float32)[None, :], (128, 1)),
        metafill=_metafill(), dumpidx=(NE * 544 + np.arange(128)).astype(np.float32).reshape(128, 1),
        amask=np.where(va, 0.0, NEG).astype(np.float32).reshape(5, 128, 640),
        _ro=ro, _co=co))
    return _CONST


CONST_SHAPES = dict(cosT=[128, N], sinT=[128, N], rperm=[128, 128], blk64=[128, 128], ident=[128, 128],
                    ones=[128, 128], tri=[128, 128], tokid=[128, 32], amask=[5, 128, 640], ebase=[128, 16],
                    metafill=[128, 68 * MW], dumpidx=[128, 1])

IN_SHAPES = dict(
    x=[N, D], ctx=[NCX, D], cc=[2, D],
    w_ada=[2, D, 6 * D], b_ada=[2, 6 * D], g_norm1=[2, D], g_norm2=[2, D], w_in=[2, D, PW], b_in=[2, PW],
    g_q=[2, DH], g_k=[2, DH], rpbx=[2, NH, 5, 128, 640], w_attn_o=[2, AW, D], conv_dw_w=[2, 31, CW],
    conv_dw_b=[2, CW], conv_ln_g=[2, CW], conv_ln_b=[2, CW], w_conv_o=[2, CW, D], sc_w=[2, 3, CW],
    w_sc_o=[2, CW, D], w_o=[2, D, D], w_router=[2, D, NE], w_e1=[2, NE, D, FF], w_e3=[2, NE, D, FF],
    w_e2=[2, NE, FF, D])


class Rot:
    def __init__(self, items):
        self.items = items
        self.i = 0

    def next(self):
        it = self.items[self.i % len(self.items)]
        self.i += 1
        return it


class Prog:
    def __init__(self, dbg=()):
        self.dbg = set(dbg)
        nc = self.nc = bass.Bass("TRN2", target_bir_lowering=False)
        self.es = ExitStack()
        self.S = Sched(nc, self.es)
        self.n = [N, NCX]
        for k, shp in IN_SHAPES.items():
            setattr(self, k, nc.dram_tensor(k, list(shp), F32, kind="ExternalInput").ap())
        for k, shp in CONST_SHAPES.items():
            setattr(self, "c_" + k, nc.dram_tensor("c_" + k, list(shp), F32, kind="ExternalInput").ap())
        self.out = nc.dram_tensor("out", [N, D], F32, kind="ExternalOutput").ap()
        self.bufs = {}
        S = self.S
        self.MOD = self.scr("MOD", [2, 2, 6 * D], F32)
        self.XC = self.scr("XC", [NCX + 128, D], F32)
        self.XE = self.scr("XE", [N + 128, D], F32)
        self.X = [self.XE, self.XC]
        self.b_X = [S.buf(), S.buf()]
        self.QP = [self.scr("QP%d" % s, [AW, self.n[s]], BF16) for s in range(2)]
        self.QR = self.scr("QR", [AW, N], BF16)
        self.KR = self.scr("KR", [AW, N], BF16)
        self.KC = self.scr("KC", [AW, NCX], BF16)
        self.V = [self.scr("V%d" % s, [self.n[s], AW], BF16) for s in range(2)]
        self.UT = [self.scr("UT%d" % s, [CW, self.n[s]], BF16) for s in range(2)]
        self.SCB = [self.scr("SCB%d" % s, [CW, self.n[s]], F32) for s in range(2)]
        self.CX = [self.scr("CX%d" % s, [CW, self.n[s]], F32) for s in range(2)]
        self.GT = [self.scr("GT%d" % s, [3 * D, self.n[s]], BF16) for s in range(2)]
        self.ATT = [self.scr("ATT%d" % s, [self.n[s], AW], BF16) for s in range(2)]
        self.SBT = [self.scr("SBT%d" % s, [CW, self.n[s]], BF16) for s in range(2)]
        self.SCT = [self.scr("SCT%d" % s, [CW, self.n[s]], BF16) for s in range(2)]
        self.H2 = [self.scr("H2%d" % s, [self.n[s] + 128, D], BF16) for s in range(2)]
        self.META = self.scr("META", [NE * 544 + 128, MW], F32)

    def scr(self, name, shape, dt):
        kind = "ExternalOutput" if name in self.dbg else "Internal"
        t = self.nc.dram_tensor(name, list(shape), dt, kind=kind).ap()
        self.bufs[name] = self.S.buf(name)
        return t

    def b(self, name):
        return self.bufs[name]


def stage_mod(P):
    S = P.S
    with Stage(S, "mod") as st:
        cT32, b_c32 = st.sb([128, 16, 2], F32)
        cT, b_cT = st.sb([128, 16, 2], BF16)
        for j in range(2):
            S.dma("sp", cT32[:, :, j], P.cc[j, :].rearrange("(kc p) -> p kc", p=128), writes=[b_c32],
                  allow_slow_non_contiguous=True)
        S.op("act", I("activation", out=cT[:], in_=cT32[:], func=AF.Silu), reads=[b_c32], writes=[b_cT])
        wbs = Rot([st.sb([128, 16, 512], BF16) for _ in range(2)])
        pss = Rot([st.ps([128, 512], F32) for _ in range(2)])
        bias2, b_bias2 = st.sb([2, 6 * D], F32)
        res, b_res = st.sb([2, 6 * D], F32)
        for l in range(2):
            for j in range(2):
                S.dma("sp", bias2[j:j + 1, :], P.b_ada[l:l + 1, :], writes=[b_bias2])
            for ch in range(24):
                wb, b_wb = wbs.next()
                pm, b_pm = pss.next()
                S.dma("pool", wb[:], P.w_ada[l, :, ch * 512:(ch + 1) * 512].rearrange("(kc p) n -> p kc n", p=128),
                      writes=[b_wb])
                S.op("pe", [I("matmul", out=pm[0:2, :], lhsT=cT[:, kc, :], rhs=wb[:, kc, :], start=(kc == 0),
                               stop=(kc == 15)) for kc in range(16)], reads=[b_cT, b_wb], writes=[b_pm])
                S.op("dve", I("tensor_tensor", out=res[0:2, ch * 512:(ch + 1) * 512], in0=pm[0:2, :],
                              in1=bias2[0:2, ch * 512:(ch + 1) * 512], op=ALU.add),
                     reads=[b_pm, b_bias2], writes=[b_res])
            S.dma("sp", P.MOD[l], res[:], reads=[b_res], writes=[P.b("MOD")])


def load_pp(S, q, tile_ap, b_tile, src_row, ncol, reads=()):
    S.dma(q, tile_ap, src_row.rearrange("(c p) -> p c", p=128), reads=list(reads), writes=[b_tile],
          allow_slow_non_contiguous=True)


def stage_inproj(P, l, s, xsrc=None):
    S = P.S
    n = P.n[s]
    lat = (s == 0)
    X = xsrc if xsrc is not None else P.X[s]
    b_X = P.b_X[s]
    G = min(n, 2048)
    npass = n // G
    W = min(512, G)
    ntt = G // W
    chunks = list(range(23)) if not (s == 1 and l == 1) else [2, 3, 4, 5]
    with Stage(S, "ip") as st:
        hT, b_hT = st.sb([128, 16, G], BF16)
        wbs = [st.sb([128, 16, 512], BF16) for _ in range(3)]
        xts = Rot([st.sb([128, D], F32) for _ in range(2)])
        xss = Rot([st.sb([128, D], BF16) for _ in range(2)])
        junk, b_junk = st.sb([128, D], BF16)
        sss = Rot([st.sb([128, 1], F32) for _ in range(2)])
        rss = Rot([st.sb([128, 1], F32) for _ in range(2)])
        idf, b_idf = st.sb([128, 128], F32)
        idb, b_idb = st.sb([128, 128], BF16)
        blk, b_blk = st.sb([128, 128], BF16)
        rperm, b_rperm = st.sb([128, 128], BF16)
        A1, b_A1 = st.sb([128, 16], F32)
        B1, b_B1 = st.sb([128, 16], F32)
        g1, b_g1 = st.sb([128, 16], F32)
        bP, b_bP = st.sb([128, 92], F32)
        bvbc, b_bvbc = st.sb([128, AW], F32)
        gq, b_gq = st.sb([128, 1], F32)
        gk, b_gk = st.sb([128, 1], F32)
        cstabs = Rot([(st.sb([128, 512], F32), st.sb([128, 512], F32)) for _ in range(2)])
        cs_cur = None
        pend = []

        def flush_pend():
            if len(pend) >= 2:
                pend[-2][1]()
                pend[-1][0]()
                pend[-1][1]()
            elif len(pend) == 1:
                pend[-1][0]()
                pend[-1][1]()
            del pend[:]
        pT, b_pT = st.ps([128, 16, 128], BF16)
        paccs = Rot([st.ps([128, 512], F32) for _ in range(3)])
        pauxs = Rot([st.ps([128, 512], F32) for _ in range(3)])
        f32t = Rot([st.sb([128, 512], F32) for _ in range(18)])
        bft = Rot([st.sb([128, 512], BF16) for _ in range(14)])

        S.dma("sp", idf[:], P.c_ident, writes=[b_idf])
        S.op("dve", I("tensor_copy", out=idb[:], in_=idf[:]), reads=[b_idf], writes=[b_idb])
        S.dma("pool", blk[:], P.c_blk64, writes=[b_blk])
        S.dma("pool", rperm[:], P.c_rperm, writes=[b_rperm])
        load_pp(S, "sp", B1[:], b_B1, P.MOD[l, s, 0:D], 16, reads=[P.b("MOD")])
        load_pp(S, "sp", A1[:], b_A1, P.MOD[l, s, D:2 * D], 16, reads=[P.b("MOD")])
        load_pp(S, "sp", g1[:], b_g1, P.g_norm1[l, :], 16)
        S.op("dve", I("scalar_tensor_tensor", out=A1[:], in0=A1[:], scalar=1.0, in1=g1[:], op0=ALU.add,
                      op1=ALU.mult), reads=[b_A1, b_g1], writes=[b_A1])
        load_pp(S, "sp", bP[:], b_bP, P.b_in[l, :], 92)
        S.dma("sp", bvbc[:], P.b_in[l, 2 * AW:3 * AW].partition_broadcast(128), writes=[b_bvbc])
        for (gt, b_gt, src, sc) in ((gq, b_gq, P.g_q, 0.125), (gk, b_gk, P.g_k, 1.0)):
            for hh in range(2):
                S.dma("sp", gt[hh * 64:(hh + 1) * 64, 0:1], src[l, :].rearrange("(p o) -> p o", o=1),
                      writes=[b_gt], allow_slow_non_contiguous=True)
            S.op("act", I("mul", out=gt[:], in_=gt[:], mul=sc), reads=[b_gt], writes=[b_gt])
        mhalf, b_mhalf = st.sb([128, 512], F32)
        S.op("pool", I("memset", ap=mhalf[:], constant=-0.5), writes=[b_mhalf])
        bPg, b_bPg = st.sb([128, 16], F32)
        S.op("dve", I("tensor_scalar", out=bPg[:, 0:8], in0=bP[:, 0:8], scalar1=gq[:, 0:1], scalar2=None,
                      op0=ALU.mult), reads=[b_bP, b_gq], writes=[b_bPg])
        S.op("dve", I("tensor_scalar", out=bPg[:, 8:16], in0=bP[:, 8:16], scalar1=gk[:, 0:1], scalar2=None,
                      op0=ALU.mult), reads=[b_bP, b_gk], writes=[b_bPg])

        def load_w(c):
            wb, b_wb = wbs[c % 3]
            S.dma("pool", wb[:], P.w_in[l, :, c * 512:(c + 1) * 512].rearrange("(kc p) n -> p kc n", p=128),
                  writes=[b_wb])

        def mm_fm(c, i, tt, pa, b_pa):
            wb, b_wb = wbs[c % 3]
            S.op("pe", [I("matmul", out=pa[:, :W], lhsT=wb[:, kc, i * 128:(i + 1) * 128],
                           rhs=hT[:, kc, tt * W:(tt + 1) * W], start=(kc == 0), stop=(kc == 15))
                        for kc in range(16)], reads=[b_wb, b_hT], writes=[b_pa])

        for ps_ in range(npass):
            for ti in range(G // 128):
                t0 = ps_ * G + ti * 128
                xt, b_xt = xts.next()
                xs, b_xs = xss.next()
                ss, b_ss = sss.next()
                rs, b_rs = rss.next()
                S.dma("sp", xt[:], X[t0:t0 + 128, :], reads=[b_X], writes=[b_xt])
                S.op("act", I("activation", out=junk[:], in_=xt[:], func=AF.Square, accum_out=ss[:]),
                     reads=[b_xt], writes=[b_junk, b_ss])
                S.op("act", I("activation", out=rs[:], in_=ss[:], func=AF.Sqrt, scale=1.0 / D, bias=EPS),
                     reads=[b_ss], writes=[b_rs])
                S.op("dve", I("reciprocal", out=rs[:], in_=rs[:]), reads=[b_rs], writes=[b_rs])
                S.op("dve", I("tensor_scalar", out=xs[:], in0=xt[:], scalar1=rs[:, 0:1], scalar2=None,
                              op0=ALU.mult), reads=[b_xt, b_rs], writes=[b_xs])
                S.op("pe", [I("transpose", out=pT[:, kc, :], in_=xs[:, kc * 128:(kc + 1) * 128], identity=idb[:])
                            for kc in range(16)], reads=[b_xs, b_idb], writes=[b_pT])
                S.op("dve", [I("tensor_scalar", out=hT[:, kc, ti * 128:(ti + 1) * 128], in0=pT[:, kc, :],
                               scalar1=A1[:, kc:kc + 1], scalar2=B1[:, kc:kc + 1], op0=ALU.mult, op1=ALU.add)
                             for kc in range(0, 8)], reads=[b_pT, b_A1, b_B1], writes=[b_hT])
                S.op("act", [I("activation", out=hT[:, kc, ti * 128:(ti + 1) * 128], in_=pT[:, kc, :],
                               func=AF.Identity, scale=A1[:, kc:kc + 1], bias=B1[:, kc:kc + 1])
                             for kc in range(8, 16)], reads=[b_pT, b_A1, b_B1], writes=[b_hT])
            load_w(chunks[0])
            for ci, c in enumerate(chunks):
                if ci + 1 < len(chunks):
                    load_w(chunks[ci + 1])
                wb, b_wb = wbs[c % 3]
                if c >= 4 and pend:
                    flush_pend()
                if c in (4, 5):
                    for ti in range(G // 128):
                        t0 = ps_ * G + ti * 128
                        pa, b_pa = paccs.next()
                        S.op("pe", [I("matmul", out=pa[:], lhsT=hT[:, kc, ti * 128:(ti + 1) * 128], rhs=wb[:, kc, :],
                                       start=(kc == 0), stop=(kc == 15)) for kc in range(16)],
                             reads=[b_wb, b_hT], writes=[b_pa])
                        ob, b_ob = bft.next()
                        S.op("dve", I("tensor_tensor", out=ob[:], in0=pa[:], in1=bvbc[:, (c - 4) * 512:(c - 3) * 512],
                                      op=ALU.add), reads=[b_pa, b_bvbc], writes=[b_ob])
                        S.dma("sp", P.V[s][t0:t0 + 128, (c - 4) * 512:(c - 3) * 512], ob[:], reads=[b_ob],
                              writes=[P.b("V%d" % s)])
                    continue
                if c in (6, 9):
                    continue
                for tt in range(ntt):
                    t0 = ps_ * G + tt * W
                    if c < 4 and lat:
                        cs_cur = cstabs.next()
                        S.dma("sp", cs_cur[0][0][:, :W], P.c_cosT[:, t0:t0 + W], writes=[cs_cur[0][1]])
                        S.dma("sp", cs_cur[1][0][:, :W], P.c_sinT[:, t0:t0 + W], writes=[cs_cur[1][1]])
                    for i in range(4):
                        fc = 4 * c + i
                        pa, b_pa = paccs.next()
                        mm_fm(c, i, tt, pa, b_pa)
                        if c < 4:
                            isq = c < 2
                            gt, b_gt = (gq, b_gq) if isq else (gk, b_gk)
                            raw, b_raw = f32t.next()
                            sq, b_sq = bft.next()
                            rstd, b_rstd = f32t.next()
                            qn, b_qn = f32t.next()
                            qb, b_qb = bft.next()
                            S.op("act", I("activation", out=raw[:, :W], in_=pa[:, :W], func=AF.Identity,
                                          bias=bPg[:, fc:fc + 1], scale=gt[:, 0:1]), reads=[b_pa, b_bPg, b_gt],
                                 writes=[b_raw])
                            S.op("act", I("activation", out=sq[:, :W], in_=pa[:, :W], func=AF.Square,
                                          bias=bP[:, fc:fc + 1], scale=1.0), reads=[b_pa, b_bP], writes=[b_sq])
                            rows = slice((fc % 8) * 128, (fc % 8 + 1) * 128)

                            def step2(isq=isq, gt=gt, b_gt=b_gt, raw=raw, b_raw=b_raw, sq=sq, b_sq=b_sq, rstd=rstd,
                                      b_rstd=b_rstd, qn=qn, b_qn=b_qn, rows=rows, t0=t0, qb=qb, b_qb=b_qb):
                                px, b_px = pauxs.next()
                                S.op("pe", I("matmul", out=px[:, :W], lhsT=blk[:], rhs=sq[:, :W], start=True,
                                             stop=True), reads=[b_sq, b_blk], writes=[b_px])
                                S.op("act", I("activation", out=rstd[:, :W], in_=px[:, :W], func=AF.Identity,
                                              bias=EPS, scale=1.0), reads=[b_px], writes=[b_rstd])
                                S.op("pool", I("tensor_tensor", out=rstd[:, :W], in0=rstd[:, :W], in1=mhalf[:, :W],
                                               op=ALU.pow), reads=[b_rstd, b_mhalf], writes=[b_rstd])
                                S.op("dve", I("tensor_tensor", out=qn[:, :W], in0=raw[:, :W], in1=rstd[:, :W],
                                              op=ALU.mult), reads=[b_raw, b_rstd], writes=[b_qn])
                                S.op("act", I("copy", out=qb[:, :W], in_=qn[:, :W]), reads=[b_qn], writes=[b_qb])
                                if isq:
                                    S.dma("sp", P.QP[s][rows, t0:t0 + W], qb[:, :W], reads=[b_qb],
                                          writes=[P.b("QP%d" % s)])
                                elif not lat:
                                    S.dma("sp", P.KC[rows, t0:t0 + W], qb[:, :W], reads=[b_qb], writes=[P.b("KC")])

                            def step3(isq=isq, qn=qn, b_qn=b_qn, rows=rows, t0=t0, cs=cs_cur, qb=qb, b_qb=b_qb):
                                if not lat:
                                    return
                                (cos_t, b_cos), (sin_t, b_sin) = cs
                                py, b_py = pauxs.next()
                                t1, b_t1 = f32t.next()
                                t2, b_t2 = f32t.next()
                                ob, b_ob = bft.next()
                                S.op("pe", I("matmul", out=py[:, :W], lhsT=rperm[:], rhs=qb[:, :W], start=True,
                                             stop=True), reads=[b_qb, b_rperm], writes=[b_py])
                                S.op("pool", I("tensor_tensor", out=t1[:, :W], in0=qn[:, :W], in1=cos_t[:, :W],
                                               op=ALU.mult), reads=[b_qn, b_cos], writes=[b_t1])
                                S.op("dve", I("tensor_tensor", out=t2[:, :W], in0=py[:, :W], in1=sin_t[:, :W],
                                              op=ALU.mult), reads=[b_py, b_sin], writes=[b_t2])
                                S.op("dve", I("tensor_tensor", out=ob[:, :W], in0=t1[:, :W], in1=t2[:, :W],
                                              op=ALU.add), reads=[b_t1, b_t2], writes=[b_ob])
                                dst, nm = (P.QR, "QR") if isq else (P.KR, "KR")
                                S.dma("sp", dst[rows, t0:t0 + W], ob[:, :W], reads=[b_ob], writes=[P.b(nm)])

                            pend.append([step2, step3])
                            if len(pend) >= 2:
                                pend[-2][0]()
                            if len(pend) >= 3:
                                pend[-3][1]()
                                pend.pop(0)
                        elif c == 7:
                            pb, b_pb = paccs.next()
                            mm_fm(6, i, tt, pb, b_pb)
                            a, b_a = f32t.next()
                            sg, b_sg = f32t.next()
                            u, b_u = bft.next()
                            S.op("act", I("activation", out=a[:, :W], in_=pb[:, :W], func=AF.Identity,
                                          bias=bP[:, 24 + i:25 + i], scale=1.0), reads=[b_pb, b_bP], writes=[b_a])
                            S.op("act", I("activation", out=sg[:, :W], in_=pa[:, :W], func=AF.Sigmoid,
                                          bias=bP[:, fc:fc + 1], scale=1.0), reads=[b_pa, b_bP], writes=[b_sg])
                            S.op("dve", I("tensor_tensor", out=u[:, :W], in0=a[:, :W], in1=sg[:, :W], op=ALU.mult),
                                 reads=[b_a, b_sg], writes=[b_u])
                            S.dma("sp", P.UT[s][i * 128:(i + 1) * 128, t0:t0 + W], u[:, :W], reads=[b_u],
                                  writes=[P.b("UT%d" % s)])
                        elif c == 8:
                            a, b_a = f32t.next()
                            S.op("act", I("activation", out=a[:, :W], in_=pa[:, :W], func=AF.Identity,
                                          bias=bP[:, fc:fc + 1], scale=1.0), reads=[b_pa, b_bP], writes=[b_a])
                            S.dma("sp", P.SCB[s][i * 128:(i + 1) * 128, t0:t0 + W], a[:, :W], reads=[b_a],
                                  writes=[P.b("SCB%d" % s)])
                        elif c == 10:
                            pb, b_pb = paccs.next()
                            mm_fm(9, i, tt, pb, b_pb)
                            a, b_a = f32t.next()
                            u, b_u = f32t.next()
                            S.op("act", I("activation", out=a[:, :W], in_=pb[:, :W], func=AF.Identity,
                                          bias=bP[:, 36 + i:37 + i], scale=1.0), reads=[b_pb, b_bP], writes=[b_a])
                            S.op("dve", I("scalar_tensor_tensor", out=u[:, :W], in0=pa[:, :W],
                                          scalar=bP[:, fc:fc + 1], in1=a[:, :W], op0=ALU.add, op1=ALU.mult),
                                 reads=[b_pa, b_a, b_bP], writes=[b_u])
                            S.dma("sp", P.CX[s][i * 128:(i + 1) * 128, t0:t0 + W], u[:, :W], reads=[b_u],
                                  writes=[P.b("CX%d" % s)])
                        else:
                            ob, b_ob = bft.next()
                            S.op("act", I("activation", out=ob[:, :W], in_=pa[:, :W], func=AF.Sigmoid,
                                          bias=bP[:, fc:fc + 1], scale=1.0), reads=[b_pa, b_bP], writes=[b_ob])
                            r0 = (fc - 44) * 128
                            S.dma("sp", P.GT[s][r0:r0 + 128, t0:t0 + W], ob[:, :W], reads=[b_ob],
                                  writes=[P.b("GT%d" % s)])


_SHARED = {}


def prep_shared(inputs):
    C = _consts()
    sh = {}
    for k in IN_SHAPES:
        if k in ("x", "ctx", "cc", "rpbx"):
            continue
        sh[k] = np.ascontiguousarray(np.asarray(inputs[k], dtype=np.float32))
    rpb = np.asarray(inputs["rpb"], dtype=np.float32)
    ro = C["_ro"].reshape(5, 128, 640)
    co = C["_co"].reshape(5, 128, 640)
    sh["rpbx"] = np.ascontiguousarray(rpb[:, :, ro, co])
    for k in CONST_SHAPES:
        sh["c_" + k] = np.ascontiguousarray(C[k])
    return sh


def prep_core(inputs, b, sh):
    m = dict(sh)
    m["x"] = np.ascontiguousarray(np.asarray(inputs["x"][b], dtype=np.float32))
    m["ctx"] = np.ascontiguousarray(np.asarray(inputs["ctx"][b], dtype=np.float32))
    m["cc"] = np.ascontiguousarray(np.stack([np.asarray(inputs["c"][b]), np.asarray(inputs["c_ctx"])]).astype(np.float32))
    return m


def stage_attn(P, l, s):
    if s == 1:
        return stage_attn_ctx(P, l)
    S = P.S
    with Stage(S, "at") as st:
        Es = Rot([st.sb([128, 7, 128], BF16) for _ in range(4)])
        sbts = Rot([st.sb([128, 640], F32) for _ in range(4)])
        recs = Rot([st.sb([128, 1], F32) for _ in range(4)])
        pS1s = Rot([st.ps([128, 4, 128], F32) for _ in range(3)])
        pS2s = Rot([st.ps([128, 3, 128], F32) for _ in range(3)])
        pOs = Rot([st.ps([128, 65], F32) for _ in range(2)])
        amask, b_amask = st.sb([128, 5, 640], F32)
        S.dma("sp", amask[:], P.c_amask.rearrange("c p f -> p c f"), writes=[b_amask])
        sets = []
        for i in range(2):
            d = dict(kcT=st.sb([64, NCX], BF16), vca=st.sb([128, 2, 65], BF16), qr=st.sb([64, N], BF16),
                     qp=st.sb([64, N], BF16), kr=st.sb([64, N], BF16), va=st.sb([128, 32, 65], BF16),
                     bias=st.sb([128, 5, 640], F32), osb=st.sb([128, 32, 64], BF16))
            S.op("pool", I("memset", ap=d["vca"][0][:, :, 64:65], constant=1.0), writes=[d["vca"][1]])
            S.op("pool", I("memset", ap=d["va"][0][:, :, 64:65], constant=1.0), writes=[d["va"][1]])
            sets.append(d)

        def load(h):
            d = sets[h % 2]
            hs = slice(h * 64, (h + 1) * 64)
            S.dma("sp", d["kcT"][0][:], P.KC[hs, :], reads=[P.b("KC")], writes=[d["kcT"][1]])
            S.dma("sp", d["vca"][0][:, :, 0:64], P.V[1][:, hs].rearrange("(c p) d -> p c d", p=128),
                  reads=[P.b("V1")], writes=[d["vca"][1]])
            S.dma("sp", d["kr"][0][:], P.KR[hs, :], reads=[P.b("KR")], writes=[d["kr"][1]])
            S.dma("sp", d["qr"][0][:], P.QR[hs, :], reads=[P.b("QR")], writes=[d["qr"][1]])
            S.dma("sp", d["qp"][0][:], P.QP[0][hs, :], reads=[P.b("QP0")], writes=[d["qp"][1]])
            S.dma("sp", d["va"][0][:, :, 0:64], P.V[0][:, hs].rearrange("(t p) d -> p t d", p=128),
                  reads=[P.b("V0")], writes=[d["va"][1]])
            S.dma("sp", d["bias"][0][:], P.rpbx[l, h].rearrange("c p f -> p c f"), writes=[d["bias"][1]])
            S.op("pool", I("tensor_tensor", out=d["bias"][0][:], in0=d["bias"][0][:], in1=amask[:], op=ALU.add),
                 reads=[d["bias"][1], b_amask], writes=[d["bias"][1]])

        load(0)
        for h in range(NH):
            if h + 1 < NH:
                load(h + 1)
            d = sets[h % 2]
            hs = slice(h * 64, (h + 1) * 64)
            kcT, b_kcT = d["kcT"]
            vca, b_vca = d["vca"]
            qr, b_qr = d["qr"]
            qp, b_qp = d["qp"]
            kr, b_kr = d["kr"]
            va, b_va = d["va"]
            bias, b_bias = d["bias"]
            osb, b_osb = d["osb"]
            def qk(b):
                pS1, b_pS1 = pS1s.next()
                pS2, b_pS2 = pS2s.next()
                ks = int(np.clip(2 * b - 4, 0, 54))
                qs = slice(b * 128, (b + 1) * 128)
                S.op("pe", [I("matmul", out=pS1[:, c, :], lhsT=kr[:, (ks + 2 * c) * 64:(ks + 2 * c + 2) * 64],
                               rhs=qr[:, qs], start=True, stop=True) for c in range(4)],
                     reads=[b_kr, b_qr], writes=[b_pS1])
                S.op("pe", [I("matmul", out=pS2[:, 0, :], lhsT=kr[:, (ks + 8) * 64:(ks + 10) * 64], rhs=qr[:, qs],
                               start=True, stop=True)] +
                           [I("matmul", out=pS2[:, 1 + c, :], lhsT=kcT[:, c * 128:(c + 1) * 128], rhs=qp[:, qs],
                              start=True, stop=True) for c in range(2)],
                     reads=[b_kr, b_qr, b_kcT, b_qp], writes=[b_pS2])
                return pS1, b_pS1, pS2, b_pS2

            ahead = [qk(0), qk(1)]
            for b in range(32):
                pS1, b_pS1, pS2, b_pS2 = ahead.pop(0)
                E, b_E = Es.next()
                sbt, b_sbt = sbts.next()
                rec, b_rec = recs.next()
                pO, b_pO = pOs.next()
                case = _acase(b)
                ks = int(np.clip(2 * b - 4, 0, 54))
                S.op("dve", I("tensor_tensor", out=sbt[:, 0:512], in0=pS1[:].rearrange("p c q -> p (c q)"),
                              in1=bias[:, case, 0:512], op=ALU.add), reads=[b_pS1, b_bias], writes=[b_sbt])
                S.op("dve", I("tensor_tensor", out=sbt[:, 512:640], in0=pS2[:, 0, :], in1=bias[:, case, 512:640],
                              op=ALU.add), reads=[b_pS2, b_bias], writes=[b_sbt])
                S.op("act", I("activation", out=E[:, 0:5, :].rearrange("p c q -> p (c q)"), in_=sbt[:], func=AF.Exp),
                     reads=[b_sbt], writes=[b_E])
                S.op("act", I("activation", out=E[:, 5:7, :], in_=pS2[:, 1:3, :], func=AF.Exp), reads=[b_pS2],
                     writes=[b_E])
                if b + 2 < 32:
                    ahead.append(qk(b + 2))
                mm = [I("matmul", out=pO[:], lhsT=E[:, c, :], rhs=va[:, ks // 2 + c, :], start=(c == 0), stop=False)
                      for c in range(5)]
                mm += [I("matmul", out=pO[:], lhsT=E[:, 5 + c, :], rhs=vca[:, c, :], start=False, stop=(c == 1))
                       for c in range(2)]
                S.op("pe", mm, reads=[b_E, b_va, b_vca], writes=[b_pO])
                S.op("dve", I("reciprocal", out=rec[:], in_=pO[:, 64:65]), reads=[b_pO], writes=[b_rec])
                S.op("dve", I("tensor_scalar", out=osb[:, b, :], in0=pO[:, 0:64], scalar1=rec[:, 0:1], scalar2=None,
                              op0=ALU.mult), reads=[b_pO, b_rec], writes=[b_osb])
            S.dma("pool", P.ATT[0][:, hs].rearrange("(b p) f -> p b f", p=128), osb[:], reads=[b_osb],
                  writes=[P.b("ATT0")])


def stage_attn_ctx(P, l):
    S = P.S
    with Stage(S, "ac") as st:
        kcT, b_kcT = st.sb([64, NCX], BF16)
        vca, b_vca = st.sb([128, 2, 65], BF16)
        S.op("pool", I("memset", ap=vca[:, :, 64:65], constant=1.0), writes=[b_vca])
        Es = Rot([st.sb([128, 2, 128], BF16) for _ in range(2)])
        recs = Rot([st.sb([128, 1], F32) for _ in range(2)])
        pCs = Rot([st.ps([128, 2, 128], F32) for _ in range(2)])
        pOs = Rot([st.ps([128, 65], F32) for _ in range(2)])
        qp, b_qp = st.sb([64, NCX], BF16)
        osb, b_osb = st.sb([128, 2, 64], BF16)
        for h in range(NH):
            hs = slice(h * 64, (h + 1) * 64)
            S.dma("sp", kcT[:], P.KC[hs, :], reads=[P.b("KC")], writes=[b_kcT])
            S.dma("sp", vca[:, :, 0:64], P.V[1][:, hs].rearrange("(c p) d -> p c d", p=128), reads=[P.b("V1")],
                  writes=[b_vca])
            S.dma("sp", qp[:], P.QP[1][hs, :], reads=[P.b("QP1")], writes=[b_qp])
            for rb in range(2):
                E, b_E = Es.next()
                pC, b_pC = pCs.next()
                pO, b_pO = pOs.next()
                rec, b_rec = recs.next()
                qpsl = qp[:, rb * 128:(rb + 1) * 128]
                S.op("pe", [I("matmul", out=pC[:, c, :], lhsT=kcT[:, c * 128:(c + 1) * 128], rhs=qpsl, start=True,
                               stop=True) for c in range(2)], reads=[b_kcT, b_qp], writes=[b_pC])
                S.op("act", I("activation", out=E[:], in_=pC[:], func=AF.Exp), reads=[b_pC], writes=[b_E])
                S.op("pe", [I("matmul", out=pO[:], lhsT=E[:, c, :], rhs=vca[:, c, :], start=(c == 0), stop=(c == 1))
                            for c in range(2)], reads=[b_E, b_vca], writes=[b_pO])
                S.op("dve", I("reciprocal", out=rec[:], in_=pO[:, 64:65]), reads=[b_pO], writes=[b_rec])
                S.op("dve", I("tensor_scalar", out=osb[:, rb, :], in0=pO[:, 0:64], scalar1=rec[:, 0:1], scalar2=None,
                              op0=ALU.mult), reads=[b_pO, b_rec], writes=[b_osb])
            S.dma("sp", P.ATT[1][:, hs].rearrange("(b p) f -> p b f", p=128), osb[:], reads=[b_osb],
                  writes=[P.b("ATT1")])


def stage_conv(P, l, s):
    S = P.S
    n = P.n[s]
    W = min(512, n)
    with Stage(S, "cv") as st:
        dw, b_dw = st.sb([128, 4, 31], F32)
        scw, b_scw = st.sb([128, 4, 3], F32)
        pp, b_pp = st.sb([128, 4, 4], F32)
        for k in range(31):
            S.dma("sp", dw[:, :, k], P.conv_dw_w[l, k, :].rearrange("(c p) -> p c", p=128), writes=[b_dw],
                  allow_slow_non_contiguous=True)
        for k in range(3):
            S.dma("sp", scw[:, :, k], P.sc_w[l, k, :].rearrange("(c p) -> p c", p=128), writes=[b_scw],
                  allow_slow_non_contiguous=True)
        for i, src in enumerate((P.conv_dw_b, P.conv_ln_g, P.conv_ln_b)):
            S.dma("sp", pp[:, i, :], src[l, :].rearrange("(c p) -> p c", p=128), writes=[b_pp],
                  allow_slow_non_contiguous=True)
        ones, b_ones = st.sb([128, 128], F32)
        S.dma("sp", ones[:], P.c_ones, writes=[b_ones])
        ups = [st.sb([128, n + 30], F32) for _ in range(2)]
        for (u, b_u) in ups:
            S.op("pool", I("memset", ap=u[:], constant=0.0), writes=[b_u])
        ubs = [st.sb([128, n + 30], BF16) for _ in range(2)]
        for (u, b_u) in ubs:
            S.op("pool", I("memset", ap=u[:], constant=0.0), writes=[b_u])
        idf, b_idf = st.sb([128, 128], F32)
        S.dma("sp", idf[:], P.c_ident, writes=[b_idf])
        dg, b_dg = st.sb([128, 4, 31, 128], BF16)
        S.op("dve", [I("tensor_scalar", out=dg[:, c, k, :], in0=idf[:], scalar1=dw[:, c, k:k + 1], scalar2=None,
                       op0=ALU.mult) for c in range(4) for k in range(31)], reads=[b_idf, b_dw], writes=[b_dg])
        cv, b_cv = st.sb([128, 4, n], F32)
        b_cvc = [S.buf() for _ in range(4)]
        pcs = Rot([st.ps([128, 512], F32) for _ in range(3)])
        for c in range(4):
            ub, b_ub = ubs[c % 2]
            S.dma("sp", ub[:, 15:15 + n], P.UT[s][c * 128:(c + 1) * 128, :], reads=[P.b("UT%d" % s)], writes=[b_ub])
            for tt in range(n // W):
                pc, b_pc = pcs.next()
                S.op("pe", [I("matmul", out=pc[:, :W], lhsT=dg[:, c, k, :], rhs=ub[:, tt * W + k:tt * W + k + W],
                               start=(k == 0), stop=(k == 30)) for k in range(31)], reads=[b_dg, b_ub], writes=[b_pc])
                S.op("act", I("activation", out=cv[:, c, tt * W:(tt + 1) * W], in_=pc[:, :W], func=AF.Identity,
                              bias=pp[:, 0, c:c + 1], scale=1.0), reads=[b_pc, b_pp], writes=[b_cvc[c]])
        f32t = Rot([st.sb([128, 512], F32) for _ in range(10)])
        bft = Rot([st.sb([128, 512], BF16) for _ in range(3)])
        pst = Rot([st.ps([128, 512], F32) for _ in range(4)])
        for tt in range(n // W):
            ts = slice(tt * W, (tt + 1) * W)
            p1, b_p1 = pst.next()
            p2, b_p2 = pst.next()
            S.op("pe", [I("matmul", out=p1[:, :W], lhsT=ones[:], rhs=cv[:, c, ts], start=(c == 0), stop=(c == 3))
                        for c in range(4)], reads=b_cvc + [b_ones], writes=[b_p1])
            sqs = []
            for c in range(4):
                sq, b_sq = f32t.next()
                S.op("act", I("activation", out=sq[:, :W], in_=cv[:, c, ts], func=AF.Square), reads=[b_cvc[c]],
                     writes=[b_sq])
                sqs.append((sq, b_sq))
            S.op("pe", [I("matmul", out=p2[:, :W], lhsT=ones[:], rhs=sqs[c][0][:, :W], start=(c == 0), stop=(c == 3))
                        for c in range(4)], reads=[q[1] for q in sqs] + [b_ones], writes=[b_p2])
            mean, b_mean = f32t.next()
            msq, b_msq = f32t.next()
            var, b_var = f32t.next()
            S.op("act", I("mul", out=mean[:, :W], in_=p1[:, :W], mul=1.0 / CW), reads=[b_p1], writes=[b_mean])
            S.op("dve", I("tensor_tensor", out=msq[:, :W], in0=mean[:, :W], in1=mean[:, :W], op=ALU.mult),
                 reads=[b_mean], writes=[b_msq])
            S.op("dve", I("scalar_tensor_tensor", out=var[:, :W], in0=p2[:, :W], scalar=1.0 / CW, in1=msq[:, :W],
                          op0=ALU.mult, op1=ALU.subtract), reads=[b_p2, b_msq], writes=[b_var])
            S.op("act", I("activation", out=var[:, :W], in_=var[:, :W], func=AF.Sqrt, bias=EPS, scale=1.0),
                 reads=[b_var], writes=[b_var])
            S.op("dve", I("reciprocal", out=var[:, :W], in_=var[:, :W]), reads=[b_var], writes=[b_var])
            for c in range(4):
                y, b_y = f32t.next()
                ob, b_ob = bft.next()
                eng = "dve" if c % 2 == 0 else "pool"
                S.op(eng, I("tensor_tensor", out=y[:, :W], in0=cv[:, c, ts], in1=mean[:, :W], op=ALU.subtract),
                     reads=[b_cvc[c], b_mean], writes=[b_y])
                S.op(eng, I("tensor_tensor", out=y[:, :W], in0=y[:, :W], in1=var[:, :W], op=ALU.mult),
                     reads=[b_y, b_var], writes=[b_y])
                S.op("act", I("activation", out=ob[:, :W], in_=y[:, :W], func=AF.Silu, scale=pp[:, 1, c:c + 1],
                              bias=pp[:, 2, c:c + 1]), reads=[b_y, b_pp], writes=[b_ob])
                S.dma("sp", P.SBT[s][c * 128:(c + 1) * 128, ts], ob[:, :W], reads=[b_ob], writes=[P.b("SBT%d" % s)])
        S.barrier()
        for c in range(4):
            u, b_u = ups[c % 2]
            eng = "dve"
            S.dma("sp", u[:, 15:15 + n], P.CX[s][c * 128:(c + 1) * 128, :], reads=[P.b("CX%d" % s)], writes=[b_u])
            S.dma("sp", cv[:, 3 - c, :], P.SCB[s][c * 128:(c + 1) * 128, :], reads=[P.b("SCB%d" % s)],
                  writes=[b_cvc[3 - c]])
            S.op(eng, I("tensor_scalar", out=cv[:, c, :], in0=u[:, 14:14 + n], scalar1=scw[:, c, 0:1], scalar2=None,
                        op0=ALU.mult), reads=[b_u, b_scw], writes=[b_cvc[c]])
            for k in (1, 2):
                S.op(eng, I("scalar_tensor_tensor", out=cv[:, c, :], in0=u[:, 14 + k:14 + k + n],
                            scalar=scw[:, c, k:k + 1], in1=cv[:, c, :], op0=ALU.mult, op1=ALU.add),
                     reads=[b_u, b_cvc[c]], writes=[b_cvc[c]])
            for tt in range(n // W):
                ts = slice(tt * W, (tt + 1) * W)
                ob, b_ob = bft.next()
                S.op(eng, I("tensor_tensor", out=ob[:, :W], in0=cv[:, c, ts], in1=cv[:, 3 - c, ts], op=ALU.mult),
                     reads=[b_cvc[c], b_cvc[3 - c]], writes=[b_ob])
                S.dma("sp", P.SCT[s][c * 128:(c + 1) * 128, ts], ob[:, :W], reads=[b_ob], writes=[P.b("SCT%d" % s)])


def stage_merge(P, l, s, xsrc):
    S = P.S
    n = P.n[s]
    W = min(256, n)
    b_X = P.b_X[s]
    with Stage(S, "mg") as st:
        wao, b_wao = st.sb([128, 8, D], BF16)
        wco, b_wco = st.sb([128, 4, D], BF16)
        wso, b_wso = st.sb([128, 4, D], BF16)
        wo, b_wo = st.sb([128, 16, D], BF16)
        S.dma("pool", wao[:], P.w_attn_o[l].rearrange("(kc p) f -> p kc f", p=128), writes=[b_wao])
        S.dma("pool", wco[:], P.w_conv_o[l].rearrange("(kc p) f -> p kc f", p=128), writes=[b_wco])
        S.dma("pool", wso[:], P.w_sc_o[l].rearrange("(kc p) f -> p kc f", p=128), writes=[b_wso])
        for hf in range(2):
            S.dma("pool", wo[:, hf * 8:(hf + 1) * 8, :],
                  P.w_o[l, hf * 1024:(hf + 1) * 1024, :].rearrange("(kc p) f -> p kc f", p=128), writes=[b_wo])
        gbc, b_gbc = st.sb([128, D], F32)
        S.dma("sp", gbc[:], P.MOD[l, s, 2 * D:3 * D].partition_broadcast(128), reads=[P.b("MOD")], writes=[b_gbc])
        idf, b_idf = st.sb([128, 128], F32)
        idb, b_idb = st.sb([128, 128], BF16)
        S.dma("sp", idf[:], P.c_ident, writes=[b_idf])
        S.op("dve", I("tensor_copy", out=idb[:], in_=idf[:]), reads=[b_idf], writes=[b_idb])
        att, b_att = st.sb([128, AW], BF16)
        attT, b_attT = st.sb([128, 8, W], BF16)
        sbT, b_sbT = st.sb([128, 4, W], BF16)
        scT, b_scT = st.sb([128, 4, W], BF16)
        gT, b_gT = st.sb([128, 48, W], BF16)
        zT, b_zT = st.sb([128, 16, W], BF16)
        xts = Rot([st.sb([128, D], F32) for _ in range(2)])
        f32t = Rot([st.sb([128, 512], F32) for _ in range(6)])
        pT, b_pT = st.ps([128, 8, 128], BF16)
        pst = Rot([st.ps([128, 512], F32) for _ in range(6)])
        for tt in range(n // W):
            t0 = tt * W
            for ti in range(W // 128):
                S.dma("sp", att[:], P.ATT[s][t0 + ti * 128:t0 + (ti + 1) * 128, :], reads=[P.b("ATT%d" % s)],
                      writes=[b_att])
                S.op("pe", [I("transpose", out=pT[:, kc, :], in_=att[:, kc * 128:(kc + 1) * 128], identity=idb[:])
                            for kc in range(8)], reads=[b_att, b_idb], writes=[b_pT])
                S.op("act", I("copy", out=attT[:, :, ti * 128:(ti + 1) * 128], in_=pT[:]), reads=[b_pT],
                     writes=[b_attT])
            S.dma("sp", sbT[:], P.SBT[s][:, t0:t0 + W].rearrange("(c p) t -> p c t", p=128), reads=[P.b("SBT%d" % s)],
                  writes=[b_sbT])
            S.dma("sp", scT[:], P.SCT[s][:, t0:t0 + W].rearrange("(c p) t -> p c t", p=128), reads=[P.b("SCT%d" % s)],
                  writes=[b_scT])
            S.dma("sp", gT[:], P.GT[s][:, t0:t0 + W].rearrange("(c p) t -> p c t", p=128), reads=[P.b("GT%d" % s)],
                  writes=[b_gT])
            for fo in range(16):
                fs = slice(fo * 128, (fo + 1) * 128)
                pA, b_pA = pst.next()
                pB, b_pB = pst.next()
                pC, b_pC = pst.next()
                S.op("pe", [I("matmul", out=pA[:, :W], lhsT=wao[:, kc, fs], rhs=attT[:, kc, :], start=(kc == 0),
                               stop=(kc == 7)) for kc in range(8)], reads=[b_wao, b_attT], writes=[b_pA])
                S.op("pe", [I("matmul", out=pB[:, :W], lhsT=wco[:, kc, fs], rhs=sbT[:, kc, :], start=(kc == 0),
                               stop=(kc == 3)) for kc in range(4)], reads=[b_wco, b_sbT], writes=[b_pB])
                S.op("pe", [I("matmul", out=pC[:, :W], lhsT=wso[:, kc, fs], rhs=scT[:, kc, :], start=(kc == 0),
                               stop=(kc == 3)) for kc in range(4)], reads=[b_wso, b_scT], writes=[b_pC])
                t1, b_t1 = f32t.next()
                t2, b_t2 = f32t.next()
                t3, b_t3 = f32t.next()
                S.op("dve", I("tensor_tensor", out=t1[:, :W], in0=pA[:, :W], in1=gT[:, fo, :], op=ALU.mult),
                     reads=[b_pA, b_gT], writes=[b_t1])
                S.op("dve", I("tensor_tensor", out=t2[:, :W], in0=pB[:, :W], in1=gT[:, 16 + fo, :], op=ALU.mult),
                     reads=[b_pB, b_gT], writes=[b_t2])
                S.op("dve", I("tensor_tensor", out=t3[:, :W], in0=pC[:, :W], in1=gT[:, 32 + fo, :], op=ALU.mult),
                     reads=[b_pC, b_gT], writes=[b_t3])
                S.op("pool", I("tensor_tensor", out=t1[:, :W], in0=t1[:, :W], in1=t2[:, :W], op=ALU.add),
                     reads=[b_t1, b_t2], writes=[b_t1])
                S.op("pool", I("tensor_tensor", out=zT[:, fo, :], in0=t1[:, :W], in1=t3[:, :W], op=ALU.add),
                     reads=[b_t1, b_t3], writes=[b_zT])
            for ti in range(W // 128):
                r0 = t0 + ti * 128
                xt, b_xt = xts.next()
                S.dma("sp", xt[:], xsrc[r0:r0 + 128, :], reads=[b_X], writes=[b_xt])
                for cg in range(4):
                    cs = slice(cg * 512, (cg + 1) * 512)
                    pm, b_pm = pst.next()
                    S.op("pe", [I("matmul", out=pm[:], lhsT=zT[:, kc, ti * 128:(ti + 1) * 128], rhs=wo[:, kc, cs],
                                   start=(kc == 0), stop=(kc == 15)) for kc in range(16)],
                         reads=[b_zT, b_wo], writes=[b_pm])
                    t1, b_t1 = f32t.next()
                    S.op("dve", I("tensor_tensor", out=t1[:], in0=pm[:], in1=gbc[:, cs], op=ALU.mult),
                         reads=[b_pm, b_gbc], writes=[b_t1])
                    S.op("pool", I("tensor_tensor", out=xt[:, cs], in0=xt[:, cs], in1=t1[:], op=ALU.add),
                         reads=[b_t1, b_xt], writes=[b_xt])
                S.dma("sp", P.X[s][r0:r0 + 128, :], xt[:], reads=[b_xt], writes=[b_X])


RW = 2068
CAPS = [512, 32]
SLOT0 = [0, 512]


def stage_moe_prep(P, l, s):
    S = P.S
    n = P.n[s]
    cap = CAPS[s]
    nt = n // 128
    b_X = P.b_X[s]
    X = P.X[s]
    with Stage(S, "mp") as st:
        A2, b_A2 = st.sb([128, D], F32)
        B2, b_B2 = st.sb([128, D], F32)
        g2, b_g2 = st.sb([128, D], F32)
        S.dma("sp", A2[:], P.MOD[l, s, 4 * D:5 * D].partition_broadcast(128), reads=[P.b("MOD")], writes=[b_A2])
        S.dma("sp", B2[:], P.MOD[l, s, 3 * D:4 * D].partition_broadcast(128), reads=[P.b("MOD")], writes=[b_B2])
        S.dma("sp", g2[:], P.g_norm2[l, :].partition_broadcast(128), writes=[b_g2])
        S.op("dve", I("scalar_tensor_tensor", out=A2[:], in0=A2[:], scalar=1.0, in1=g2[:], op0=ALU.add, op1=ALU.mult),
             reads=[b_A2, b_g2], writes=[b_A2])
        wr, b_wr = st.sb([128, 16, NE], BF16)
        S.dma("pool", wr[:], P.w_router[l].rearrange("(kc p) e -> p kc e", p=128), writes=[b_wr])
        idf, b_idf = st.sb([128, 128], F32)
        idb, b_idb = st.sb([128, 128], BF16)
        trib, b_trib = st.sb([128, 128], BF16)
        oneb, b_oneb = st.sb([128, 128], BF16)
        tokid, b_tokid = st.sb([128, 32], F32)
        S.dma("sp", idf[:], P.c_ident, writes=[b_idf])
        S.op("dve", I("tensor_copy", out=idb[:], in_=idf[:]), reads=[b_idf], writes=[b_idb])
        S.dma("pool", trib[:], P.c_tri, writes=[b_trib])
        S.dma("pool", oneb[:], P.c_ones, writes=[b_oneb])
        S.dma("sp", tokid[:], P.c_tokid, writes=[b_tokid])
        ebase, b_ebase = st.sb([128, NE], F32)
        S.dma("sp", ebase[:], P.c_ebase, writes=[b_ebase])
        dumpi, b_dumpi = st.sb([128, 1], F32)
        S.dma("sp", dumpi[:], P.c_dumpidx, writes=[b_dumpi])
        affT, b_affT = st.sb([NE, n], F32)
        maskT, b_maskT = st.sb([NE, n], BF16)
        junkT, b_junkT = st.sb([NE, n], BF16)
        affs, b_affs = st.sb([128, nt, NE], F32)
        xts = Rot([st.sb([128, D], F32) for _ in range(2)])
        h2xs = Rot([st.sb([128, RW], F32) for _ in range(2)])
        h2bs = Rot([st.sb([128, D], BF16) for _ in range(2)])
        h2Ts = Rot([st.sb([128, 16, 128], BF16) for _ in range(2)])
        junk, b_junk = st.sb([128, D], BF16)
        sm = Rot([st.sb([128, NE], F32) for _ in range(8)])
        smb = Rot([st.sb([128, NE], BF16) for _ in range(2)])
        smi = Rot([st.sb([128, NE], I32) for _ in range(2)])
        c1 = Rot([st.sb([128, 1], F32) for _ in range(6)])
        pT, b_pT = st.ps([128, 16, 128], BF16)
        pl = Rot([st.ps([128, NE], F32) for _ in range(2)])
        pA = Rot([st.ps([NE, 128], F32) for _ in range(2)])
        pm = Rot([st.ps([128, NE], BF16) for _ in range(1)])
        for ti in range(nt):
            r0 = ti * 128
            xt, b_xt = xts.next()
            h2x, b_h2x = h2xs.next()
            h2b, b_h2b = h2bs.next()
            h2T, b_h2T = h2Ts.next()
            ss, b_ss = c1.next()
            rs, b_rs = c1.next()
            se, b_se = c1.next()
            S.dma("sp", xt[:], X[r0:r0 + 128, :], reads=[b_X], writes=[b_xt])
            S.op("act", I("activation", out=junk[:], in_=xt[:], func=AF.Square, accum_out=ss[:]), reads=[b_xt],
                 writes=[b_junk, b_ss])
            S.op("act", I("activation", out=rs[:], in_=ss[:], func=AF.Sqrt, scale=1.0 / D, bias=EPS), reads=[b_ss],
                 writes=[b_rs])
            S.op("dve", I("reciprocal", out=rs[:], in_=rs[:]), reads=[b_rs], writes=[b_rs])
            S.op("dve", I("scalar_tensor_tensor", out=h2x[:, 0:D], in0=xt[:], scalar=rs[:, 0:1], in1=A2[:],
                          op0=ALU.mult, op1=ALU.mult), reads=[b_xt, b_rs, b_A2], writes=[b_h2x])
            S.op("pool", I("tensor_tensor", out=h2x[:, 0:D], in0=h2x[:, 0:D], in1=B2[:], op=ALU.add),
                 reads=[b_h2x, b_B2], writes=[b_h2x])
            S.op("act", I("copy", out=h2b[:], in_=h2x[:, 0:D]), reads=[b_h2x], writes=[b_h2b])
            S.op("pe", [I("transpose", out=pT[:, kc, :], in_=h2b[:, kc * 128:(kc + 1) * 128], identity=idb[:])
                        for kc in range(16)], reads=[b_h2b, b_idb], writes=[b_pT])
            S.op("dve", I("tensor_copy", out=h2T[:], in_=pT[:]), reads=[b_pT], writes=[b_h2T])
            S.dma("sp", P.H2[s][r0:r0 + 128, :], h2b[:], reads=[b_h2b], writes=[P.b("H2%d" % s)])
            plg, b_plg = pl.next()
            S.op("pe", [I("matmul", out=plg[:], lhsT=h2T[:, kc, :], rhs=wr[:, kc, :], start=(kc == 0), stop=(kc == 15))
                        for kc in range(16)], reads=[b_h2T, b_wr], writes=[b_plg])
            ex, b_ex = sm.next()
            S.op("act", I("activation", out=ex[:], in_=plg[:], func=AF.Exp, accum_out=se[:]), reads=[b_plg],
                 writes=[b_ex, b_se])
            S.op("dve", I("reciprocal", out=se[:], in_=se[:]), reads=[b_se], writes=[b_se])
            S.op("dve", I("tensor_scalar", out=affs[:, ti, :], in0=ex[:], scalar1=se[:, 0:1], scalar2=None,
                          op0=ALU.mult), reads=[b_ex, b_se], writes=[b_affs])
            pa, b_pa = pA.next()
            S.op("pe", I("transpose", out=pa[:], in_=affs[:, ti, :], identity=idf[:]), reads=[b_affs, b_idf],
                 writes=[b_pa])
            S.op("act", I("copy", out=affT[:, r0:r0 + 128], in_=pa[:]), reads=[b_pa], writes=[b_affT])
        lo, b_lo = st.sb([128, 1], F32)
        mid, b_mid = st.sb([128, 1], F32)
        cnt, b_cnt = st.sb([128, 1], F32)
        inc, b_inc = st.sb([128, 1], F32)
        S.op("dve", I("memset", ap=lo[0:NE, :], constant=0.0), writes=[b_lo])
        for it in range(26):
            step = 0.5 ** (it + 1)
            S.op("dve", I("tensor_scalar", out=mid[0:NE, :], in0=lo[0:NE, :], scalar1=step, scalar2=None,
                          op0=ALU.add), reads=[b_lo], writes=[b_mid])
            S.op("dve", I("tensor_scalar", out=junkT[:], in0=affT[:], scalar1=mid[0:NE, 0:1], scalar2=None,
                          op0=ALU.is_ge, op1=ALU.add, accum_out=cnt[0:NE, :]), reads=[b_affT, b_mid],
                 writes=[b_junkT, b_cnt])
            S.op("dve", I("tensor_scalar", out=inc[0:NE, :], in0=cnt[0:NE, :], scalar1=cap - 0.5, scalar2=step,
                          op0=ALU.is_ge, op1=ALU.mult), reads=[b_cnt], writes=[b_inc])
            S.op("dve", I("tensor_tensor", out=lo[0:NE, :], in0=lo[0:NE, :], in1=inc[0:NE, :], op=ALU.add),
                 reads=[b_lo, b_inc], writes=[b_lo])
        S.op("dve", I("tensor_scalar", out=maskT[:], in0=affT[:], scalar1=lo[0:NE, 0:1], scalar2=None, op0=ALU.is_ge),
             reads=[b_affT, b_lo], writes=[b_maskT])
        carry, b_carry = st.sb([128, NE], F32)
        S.op("dve", I("memset", ap=carry[:], constant=float(SLOT0[s])), writes=[b_carry])
        metas = Rot([st.sb([128, MW], F32) for _ in range(2)])
        bMETA = P.b("META")
        for ti in range(nt):
            r0 = ti * 128
            mt, b_mt = metas.next()
            pmk, b_pmk = pm.next()
            mk, b_mk = sm.next()
            mkb, b_mkb = smb.next()
            pos, b_pos = sm.next()
            t1, b_t1 = sm.next()
            t2, b_t2 = sm.next()
            idx, b_idx = smi.next()
            pp, b_pp = pl.next()
            pc, b_pc = pl.next()
            S.op("dve", I("memset", ap=mt[:], constant=0.0), writes=[b_mt])
            S.op("act", I("copy", out=mt[:, 0:1], in_=tokid[:, ti:ti + 1]), reads=[b_tokid], writes=[b_mt])
            S.op("act", I("copy", out=mt[:, 1:1 + NE], in_=affs[:, ti, :]), reads=[b_affs], writes=[b_mt])
            S.op("pe", I("transpose", out=pmk[:], in_=maskT[:, r0:r0 + 128], identity=idb[0:NE, 0:NE]),
                 reads=[b_maskT, b_idb], writes=[b_pmk])
            S.op("dve", I("tensor_copy", out=mk[:], in_=pmk[:]), reads=[b_pmk], writes=[b_mk])
            S.op("act", I("copy", out=mkb[:], in_=pmk[:]), reads=[b_pmk], writes=[b_mkb])
            S.op("pe", I("matmul", out=pp[:], lhsT=trib[:], rhs=mkb[:], start=True, stop=True), reads=[b_trib, b_mkb],
                 writes=[b_pp])
            S.op("pe", I("matmul", out=pc[:], lhsT=oneb[:], rhs=mkb[:], start=True, stop=True), reads=[b_oneb, b_mkb],
                 writes=[b_pc])
            S.op("dve", I("tensor_tensor", out=pos[:], in0=pp[:], in1=carry[:], op=ALU.add), reads=[b_pp, b_carry],
                 writes=[b_pos])
            S.op("dve", I("tensor_scalar", out=t1[:], in0=pos[:], scalar1=SLOT0[s] + cap - 0.5, scalar2=None,
                          op0=ALU.is_lt), reads=[b_pos], writes=[b_t1])
            S.op("dve", I("tensor_tensor", out=t1[:], in0=t1[:], in1=mk[:], op=ALU.mult), reads=[b_t1, b_mk],
                 writes=[b_t1])
            S.op("dve", I("tensor_tensor", out=pos[:], in0=pos[:], in1=ebase[:], op=ALU.add), reads=[b_pos, b_ebase],
                 writes=[b_pos])
            S.op("dve", I("tensor_scalar", out=pos[:], in0=pos[:], scalar1=dumpi[:, 0:1], scalar2=None,
                          op0=ALU.subtract), reads=[b_pos, b_dumpi], writes=[b_pos])
            S.op("dve", I("tensor_tensor", out=t2[:], in0=pos[:], in1=t1[:], op=ALU.mult), reads=[b_pos, b_t1],
                 writes=[b_t2])
            S.op("dve", I("tensor_scalar", out=t2[:], in0=t2[:], scalar1=dumpi[:, 0:1], scalar2=None, op0=ALU.add),
                 reads=[b_t2, b_dumpi], writes=[b_t2])
            S.op("dve", I("tensor_copy", out=idx[:], in_=t2[:]), reads=[b_t2], writes=[b_idx])
            S.op("dve", I("tensor_tensor", out=carry[:], in0=carry[:], in1=pc[:], op=ALU.add), reads=[b_carry, b_pc],
                 writes=[b_carry])
            for e in range(NE):
                S.dma_fn("pool", I("indirect_dma_start", out=P.META,
                                   out_offset=bass.IndirectOffsetOnAxis(ap=idx[:, e:e + 1], axis=0),
                                   in_=mt[:, :], in_offset=None, bounds_check=None),
                         reads=[b_mt, b_idx], writes=[bMETA])


def stage_experts_dense(P, l, streams):
    S = P.S
    with Stage(S, "ex") as st:
        w1, b_w1 = st.sb([128, 16, FF], BF16)
        w3, b_w3 = st.sb([128, 16, FF], BF16)
        w2, b_w2 = st.sb([128, 8, D], BF16)
        h2Ts = Rot([st.sb([128, 16, 512], BF16) for _ in range(2)])
        hT, b_hT = st.sb([128, 8, 512], BF16)
        m5 = {}
        gt = {}
        bxt = {}
        for s in streams:
            nt = P.n[s] // 128
            m5[s] = st.sb([128, D], F32)
            S.dma("sp", m5[s][0][:], P.MOD[l, s, 5 * D:6 * D].partition_broadcast(128), reads=[P.b("MOD")],
                  writes=[m5[s][1]])
            gt[s] = st.sb([128, nt, NE], F32)
            S.dma("sp", gt[s][0][:], P.GATE[s].rearrange("(t p) e -> p t e", p=128), reads=[P.b("GATE%d" % s)],
                  writes=[gt[s][1]])
            bxt[s] = [S.buf() for _ in range(nt)]
        yos = Rot([st.sb([128, D], F32) for _ in range(2)])
        xts = Rot([st.sb([128, D], F32) for _ in range(2)])
        f32t = Rot([st.sb([128, 512], F32) for _ in range(3)])
        pst = Rot([st.ps([128, 512], F32) for _ in range(6)])
        for e in range(NE):
            S.dma("pool", w1[:], P.w_e1[l, e].rearrange("(kc p) f -> p kc f", p=128), writes=[b_w1])
            S.dma("pool", w3[:], P.w_e3[l, e].rearrange("(kc p) f -> p kc f", p=128), writes=[b_w3])
            S.dma("pool", w2[:], P.w_e2[l, e].rearrange("(kc p) f -> p kc f", p=128), writes=[b_w2])
            for s in streams:
                n = P.n[s]
                W = min(512, n)
                for tt in range(n // W):
                    t0 = tt * W
                    h2T, b_h2T = h2Ts.next()
                    S.dma("sp", h2T[:, :, 0:W], P.H2T[s][:, t0:t0 + W].rearrange("(kc p) t -> p kc t", p=128),
                          reads=[P.b("H2T%d" % s)], writes=[b_h2T])
                    for ffc in range(8):
                        fs = slice(ffc * 128, (ffc + 1) * 128)
                        pa, b_pa = pst.next()
                        pb, b_pb = pst.next()
                        S.op("pe", [I("matmul", out=pa[:, 0:W], lhsT=w1[:, kc, fs], rhs=h2T[:, kc, 0:W],
                                       start=(kc == 0), stop=(kc == 15)) for kc in range(16)],
                             reads=[b_w1, b_h2T], writes=[b_pa])
                        S.op("pe", [I("matmul", out=pb[:, 0:W], lhsT=w3[:, kc, fs], rhs=h2T[:, kc, 0:W],
                                       start=(kc == 0), stop=(kc == 15)) for kc in range(16)],
                             reads=[b_w3, b_h2T], writes=[b_pb])
                        sa, b_sa = f32t.next()
                        S.op("act", I("activation", out=sa[:, 0:W], in_=pa[:, 0:W], func=AF.Silu), reads=[b_pa],
                             writes=[b_sa])
                        S.op("dve", I("tensor_tensor", out=hT[:, ffc, 0:W], in0=sa[:, 0:W], in1=pb[:, 0:W],
                                      op=ALU.mult), reads=[b_sa, b_pb], writes=[b_hT])
                    for ti in range(W // 128):
                        tix = (t0 // 128) + ti
                        r0 = t0 + ti * 128
                        yo, b_yo = yos.next()
                        xt, b_xt = xts.next()
                        S.dma("sp", xt[:], P.X[s][r0:r0 + 128, :], reads=[bxt[s][tix]], writes=[b_xt])
                        for cg in range(4):
                            cs = slice(cg * 512, (cg + 1) * 512)
                            py, b_py = pst.next()
                            S.op("pe", [I("matmul", out=py[:], lhsT=hT[:, ffc, ti * 128:(ti + 1) * 128],
                                           rhs=w2[:, ffc, cs], start=(ffc == 0), stop=(ffc == 7))
                                        for ffc in range(8)], reads=[b_hT, b_w2], writes=[b_py])
                            S.op("dve", I("scalar_tensor_tensor", out=yo[:, cs], in0=py[:],
                                          scalar=gt[s][0][:, tix, e:e + 1], in1=m5[s][0][:, cs], op0=ALU.mult,
                                          op1=ALU.mult), reads=[b_py, gt[s][1], m5[s][1]], writes=[b_yo])
                            S.op("pool", I("tensor_tensor", out=xt[:, cs], in0=xt[:, cs], in1=yo[:, cs], op=ALU.add),
                                 reads=[b_yo, b_xt], writes=[b_xt])
                        S.dma("sp", P.X[s][r0:r0 + 128, :], xt[:], reads=[b_xt], writes=[bxt[s][tix]])


def stage_experts(P, l, streams):
    S = P.S
    tiles = []
    for s in streams:
        cap = CAPS[s]
        for r in range(0, cap, 128):
            tiles.append((s, SLOT0[s] + r, min(128, cap - r)))
    groups = [(SLOT0[s], CAPS[s]) for s in streams]
    NS = 544
    bMETA = P.b("META")
    with Stage(S, "ex") as st:
        w1, b_w1 = st.sb([128, 16, FF], BF16)
        w3, b_w3 = st.sb([128, 16, FF], BF16)
        w2, b_w2 = st.sb([128, 8, D], BF16)
        xsT, b_xsT = st.sb([128, 16, NS], BF16)
        hT, b_hT = st.sb([128, 8, NS], BF16)
        m5 = {}
        for s in streams:
            m5[s] = st.sb([128, D], F32)
            S.dma("sp", m5[s][0][:], P.MOD[l, s, 5 * D:6 * D].partition_broadcast(128), reads=[P.b("MOD")],
                  writes=[m5[s][1]])
        idf, b_idf = st.sb([128, 128], F32)
        idb, b_idb = st.sb([128, 128], BF16)
        S.dma("sp", idf[:], P.c_ident, writes=[b_idf])
        S.op("dve", I("tensor_copy", out=idb[:], in_=idf[:]), reads=[b_idf], writes=[b_idb])
        mts = Rot([st.sb([128, MW], F32) for _ in range(16)])
        xbs = Rot([st.sb([128, D], BF16) for _ in range(8)])
        yos = Rot([st.sb([128, D], F32) for _ in range(2)])
        tks = Rot([st.sb([128, 1], I32) for _ in range(16)])
        f32t = Rot([st.sb([128, 512], F32) for _ in range(3)])
        pT, b_pT = st.ps([128, 16, 128], BF16)
        pst = Rot([st.ps([128, 512], F32) for _ in range(5)])

        def load13(e):
            S.dma("pool", w1[:], P.w_e1[l, e].rearrange("(kc p) f -> p kc f", p=128), writes=[b_w1])
            S.dma("pool", w3[:], P.w_e3[l, e].rearrange("(kc p) f -> p kc f", p=128), writes=[b_w3])

        def load2(e):
            S.dma("pool", w2[:], P.w_e2[l, e].rearrange("(kc p) f -> p kc f", p=128), writes=[b_w2])

        def gather(e):
            meta = []
            for (s, r0, nr) in tiles:
                mt, b_mt = mts.next()
                xb, b_xb = xbs.next()
                tk, b_tk = tks.next()
                S.dma("sp", mt[0:nr, :], P.META[e * 544 + r0:e * 544 + r0 + nr, :], reads=[bMETA], writes=[b_mt])
                S.op("dve", I("tensor_copy", out=tk[0:nr, :], in_=mt[0:nr, 0:1]), reads=[b_mt], writes=[b_tk])
                S.dma_fn("pool", I("indirect_dma_start", out=xb[0:nr, :], out_offset=None, in_=P.H2[s],
                                   in_offset=bass.IndirectOffsetOnAxis(ap=tk[0:nr, 0:1], axis=0), bounds_check=None),
                         reads=[b_tk, P.b("H2%d" % s)], writes=[b_xb])
                meta.append((tk, b_tk, mt, b_mt, xb, b_xb))
            return meta

        load13(0)
        load2(0)
        meta = gather(0)
        for e in range(NE):
            for ti_, (s, r0, nr) in enumerate(tiles):
                tk, b_tk, mt, b_mt, xb, b_xb = meta[ti_]
                S.op("pe", [I("transpose", out=pT[:, kc, 0:nr], in_=xb[0:nr, kc * 128:(kc + 1) * 128],
                               identity=idb[0:nr, 0:nr]) for kc in range(16)], reads=[b_xb, b_idb], writes=[b_pT])
                S.op("dve", I("tensor_copy", out=xsT[:, :, r0:r0 + nr], in_=pT[:, :, 0:nr]), reads=[b_pT],
                     writes=[b_xsT])
            for ffc in range(8):
                fs = slice(ffc * 128, (ffc + 1) * 128)
                for (c0, cn) in groups:
                    pa, b_pa = pst.next()
                    pb, b_pb = pst.next()
                    S.op("pe", [I("matmul", out=pa[:, 0:cn], lhsT=w1[:, kc, fs], rhs=xsT[:, kc, c0:c0 + cn],
                                   start=(kc == 0), stop=(kc == 15)) for kc in range(16)],
                         reads=[b_w1, b_xsT], writes=[b_pa])
                    S.op("pe", [I("matmul", out=pb[:, 0:cn], lhsT=w3[:, kc, fs], rhs=xsT[:, kc, c0:c0 + cn],
                                   start=(kc == 0), stop=(kc == 15)) for kc in range(16)],
                         reads=[b_w3, b_xsT], writes=[b_pb])
                    sa, b_sa = f32t.next()
                    S.op("act", I("activation", out=sa[:, 0:cn], in_=pa[:, 0:cn], func=AF.Silu), reads=[b_pa],
                         writes=[b_sa])
                    S.op("dve", I("tensor_tensor", out=hT[:, ffc, c0:c0 + cn], in0=sa[:, 0:cn], in1=pb[:, 0:cn],
                                  op=ALU.mult), reads=[b_sa, b_pb], writes=[b_hT])
            if e + 1 < NE:
                load13(e + 1)
                meta_next = gather(e + 1)
            for ti_, (s, r0, nr) in enumerate(tiles):
                tk, b_tk, mt, b_mt, xb, b_xb = meta[ti_]
                yo, b_yo = yos.next()
                for cg in range(4):
                    cs = slice(cg * 512, (cg + 1) * 512)
                    py, b_py = pst.next()
                    S.op("pe", [I("matmul", out=py[0:nr, :], lhsT=hT[:, ffc, r0:r0 + nr], rhs=w2[:, ffc, cs],
                                   start=(ffc == 0), stop=(ffc == 7)) for ffc in range(8)],
                         reads=[b_hT, b_w2], writes=[b_py])
                    S.op("dve", I("scalar_tensor_tensor", out=yo[0:nr, cs], in0=py[0:nr, :],
                                  scalar=mt[0:nr, 1 + e:2 + e], in1=m5[s][0][0:nr, cs], op0=ALU.mult, op1=ALU.mult),
                         reads=[b_py, b_mt, m5[s][1]], writes=[b_yo])
                S.dma_fn("pool", I("indirect_dma_start", out=P.X[s],
                                   out_offset=bass.IndirectOffsetOnAxis(ap=tk[0:nr, 0:1], axis=0),
                                   in_=yo[0:nr, :], in_offset=None, bounds_check=None, compute_op=ALU.add),
                         reads=[b_yo, b_tk, P.b_X[s]], writes=[P.b_X[s]])
            if e + 1 < NE:
                load2(e + 1)
                meta = meta_next


def stage_meta_reset(P):
    S = P.S
    with Stage(S, "xr") as st:
        fill, b_fill = st.sb([128, 68 * MW], F32)
        S.dma("sp", fill[:], P.c_metafill, writes=[b_fill])
        S.dma("sp", P.META[0:NE * 544, :].rearrange("(p a) w -> p (a w)", p=128), fill[:], reads=[b_fill],
              writes=[P.b("META")])


def stage_init(P):
    S = P.S
    with Stage(S, "in") as st:
        z, b_z = st.sb([128, D], F32)
        zb, b_zb = st.sb([128, D], BF16)
        S.op("dve", I("memset", ap=z[:], constant=0.0), writes=[b_z])
        S.op("pool", I("memset", ap=zb[:], constant=0.0), writes=[b_zb])
        for s in range(2):
            n = P.n[s]
            S.dma("sp", P.X[s][n:n + 128, :], z[:], reads=[b_z], writes=[P.b_X[s]])
            S.dma("sp", P.H2[s][n:n + 128, :], zb[:], reads=[b_zb], writes=[P.b("H2%d" % s)])


def stage_final(P):
    S = P.S
    with Stage(S, "fi") as st:
        xts = Rot([st.sb([128, 4, D], F32) for _ in range(3)])
        bo = S.buf()
        for i in range(N // 512):
            xt, b_xt = xts.next()
            S.dma("sp", xt[:], P.X[0][i * 512:(i + 1) * 512, :].rearrange("(a p) f -> p a f", p=128),
                  reads=[P.b_X[0]], writes=[b_xt])
            S.dma("act", P.out[i * 512:(i + 1) * 512, :].rearrange("(a p) f -> p a f", p=128), xt[:], reads=[b_xt],
                  writes=[bo])


def build_program(dbg=()):
    P = Prog(dbg=dbg)
    stage_init(P)
    stage_mod(P)
    for l in range(2):
        last = (l == 1)
        xl = P.x if l == 0 else P.X[0]
        xc = P.ctx if l == 0 else P.X[1]
        stage_inproj(P, l, 1, xsrc=xc)
        stage_inproj(P, l, 0, xsrc=xl)
        if not last:
            stage_attn(P, l, 1)
        stage_attn(P, l, 0)
        if not last:
            stage_conv(P, l, 1)
        stage_conv(P, l, 0)
        if not last:
            stage_merge(P, l, 1, xc)
        stage_merge(P, l, 0, xl)
        stage_meta_reset(P)
        streams = [0] if last else [0, 1]
        for s in streams:
            stage_moe_prep(P, l, s)
        stage_experts(P, l, streams)
    stage_final(P)
    P.S.finish()
    return P


_PROG = {}


def kernel(**inputs):
    if "p" not in _PROG:
        _PROG["p"] = build_program()
    P = _PROG["p"]
    sh = prep_shared(inputs)
    in_maps = [prep_core(inputs, b % 4, sh) for b in range(8)]
    res = run_bass_kernel_spmd(P.nc, in_maps, core_ids=list(range(8)))
    out = np.stack([np.asarray(res.results[b]["out"], dtype=np.float32) for b in range(4)], axis=0)
    return out
```

```python
import numpy as np
import concourse.bass as bass
import concourse.mybir as mybir
from concourse.bass_utils import run_bass_kernel_spmd
from contextlib import ExitStack

F32 = mybir.dt.float32
BF16 = mybir.dt.bfloat16
I32 = mybir.dt.int32
ALU = mybir.AluOpType
AF = mybir.ActivationFunctionType

N = 4096
NCX = 256
D = 2048
NH = 16
DH = 64
AW = 1024
CW = 512
PW = 11776
NE = 16
FF = 1024
GW = 64
EPS = 1e-6
NEG = -30000.0
MW = 20


def I(name, **kw):
    return (name, kw)


class Buf:
    __slots__ = ("name", "w", "r")

    def __init__(self, name):
        self.name = name
        self.w = None
        self.r = {}


class Sched:
    ENG = ("pe", "act", "dve", "pool", "sp")
    DQ = ("sp", "pool", "act")

    def __init__(self, nc, es, ndma=8):
        self.nc = nc
        self.es = es
        self.prog = {e: [] for e in self.ENG}
        self.sems = {}
        self.cnt = {}
        for e in self.ENG:
            self.sems[e] = es.enter_context(nc.semaphore("s_" + e))
            self.cnt[e] = 0
        self.ndma = ndma
        self.dcnt = {}
        for q in self.DQ:
            self.dcnt[q] = 0
            for i in range(ndma):
                self.sems[("d", q, i)] = es.enter_context(nc.semaphore("d_%s_%d" % (q, i)))
        self.seen = {e: {} for e in self.ENG}
        self.nbuf = 0
        self.ninst = 0

    def buf(self, name=None):
        self.nbuf += 1
        return Buf(name or "b%d" % self.nbuf)

    def _wait(self, e, ev):
        key, val = ev
        if self.seen[e].get(key, 0) >= val:
            return
        self.seen[e][key] = val
        sem = self.sems[key]
        self.prog[e].append(lambda eng, sem=sem, val=val: eng.wait_ge(sem, val))

    def _deps(self, e, reads, writes):
        for b in reads:
            if b.w is not None:
                if not (b.w[0] == e and e == "pe"):
                    self._wait(e, b.w)
        for b in writes:
            if b.w is not None and b.w[0] != e:
                self._wait(e, b.w)
            for k, v in b.r.items():
                if k != e:
                    self._wait(e, (k, v))

    def _mark(self, ev, reads, writes):
        k, v = ev
        for b in reads:
            if b.r.get(k, 0) < v:
                b.r[k] = v
        for b in writes:
            b.w = ev
            b.r = {}

    def op(self, e, insts, reads=(), writes=()):
        if isinstance(insts, tuple):
            insts = [insts]
        self._deps(e, reads, writes)
        self.cnt[e] += 1
        val = self.cnt[e]
        sem = self.sems[e]
        self.ninst += len(insts)

        def run(eng, insts=insts, sem=sem):
            r = None
            for name, kw in insts:
                r = getattr(eng, name)(**kw)
            r.then_inc(sem, 1)
        self.prog[e].append(run)
        self._mark((e, val), reads, writes)

    def dma(self, q, out, in_, reads=(), writes=(), **kw):
        self.dma_fn(q, I("dma_start", out=out, in_=in_, **kw), reads, writes)

    def dma_fn(self, q, inst, reads=(), writes=()):
        self._deps(q, reads, writes)
        i = self.dcnt[q]
        self.dcnt[q] += 1
        slot = i % self.ndma
        val = 16 * (i // self.ndma + 1)
        key = ("d", q, slot)
        if val > 16:
            self._wait(q, (key, val - 16))
        sem = self.sems[key]
        self.ninst += 1
        self.prog[q].append(lambda eng, inst=inst, sem=sem: getattr(eng, inst[0])(**inst[1]).then_inc(sem, 16))
        self._mark((key, val), reads, writes)

    def all_events(self):
        evs = [(e, self.cnt[e]) for e in self.ENG if self.cnt[e] > 0]
        for q in self.DQ:
            for s in range(self.ndma):
                n = (self.dcnt[q] - 1 - s) // self.ndma + 1 if self.dcnt[q] > s else 0
                if n > 0:
                    evs.append((("d", q, s), 16 * n))
        return evs

    def barrier(self, engines=None):
        evs = self.all_events()
        for e in (engines or self.ENG):
            for ev in evs:
                if ev[0] != e:
                    self._wait(e, ev)

    def flush(self):
        nc = self.nc
        prog = self.prog
        if not any(prog[e] for e in self.ENG):
            return
        with nc.Block() as block:
            @block.tensor
            def _(eng):
                for f in prog["pe"]:
                    f(eng)

            @block.scalar
            def _(eng):
                for f in prog["act"]:
                    f(eng)

            @block.vector
            def _(eng):
                for f in prog["dve"]:
                    f(eng)

            @block.gpsimd
            def _(eng):
                for f in prog["pool"]:
                    f(eng)

            @block.sync
            def _(eng):
                for f in prog["sp"]:
                    f(eng)
        self.prog = {e: [] for e in self.ENG}

    def finish(self):
        for ev in self.all_events():
            if ev[0] != "sp":
                self._wait("sp", ev)
        self.flush()


class Stage:
    _uid = [0]

    def __init__(self, S, name):
        self.S = S
        Stage._uid[0] += 1
        self.name = "%s%d" % (name, Stage._uid[0])
        self.k = 0

    def __enter__(self):
        self.S.barrier()
        self.st = ExitStack()
        self.st.__enter__()
        return self

    def __exit__(self, *a):
        if a[0] is None:
            self.S.barrier()
            self.S.flush()
        return self.st.__exit__(*a)

    def sb(self, shape, dt, name=None):
        self.k += 1
        t = self.st.enter_context(self.S.nc.sbuf_tensor("%s_%s%d" % (self.name, name or "t", self.k), list(shape), dt))
        return t, self.S.buf()

    def ps(self, shape, dt, name=None):
        self.k += 1
        t = self.st.enter_context(self.S.nc.psum_tensor("%s_%s%d" % (self.name, name or "p", self.k), list(shape), dt))
        return t, self.S.buf()


def _rope_tables():
    quarter = 16
    freqs = 1.0 / (10000.0 ** (np.arange(quarter, dtype=np.float32) / quarter))
    t = np.arange(N)
    row, col = t // GW, t % GW
    cos = np.zeros((128, N), np.float32)
    sin = np.zeros((128, N), np.float32)
    for p in range(128):
        d = p % 64
        hh, i = d // 32, d % 32
        pos = (row if hh == 0 else col).astype(np.float32)
        ang = pos * freqs[i % 16]
        cos[p] = np.cos(ang)
        sin[p] = np.sin(ang)
    rperm = np.zeros((128, 128), np.float32)
    for m in range(128):
        i = (m % 64) % 32
        if i < 16:
            rperm[m + 16, m] = -1.0
        else:
            rperm[m - 16, m] = 1.0
    return cos, sin, rperm


ACASE_B = [0, 1, 2, 30, 31]


def _acase(b):
    return 0 if b == 0 else 1 if b == 1 else 3 if b == 30 else 4 if b == 31 else 2


def _attn_geometry():
    ro = np.zeros((5, 128, 5, 128), np.int64)
    co = np.zeros((5, 128, 5, 128), np.int64)
    va = np.zeros((5, 128, 5, 128), bool)
    kcol = np.arange(64)
    qcol = np.arange(64)
    qcs = np.clip(qcol - 8, 0, 48)
    cv = (kcol[:, None] >= qcs[None, :]) & (kcol[:, None] < qcs[None, :] + 16)
    cof = np.clip(kcol[:, None] - qcol[None, :] + 15, 0, 30)
    for ci, b in enumerate(ACASE_B):
        kstart = int(np.clip(2 * b - 4, 0, 54))
        for c in range(5):
            for kk in range(2):
                krow = kstart + 2 * c + kk
                for qr in range(2):
                    r = 2 * b + qr
                    rs = int(np.clip(r - 4, 0, 56))
                    rv = rs <= krow < rs + 8
                    ps = slice(kk * 64, kk * 64 + 64)
                    qs = slice(qr * 64, qr * 64 + 64)
                    ro[ci, ps, c, qs] = int(np.clip(krow - r + 7, 0, 14))
                    co[ci, ps, c, qs] = cof
                    va[ci, ps, c, qs] = cv & rv
    return ro, co, va


def _metafill():
    m = np.zeros((NE * 544, MW), np.float32)
    row = np.arange(NE * 544)
    slot = row % 544
    m[:, 0] = np.where(slot < 512, N, NCX) + (row % 128)
    return m.reshape(128, 68 * MW)


_CONST = {}


def _consts():
    if _CONST:
        return _CONST
    cos, sin, rperm = _rope_tables()
    ro, co, va = _attn_geometry()
    blk = np.zeros((128, 128), np.float32)
    blk[:64, :64] = 1.0 / 64
    blk[64:, 64:] = 1.0 / 64
    tri = np.triu(np.ones((128, 128), np.float32), 1)
    _CONST.update(dict(
        cosT=cos, sinT=sin, rperm=rperm, blk64=blk, ident=np.eye(128, dtype=np.float32),
        ones=np.ones((128, 128), np.float32), tri=tri,
        tokid=(np.arange(32)[None, :] * 128 + np.arange(128)[:, None]).astype(np.float32),
        ebase=np.tile((np.arange(16) * 544).astype(np.float32)[None, :], (128, 1)),
        metafill=_metafill(), dumpidx=(NE * 544 + np.arange(128)).astype(np.float32).reshape(128, 1),
        amask=np.where(va, 0.0, NEG).astype(np.float32).reshape(5, 128, 640),
        _ro=ro, _co=co))
    return _CONST


CONST_SHAPES = dict(cosT=[128, N], sinT=[128, N], rperm=[128, 128], blk64=[128, 128], ident=[128, 128],
                    ones=[128, 128], tri=[128, 128], tokid=[128, 32], amask=[5, 128, 640], ebase=[128, 16],
                    metafill=[128, 68 * MW], dumpidx=[128, 1])

IN_SHAPES = dict(
    x=[N, D], ctx=[NCX, D], cc=[2, D],
    w_ada=[2, D, 6 * D], b_ada=[2, 6 * D], g_norm1=[2, D], g_norm2=[2, D], w_in=[2, D, PW], b_in=[2, PW],
    g_q=[2, DH], g_k=[2, DH], rpbx=[2, NH, 5, 128, 640], w_attn_o=[2, AW, D], conv_dw_w=[2, 31, CW],
    conv_dw_b=[2, CW], conv_ln_g=[2, CW], conv_ln_b=[2, CW], w_conv_o=[2, CW, D], sc_w=[2, 3, CW],
    w_sc_o=[2, CW, D], w_o=[2, D, D], w_router=[2, D, NE], w_e1=[2, NE, D, FF], w_e3=[2, NE, D, FF],
    w_e2=[2, NE, FF, D])


class Rot:
    def __init__(self, items):
        self.items = items
        self.i = 0

    def next(self):
        it = self.items[self.i % len(self.items)]
        self.i += 1
        return it


class Prog:
    def __init__(self, dbg=()):
        self.dbg = set(dbg)
        nc = self.nc = bass.Bass("TRN2", target_bir_lowering=False)
        self.es = ExitStack()
        self.S = Sched(nc, self.es)
        self.n = [N, NCX]
        for k, shp in IN_SHAPES.items():
            setattr(self, k, nc.dram_tensor(k, list(shp), F32, kind="ExternalInput").ap())
        for k, shp in CONST_SHAPES.items():
            setattr(self, "c_" + k, nc.dram_tensor("c_" + k, list(shp), F32, kind="ExternalInput").ap())
        self.out = nc.dram_tensor("out", [N, D], F32, kind="ExternalOutput").ap()
        self.bufs = {}
        S = self.S
        self.MOD = self.scr("MOD", [2, 2, 6 * D], F32)
        self.XC = self.scr("XC", [NCX + 128, D], F32)
        self.XE = self.scr("XE", [N + 128, D], F32)
        self.X = [self.XE, self.XC]
        self.b_X = [S.buf(), S.buf()]
        self.QP = [self.scr("QP%d" % s, [AW, self.n[s]], BF16) for s in range(2)]
        self.QR = self.scr("QR", [AW, N], BF16)
        self.KR = self.scr("KR", [AW, N], BF16)
        self.KC = self.scr("KC", [AW, NCX], BF16)
        self.V = [self.scr("V%d" % s, [self.n[s], AW], BF16) for s in range(2)]
        self.UT = [self.scr("UT%d" % s, [CW, self.n[s]], BF16) for s in range(2)]
        self.SCB = [self.scr("SCB%d" % s, [CW, self.n[s]], F32) for s in range(2)]
        self.CX = [self.scr("CX%d" % s, [CW, self.n[s]], F32) for s in range(2)]
        self.GT = [self.scr("GT%d" % s, [3 * D, self.n[s]], BF16) for s in range(2)]
        self.ATT = [self.scr("ATT%d" % s, [self.n[s], AW], BF16) for s in range(2)]
        self.SBT = [self.scr("SBT%d" % s, [CW, self.n[s]], BF16) for s in range(2)]
        self.SCT = [self.scr("SCT%d" % s, [CW, self.n[s]], BF16) for s in range(2)]
        self.H2 = [self.scr("H2%d" % s, [self.n[s] + 128, D], BF16) for s in range(2)]
        self.META = self.scr("META", [NE * 544 + 128, MW], F32)

    def scr(self, name, shape, dt):
        kind = "ExternalOutput" if name in self.dbg else "Internal"
        t = self.nc.dram_tensor(name, list(shape), dt, kind=kind).ap()
        self.bufs[name] = self.S.buf(name)
        return t

    def b(self, name):
        return self.bufs[name]


def stage_mod(P):
    S = P.S
    with Stage(S, "mod") as st:
        cT32, b_c32 = st.sb([128, 16, 2], F32)
        cT, b_cT = st.sb([128, 16, 2], BF16)
        for j in range(2):
            S.dma("sp", cT32[:, :, j], P.cc[j, :].rearrange("(kc p) -> p kc", p=128), writes=[b_c32],
                  allow_slow_non_contiguous=True)
        S.op("act", I("activation", out=cT[:], in_=cT32[:], func=AF.Silu), reads=[b_c32], writes=[b_cT])
        wbs = Rot([st.sb([128, 16, 512], BF16) for _ in range(2)])
        pss = Rot([st.ps([128, 512], F32) for _ in range(2)])
        bias2, b_bias2 = st.sb([2, 6 * D], F32)
        res, b_res = st.sb([2, 6 * D], F32)
        for l in range(2):
            for j in range(2):
                S.dma("sp", bias2[j:j + 1, :], P.b_ada[l:l + 1, :], writes=[b_bias2])
            for ch in range(24):
                wb, b_wb = wbs.next()
                pm, b_pm = pss.next()
                S.dma("pool", wb[:], P.w_ada[l, :, ch * 512:(ch + 1) * 512].rearrange("(kc p) n -> p kc n", p=128),
                      writes=[b_wb])
                S.op("pe", [I("matmul", out=pm[0:2, :], lhsT=cT[:, kc, :], rhs=wb[:, kc, :], start=(kc == 0),
                               stop=(kc == 15)) for kc in range(16)], reads=[b_cT, b_wb], writes=[b_pm])
                S.op("dve", I("tensor_tensor", out=res[0:2, ch * 512:(ch + 1) * 512], in0=pm[0:2, :],
                              in1=bias2[0:2, ch * 512:(ch + 1) * 512], op=ALU.add),
                     reads=[b_pm, b_bias2], writes=[b_res])
            S.dma("sp", P.MOD[l], res[:], reads=[b_res], writes=[P.b("MOD")])


def load_pp(S, q, tile_ap, b_tile, src_row, ncol, reads=()):
    S.dma(q, tile_ap, src_row.rearrange("(c p) -> p c", p=128), reads=list(reads), writes=[b_tile],
          allow_slow_non_contiguous=True)


def stage_inproj(P, l, s, xsrc=None):
    S = P.S
    n = P.n[s]
    lat = (s == 0)
    X = xsrc if xsrc is not None else P.X[s]
    b_X = P.b_X[s]
    G = min(n, 2048)
    npass = n // G
    W = min(512, G)
    ntt = G // W
    chunks = list(range(23)) if not (s == 1 and l == 1) else [2, 3, 4, 5]
    with Stage(S, "ip") as st:
        hT, b_hT = st.sb([128, 16, G], BF16)
        wbs = [st.sb([128, 16, 512], BF16) for _ in range(3)]
        xts = Rot([st.sb([128, D], F32) for _ in range(2)])
        xss = Rot([st.sb([128, D], BF16) for _ in range(2)])
        junk, b_junk = st.sb([128, D], BF16)
        sss = Rot([st.sb([128, 1], F32) for _ in range(2)])
        rss = Rot([st.sb([128, 1], F32) for _ in range(2)])
        idf, b_idf = st.sb([128, 128], F32)
        idb, b_idb = st.sb([128, 128], BF16)
        blk, b_blk = st.sb([128, 128], BF16)
        rperm, b_rperm = st.sb([128, 128], BF16)
        A1, b_A1 = st.sb([128, 16], F32)
        B1, b_B1 = st.sb([128, 16], F32)
        g1, b_g1 = st.sb([128, 16], F32)
        bP, b_bP = st.sb([128, 92], F32)
        bvbc, b_bvbc = st.sb([128, AW], F32)
        gq, b_gq = st.sb([128, 1], F32)
        gk, b_gk = st.sb([128, 1], F32)
        cstabs = Rot([(st.sb([128, 512], F32), st.sb([128, 512], F32)) for _ in range(2)])
        cs_cur = None
        pend = []

        def flush_pend():
            if len(pend) >= 2:
                pend[-2][1]()
                pend[-1][0]()
                pend[-1][1]()
            elif len(pend) == 1:
                pend[-1][0]()
                pend[-1][1]()
            del pend[:]
        pT, b_pT = st.ps([128, 16, 128], BF16)
        paccs = Rot([st.ps([128, 512], F32) for _ in range(3)])
        pauxs = Rot([st.ps([128, 512], F32) for _ in range(3)])
        f32t = Rot([st.sb([128, 512], F32) for _ in range(18)])
        bft = Rot([st.sb([128, 512], BF16) for _ in range(14)])

        S.dma("sp", idf[:], P.c_ident, writes=[b_idf])
        S.op("dve", I("tensor_copy", out=idb[:], in_=idf[:]), reads=[b_idf], writes=[b_idb])
        S.dma("pool", blk[:], P.c_blk64, writes=[b_blk])
        S.dma("pool", rperm[:], P.c_rperm, writes=[b_rperm])
        load_pp(S, "sp", B1[:], b_B1, P.MOD[l, s, 0:D], 16, reads=[P.b("MOD")])
        load_pp(S, "sp", A1[:], b_A1, P.MOD[l, s, D:2 * D], 16, reads=[P.b("MOD")])
        load_pp(S, "sp", g1[:], b_g1, P.g_norm1[l, :], 16)
        S.op("dve", I("scalar_tensor_tensor", out=A1[:], in0=A1[:], scalar=1.0, in1=g1[:], op0=ALU.add,
                      op1=ALU.mult), reads=[b_A1, b_g1], writes=[b_A1])
        load_pp(S, "sp", bP[:], b_bP, P.b_in[l, :], 92)
        S.dma("sp", bvbc[:], P.b_in[l, 2 * AW:3 * AW].partition_broadcast(128), writes=[b_bvbc])
        for (gt, b_gt, src, sc) in ((gq, b_gq, P.g_q, 0.125), (gk, b_gk, P.g_k, 1.0)):
            for hh in range(2):
                S.dma("sp", gt[hh * 64:(hh + 1) * 64, 0:1], src[l, :].rearrange("(p o) -> p o", o=1),
                      writes=[b_gt], allow_slow_non_contiguous=True)
            S.op("act", I("mul", out=gt[:], in_=gt[:], mul=sc), reads=[b_gt], writes=[b_gt])

        def load_w(c):
            wb, b_wb = wbs[c % 3]
            S.dma("pool", wb[:], P.w_in[l, :, c * 512:(c + 1) * 512].rearrange("(kc p) n -> p kc n", p=128),
                  writes=[b_wb])

        def mm_fm(c, i, tt, pa, b_pa):
            wb, b_wb = wbs[c % 3]
            S.op("pe", [I("matmul", out=pa[:, :W], lhsT=wb[:, kc, i * 128:(i + 1) * 128],
                           rhs=hT[:, kc, tt * W:(tt + 1) * W], start=(kc == 0), stop=(kc == 15))
                        for kc in range(16)], reads=[b_wb, b_hT], writes=[b_pa])

        for ps_ in range(npass):
            for ti in range(G // 128):
                t0 = ps_ * G + ti * 128
                xt, b_xt = xts.next()
                xs, b_xs = xss.next()
                ss, b_ss = sss.next()
                rs, b_rs = rss.next()
                S.dma("sp", xt[:], X[t0:t0 + 128, :], reads=[b_X], writes=[b_xt])
                S.op("act", I("activation", out=junk[:], in_=xt[:], func=AF.Square, accum_out=ss[:]),
                     reads=[b_xt], writes=[b_junk, b_ss])
                S.op("act", I("activation", out=rs[:], in_=ss[:], func=AF.Sqrt, scale=1.0 / D, bias=EPS),
                     reads=[b_ss], writes=[b_rs])
                S.op("dve", I("reciprocal", out=rs[:], in_=rs[:]), reads=[b_rs], writes=[b_rs])
                S.op("dve", I("tensor_scalar", out=xs[:], in0=xt[:], scalar1=rs[:, 0:1], scalar2=None,
                              op0=ALU.mult), reads=[b_xt, b_rs], writes=[b_xs])
                S.op("pe", [I("transpose", out=pT[:, kc, :], in_=xs[:, kc * 128:(kc + 1) * 128], identity=idb[:])
                            for kc in range(16)], reads=[b_xs, b_idb], writes=[b_pT])
                S.op("dve", [I("tensor_scalar", out=hT[:, kc, ti * 128:(ti + 1) * 128], in0=pT[:, kc, :],
                               scalar1=A1[:, kc:kc + 1], scalar2=B1[:, kc:kc + 1], op0=ALU.mult, op1=ALU.add)
                             for kc in range(0, 8)], reads=[b_pT, b_A1, b_B1], writes=[b_hT])
                S.op("act", [I("activation", out=hT[:, kc, ti * 128:(ti + 1) * 128], in_=pT[:, kc, :],
                               func=AF.Identity, scale=A1[:, kc:kc + 1], bias=B1[:, kc:kc + 1])
                             for kc in range(8, 16)], reads=[b_pT, b_A1, b_B1], writes=[b_hT])
            load_w(chunks[0])
            for ci, c in enumerate(chunks):
                if ci + 1 < len(chunks):
                    load_w(chunks[ci + 1])
                wb, b_wb = wbs[c % 3]
                if c >= 4 and pend:
                    flush_pend()
                if c in (4, 5):
                    for ti in range(G // 128):
                        t0 = ps_ * G + ti * 128
                        pa, b_pa = paccs.next()
                        S.op("pe", [I("matmul", out=pa[:], lhsT=hT[:, kc, ti * 128:(ti + 1) * 128], rhs=wb[:, kc, :],
                                       start=(kc == 0), stop=(kc == 15)) for kc in range(16)],
                             reads=[b_wb, b_hT], writes=[b_pa])
                        ob, b_ob = bft.next()
                        S.op("dve", I("tensor_tensor", out=ob[:], in0=pa[:], in1=bvbc[:, (c - 4) * 512:(c - 3) * 512],
                                      op=ALU.add), reads=[b_pa, b_bvbc], writes=[b_ob])
                        S.dma("sp", P.V[s][t0:t0 + 128, (c - 4) * 512:(c - 3) * 512], ob[:], reads=[b_ob],
                              writes=[P.b("V%d" % s)])
                    continue
                if c in (6, 9):
                    continue
                for tt in range(ntt):
                    t0 = ps_ * G + tt * W
                    if c < 4 and lat:
                        cs_cur = cstabs.next()
                        S.dma("sp", cs_cur[0][0][:, :W], P.c_cosT[:, t0:t0 + W], writes=[cs_cur[0][1]])
                        S.dma("sp", cs_cur[1][0][:, :W], P.c_sinT[:, t0:t0 + W], writes=[cs_cur[1][1]])
                    for i in range(4):
                        fc = 4 * c + i
                        pa, b_pa = paccs.next()
                        mm_fm(c, i, tt, pa, b_pa)
                        if c < 4:
                            isq = c < 2
                            gt, b_gt = (gq, b_gq) if isq else (gk, b_gk)
                            raw, b_raw = f32t.next()
                            sq, b_sq = bft.next()
                            rstd, b_rstd = f32t.next()
                            qn, b_qn = f32t.next()
                            qb, b_qb = bft.next()
                            S.op("act", I("activation", out=raw[:, :W], in_=pa[:, :W], func=AF.Identity,
                                          bias=bP[:, fc:fc + 1], scale=1.0), reads=[b_pa, b_bP], writes=[b_raw])
                            S.op("act", I("activation", out=sq[:, :W], in_=pa[:, :W], func=AF.Square,
                                          bias=bP[:, fc:fc + 1], scale=1.0), reads=[b_pa, b_bP], writes=[b_sq])
                            rows = slice((fc % 8) * 128, (fc % 8 + 1) * 128)

                            def step2(isq=isq, gt=gt, b_gt=b_gt, raw=raw, b_raw=b_raw, sq=sq, b_sq=b_sq, rstd=rstd,
                                      b_rstd=b_rstd, qn=qn, b_qn=b_qn, rows=rows, t0=t0, qb=qb, b_qb=b_qb):
                                px, b_px = pauxs.next()
                                S.op("pe", I("matmul", out=px[:, :W], lhsT=blk[:], rhs=sq[:, :W], start=True,
                                             stop=True), reads=[b_sq, b_blk], writes=[b_px])
                                S.op("act", I("activation", out=rstd[:, :W], in_=px[:, :W], func=AF.Sqrt, bias=EPS,
                                              scale=1.0), reads=[b_px], writes=[b_rstd])
                                S.op("dve", I("reciprocal", out=rstd[:, :W], in_=rstd[:, :W]), reads=[b_rstd],
                                     writes=[b_rstd])
                                S.op("dve", I("scalar_tensor_tensor", out=qn[:, :W], in0=raw[:, :W],
                                              scalar=gt[:, 0:1], in1=rstd[:, :W], op0=ALU.mult, op1=ALU.mult),
                                     reads=[b_raw, b_rstd, b_gt], writes=[b_qn])
                                S.op("act", I("copy", out=qb[:, :W], in_=qn[:, :W]), reads=[b_qn], writes=[b_qb])
                                if isq:
                                    S.dma("sp", P.QP[s][rows, t0:t0 + W], qb[:, :W], reads=[b_qb],
                                          writes=[P.b("QP%d" % s)])
                                elif not lat:
                                    S.dma("sp", P.KC[rows, t0:t0 + W], qb[:, :W], reads=[b_qb], writes=[P.b("KC")])

                            def step3(isq=isq, qn=qn, b_qn=b_qn, rows=rows, t0=t0, cs=cs_cur, qb=qb, b_qb=b_qb):
                                if not lat:
                                    return
                                (cos_t, b_cos), (sin_t, b_sin) = cs
                                py, b_py = pauxs.next()
                                t1, b_t1 = f32t.next()
                                t2, b_t2 = f32t.next()
                                ob, b_ob = bft.next()
                                S.op("pe", I("matmul", out=py[:, :W], lhsT=rperm[:], rhs=qb[:, :W], start=True,
                                             stop=True), reads=[b_qb, b_rperm], writes=[b_py])
                                S.op("pool", I("tensor_tensor", out=t1[:, :W], in0=qn[:, :W], in1=cos_t[:, :W],
                                               op=ALU.mult), reads=[b_qn, b_cos], writes=[b_t1])
                                S.op("dve", I("tensor_tensor", out=t2[:, :W], in0=py[:, :W], in1=sin_t[:, :W],
                                              op=ALU.mult), reads=[b_py, b_sin], writes=[b_t2])
                                S.op("dve", I("tensor_tensor", out=ob[:, :W], in0=t1[:, :W], in1=t2[:, :W],
                                              op=ALU.add), reads=[b_t1, b_t2], writes=[b_ob])
                                dst, nm = (P.QR, "QR") if isq else (P.KR, "KR")
                                S.dma("sp", dst[rows, t0:t0 + W], ob[:, :W], reads=[b_ob], writes=[P.b(nm)])

                            pend.append([step2, step3])
                            if len(pend) >= 2:
                                pend[-2][0]()
                            if len(pend) >= 3:
                                pend[-3][1]()
                                pend.pop(0)
                        elif c == 7:
                            pb, b_pb = paccs.next()
                            mm_fm(6, i, tt, pb, b_pb)
                            a, b_a = f32t.next()
                            sg, b_sg = f32t.next()
                            u, b_u = bft.next()
                            S.op("act", I("activation", out=a[:, :W], in_=pb[:, :W], func=AF.Identity,
                                          bias=bP[:, 24 + i:25 + i], scale=1.0), reads=[b_pb, b_bP], writes=[b_a])
                            S.op("act", I("activation", out=sg[:, :W], in_=pa[:, :W], func=AF.Sigmoid,
                                          bias=bP[:, fc:fc + 1], scale=1.0), reads=[b_pa, b_bP], writes=[b_sg])
                            S.op("dve", I("tensor_tensor", out=u[:, :W], in0=a[:, :W], in1=sg[:, :W], op=ALU.mult),
                                 reads=[b_a, b_sg], writes=[b_u])
                            S.dma("sp", P.UT[s][i * 128:(i + 1) * 128, t0:t0 + W], u[:, :W], reads=[b_u],
                                  writes=[P.b("UT%d" % s)])
                        elif c == 8:
                            a, b_a = f32t.next()
                            S.op("act", I("activation", out=a[:, :W], in_=pa[:, :W], func=AF.Identity,
                                          bias=bP[:, fc:fc + 1], scale=1.0), reads=[b_pa, b_bP], writes=[b_a])
                            S.dma("sp", P.SCB[s][i * 128:(i + 1) * 128, t0:t0 + W], a[:, :W], reads=[b_a],
                                  writes=[P.b("SCB%d" % s)])
                        elif c == 10:
                            pb, b_pb = paccs.next()
                            mm_fm(9, i, tt, pb, b_pb)
                            a, b_a = f32t.next()
                            u, b_u = f32t.next()
                            S.op("act", I("activation", out=a[:, :W], in_=pb[:, :W], func=AF.Identity,
                                          bias=bP[:, 36 + i:37 + i], scale=1.0), reads=[b_pb, b_bP], writes=[b_a])
                            S.op("dve", I("scalar_tensor_tensor", out=u[:, :W], in0=pa[:, :W],
                                          scalar=bP[:, fc:fc + 1], in1=a[:, :W], op0=ALU.add, op1=ALU.mult),
                                 reads=[b_pa, b_a, b_bP], writes=[b_u])
                            S.dma("sp", P.CX[s][i * 128:(i + 1) * 128, t0:t0 + W], u[:, :W], reads=[b_u],
                                  writes=[P.b("CX%d" % s)])
                        else:
                            ob, b_ob = bft.next()
                            S.op("act", I("activation", out=ob[:, :W], in_=pa[:, :W], func=AF.Sigmoid,
                                          bias=bP[:, fc:fc + 1], scale=1.0), reads=[b_pa, b_bP], writes=[b_ob])
                            r0 = (fc - 44) * 128
                            S.dma("sp", P.GT[s][r0:r0 + 128, t0:t0 + W], ob[:, :W], reads=[b_ob],
                                  writes=[P.b("GT%d" % s)])


_SHARED = {}


def prep_shared(inputs):
    C = _consts()
    sh = {}
    for k in IN_SHAPES:
        if k in ("x", "ctx", "cc", "rpbx"):
            continue
        sh[k] = np.ascontiguousarray(np.asarray(inputs[k], dtype=np.float32))
    rpb = np.asarray(inputs["rpb"], dtype=np.float32)
    ro = C["_ro"].reshape(5, 128, 640)
    co = C["_co"].reshape(5, 128, 640)
    sh["rpbx"] = np.ascontiguousarray(rpb[:, :, ro, co])
    for k in CONST_SHAPES:
        sh["c_" + k] = np.ascontiguousarray(C[k])
    return sh


def prep_core(inputs, b, sh):
    m = dict(sh)
    m["x"] = np.ascontiguousarray(np.asarray(inputs["x"][b], dtype=np.float32))
    m["ctx"] = np.ascontiguousarray(np.asarray(inputs["ctx"][b], dtype=np.float32))
    m["cc"] = np.ascontiguousarray(np.stack([np.asarray(inputs["c"][b]), np.asarray(inputs["c_ctx"])]).astype(np.float32))
    return m


def stage_attn(P, l, s):
    if s == 1:
        return stage_attn_ctx(P, l)
    S = P.S
    with Stage(S, "at") as st:
        Es = Rot([st.sb([128, 7, 128], BF16) for _ in range(4)])
        sbts = Rot([st.sb([128, 640], F32) for _ in range(4)])
        recs = Rot([st.sb([128, 1], F32) for _ in range(4)])
        pS1s = Rot([st.ps([128, 4, 128], F32) for _ in range(3)])
        pS2s = Rot([st.ps([128, 3, 128], F32) for _ in range(3)])
        pOs = Rot([st.ps([128, 65], F32) for _ in range(2)])
        amask, b_amask = st.sb([128, 5, 640], F32)
        S.dma("sp", amask[:], P.c_amask.rearrange("c p f -> p c f"), writes=[b_amask])
        sets = []
        for i in range(2):
            d = dict(kcT=st.sb([64, NCX], BF16), vca=st.sb([128, 2, 65], BF16), qr=st.sb([64, N], BF16),
                     qp=st.sb([64, N], BF16), kr=st.sb([64, N], BF16), va=st.sb([128, 32, 65], BF16),
                     bias=st.sb([128, 5, 640], F32), osb=st.sb([128, 32, 64], BF16))
            S.op("pool", I("memset", ap=d["vca"][0][:, :, 64:65], constant=1.0), writes=[d["vca"][1]])
            S.op("pool", I("memset", ap=d["va"][0][:, :, 64:65], constant=1.0), writes=[d["va"][1]])
            sets.append(d)

        def load(h):
            d = sets[h % 2]
            hs = slice(h * 64, (h + 1) * 64)
            S.dma("sp", d["kcT"][0][:], P.KC[hs, :], reads=[P.b("KC")], writes=[d["kcT"][1]])
            S.dma("sp", d["vca"][0][:, :, 0:64], P.V[1][:, hs].rearrange("(c p) d -> p c d", p=128),
                  reads=[P.b("V1")], writes=[d["vca"][1]])
            S.dma("sp", d["kr"][0][:], P.KR[hs, :], reads=[P.b("KR")], writes=[d["kr"][1]])
            S.dma("sp", d["qr"][0][:], P.QR[hs, :], reads=[P.b("QR")], writes=[d["qr"][1]])
            S.dma("sp", d["qp"][0][:], P.QP[0][hs, :], reads=[P.b("QP0")], writes=[d["qp"][1]])
            S.dma("sp", d["va"][0][:, :, 0:64], P.V[0][:, hs].rearrange("(t p) d -> p t d", p=128),
                  reads=[P.b("V0")], writes=[d["va"][1]])
            S.dma("sp", d["bias"][0][:], P.rpbx[l, h].rearrange("c p f -> p c f"), writes=[d["bias"][1]])
            S.op("pool", I("tensor_tensor", out=d["bias"][0][:], in0=d["bias"][0][:], in1=amask[:], op=ALU.add),
                 reads=[d["bias"][1], b_amask], writes=[d["bias"][1]])

        load(0)
        for h in range(NH):
            if h + 1 < NH:
                load(h + 1)
            d = sets[h % 2]
            hs = slice(h * 64, (h + 1) * 64)
            kcT, b_kcT = d["kcT"]
            vca, b_vca = d["vca"]
            qr, b_qr = d["qr"]
            qp, b_qp = d["qp"]
            kr, b_kr = d["kr"]
            va, b_va = d["va"]
            bias, b_bias = d["bias"]
            osb, b_osb = d["osb"]
            def qk(b):
                pS1, b_pS1 = pS1s.next()
                pS2, b_pS2 = pS2s.next()
                ks = int(np.clip(2 * b - 4, 0, 54))
                qs = slice(b * 128, (b + 1) * 128)
                S.op("pe", [I("matmul", out=pS1[:, c, :], lhsT=kr[:, (ks + 2 * c) * 64:(ks + 2 * c + 2) * 64],
                               rhs=qr[:, qs], start=True, stop=True) for c in range(4)],
                     reads=[b_kr, b_qr], writes=[b_pS1])
                S.op("pe", [I("matmul", out=pS2[:, 0, :], lhsT=kr[:, (ks + 8) * 64:(ks + 10) * 64], rhs=qr[:, qs],
                               start=True, stop=True)] +
                           [I("matmul", out=pS2[:, 1 + c, :], lhsT=kcT[:, c * 128:(c + 1) * 128], rhs=qp[:, qs],
                              start=True, stop=True) for c in range(2)],
                     reads=[b_kr, b_qr, b_kcT, b_qp], writes=[b_pS2])
                return pS1, b_pS1, pS2, b_pS2

            ahead = [qk(0), qk(1)]
            for b in range(32):
                pS1, b_pS1, pS2, b_pS2 = ahead.pop(0)
                E, b_E = Es.next()
                sbt, b_sbt = sbts.next()
                rec, b_rec = recs.next()
                pO, b_pO = pOs.next()
                case = _acase(b)
                ks = int(np.clip(2 * b - 4, 0, 54))
                S.op("dve", I("tensor_tensor", out=sbt[:, 0:512], in0=pS1[:].rearrange("p c q -> p (c q)"),
                              in1=bias[:, case, 0:512], op=ALU.add), reads=[b_pS1, b_bias], writes=[b_sbt])
                S.op("dve", I("tensor_tensor", out=sbt[:, 512:640], in0=pS2[:, 0, :], in1=bias[:, case, 512:640],
                              op=ALU.add), reads=[b_pS2, b_bias], writes=[b_sbt])
                S.op("act", I("activation", out=E[:, 0:5, :].rearrange("p c q -> p (c q)"), in_=sbt[:], func=AF.Exp),
                     reads=[b_sbt], writes=[b_E])
                S.op("act", I("activation", out=E[:, 5:7, :], in_=pS2[:, 1:3, :], func=AF.Exp), reads=[b_pS2],
                     writes=[b_E])
                if b + 2 < 32:
                    ahead.append(qk(b + 2))
                mm = [I("matmul", out=pO[:], lhsT=E[:, c, :], rhs=va[:, ks // 2 + c, :], start=(c == 0), stop=False)
                      for c in range(5)]
                mm += [I("matmul", out=pO[:], lhsT=E[:, 5 + c, :], rhs=vca[:, c, :], start=False, stop=(c == 1))
                       for c in range(2)]
                S.op("pe", mm, reads=[b_E, b_va, b_vca], writes=[b_pO])
                S.op("dve", I("reciprocal", out=rec[:], in_=pO[:, 64:65]), reads=[b_pO], writes=[b_rec])
                S.op("dve", I("tensor_scalar", out=osb[:, b, :], in0=pO[:, 0:64], scalar1=rec[:, 0:1], scalar2=None,
                              op0=ALU.mult), reads=[b_pO, b_rec], writes=[b_osb])
            S.dma("pool", P.ATT[0][:, hs].rearrange("(b p) f -> p b f", p=128), osb[:], reads=[b_osb],
                  writes=[P.b("ATT0")])


def stage_attn_ctx(P, l):
    S = P.S
    with Stage(S, "ac") as st:
        kcT, b_kcT = st.sb([64, NCX], BF16)
        vca, b_vca = st.sb([128, 2, 65], BF16)
        S.op("pool", I("memset", ap=vca[:, :, 64:65], constant=1.0), writes=[b_vca])
        Es = Rot([st.sb([128, 2, 128], BF16) for _ in range(2)])
        recs = Rot([st.sb([128, 1], F32) for _ in range(2)])
        pCs = Rot([st.ps([128, 2, 128], F32) for _ in range(2)])
        pOs = Rot([st.ps([128, 65], F32) for _ in range(2)])
        qp, b_qp = st.sb([64, NCX], BF16)
        osb, b_osb = st.sb([128, 2, 64], BF16)
        for h in range(NH):
            hs = slice(h * 64, (h + 1) * 64)
            S.dma("sp", kcT[:], P.KC[hs, :], reads=[P.b("KC")], writes=[b_kcT])
            S.dma("sp", vca[:, :, 0:64], P.V[1][:, hs].rearrange("(c p) d -> p c d", p=128), reads=[P.b("V1")],
                  writes=[b_vca])
            S.dma("sp", qp[:], P.QP[1][hs, :], reads=[P.b("QP1")], writes=[b_qp])
            for rb in range(2):
                E, b_E = Es.next()
                pC, b_pC = pCs.next()
                pO, b_pO = pOs.next()
                rec, b_rec = recs.next()
                qpsl = qp[:, rb * 128:(rb + 1) * 128]
                S.op("pe", [I("matmul", out=pC[:, c, :], lhsT=kcT[:, c * 128:(c + 1) * 128], rhs=qpsl, start=True,
                               stop=True) for c in range(2)], reads=[b_kcT, b_qp], writes=[b_pC])
                S.op("act", I("activation", out=E[:], in_=pC[:], func=AF.Exp), reads=[b_pC], writes=[b_E])
                S.op("pe", [I("matmul", out=pO[:], lhsT=E[:, c, :], rhs=vca[:, c, :], start=(c == 0), stop=(c == 1))
                            for c in range(2)], reads=[b_E, b_vca], writes=[b_pO])
                S.op("dve", I("reciprocal", out=rec[:], in_=pO[:, 64:65]), reads=[b_pO], writes=[b_rec])
                S.op("dve", I("tensor_scalar", out=osb[:, rb, :], in0=pO[:, 0:64], scalar1=rec[:, 0:1], scalar2=None,
                              op0=ALU.mult), reads=[b_pO, b_rec], writes=[b_osb])
            S.dma("sp", P.ATT[1][:, hs].rearrange("(b p) f -> p b f", p=128), osb[:], reads=[b_osb],
                  writes=[P.b("ATT1")])


def stage_conv(P, l, s):
    S = P.S
    n = P.n[s]
    W = min(512, n)
    with Stage(S, "cv") as st:
        dw, b_dw = st.sb([128, 4, 31], F32)
        scw, b_scw = st.sb([128, 4, 3], F32)
        pp, b_pp = st.sb([128, 4, 4], F32)
        for k in range(31):
            S.dma("sp", dw[:, :, k], P.conv_dw_w[l, k, :].rearrange("(c p) -> p c", p=128), writes=[b_dw],
                  allow_slow_non_contiguous=True)
        for k in range(3):
            S.dma("sp", scw[:, :, k], P.sc_w[l, k, :].rearrange("(c p) -> p c", p=128), writes=[b_scw],
                  allow_slow_non_contiguous=True)
        for i, src in enumerate((P.conv_dw_b, P.conv_ln_g, P.conv_ln_b)):
            S.dma("sp", pp[:, i, :], src[l, :].rearrange("(c p) -> p c", p=128), writes=[b_pp],
                  allow_slow_non_contiguous=True)
        ones, b_ones = st.sb([128, 128], F32)
        S.dma("sp", ones[:], P.c_ones, writes=[b_ones])
        ups = [st.sb([128, n + 30], F32) for _ in range(2)]
        for (u, b_u) in ups:
            S.op("pool", I("memset", ap=u[:], constant=0.0), writes=[b_u])
        ubs = [st.sb([128, n + 30], BF16) for _ in range(2)]
        for (u, b_u) in ubs:
            S.op("pool", I("memset", ap=u[:], constant=0.0), writes=[b_u])
        idf, b_idf = st.sb([128, 128], F32)
        S.dma("sp", idf[:], P.c_ident, writes=[b_idf])
        dg, b_dg = st.sb([128, 4, 31, 128], BF16)
        S.op("dve", [I("tensor_scalar", out=dg[:, c, k, :], in0=idf[:], scalar1=dw[:, c, k:k + 1], scalar2=None,
                       op0=ALU.mult) for c in range(4) for k in range(31)], reads=[b_idf, b_dw], writes=[b_dg])
        cv, b_cv = st.sb([128, 4, n], F32)
        b_cvc = [S.buf() for _ in range(4)]
        pcs = Rot([st.ps([128, 512], F32) for _ in range(3)])
        for c in range(4):
            ub, b_ub = ubs[c % 2]
            S.dma("sp", ub[:, 15:15 + n], P.UT[s][c * 128:(c + 1) * 128, :], reads=[P.b("UT%d" % s)], writes=[b_ub])
            for tt in range(n // W):
                pc, b_pc = pcs.next()
                S.op("pe", [I("matmul", out=pc[:, :W], lhsT=dg[:, c, k, :], rhs=ub[:, tt * W + k:tt * W + k + W],
                               start=(k == 0), stop=(k == 30)) for k in range(31)], reads=[b_dg, b_ub], writes=[b_pc])
                S.op("act", I("activation", out=cv[:, c, tt * W:(tt + 1) * W], in_=pc[:, :W], func=AF.Identity,
                              bias=pp[:, 0, c:c + 1], scale=1.0), reads=[b_pc, b_pp], writes=[b_cvc[c]])
        f32t = Rot([st.sb([128, 512], F32) for _ in range(10)])
        bft = Rot([st.sb([128, 512], BF16) for _ in range(3)])
        pst = Rot([st.ps([128, 512], F32) for _ in range(4)])
        for tt in range(n // W):
            ts = slice(tt * W, (tt + 1) * W)
            p1, b_p1 = pst.next()
            p2, b_p2 = pst.next()
            S.op("pe", [I("matmul", out=p1[:, :W], lhsT=ones[:], rhs=cv[:, c, ts], start=(c == 0), stop=(c == 3))
                        for c in range(4)], reads=b_cvc + [b_ones], writes=[b_p1])
            sqs = []
            for c in range(4):
                sq, b_sq = f32t.next()
                S.op("act", I("activation", out=sq[:, :W], in_=cv[:, c, ts], func=AF.Square), reads=[b_cvc[c]],
                     writes=[b_sq])
                sqs.append((sq, b_sq))
            S.op("pe", [I("matmul", out=p2[:, :W], lhsT=ones[:], rhs=sqs[c][0][:, :W], start=(c == 0), stop=(c == 3))
                        for c in range(4)], reads=[q[1] for q in sqs] + [b_ones], writes=[b_p2])
            mean, b_mean = f32t.next()
            msq, b_msq = f32t.next()
            var, b_var = f32t.next()
            S.op("act", I("mul", out=mean[:, :W], in_=p1[:, :W], mul=1.0 / CW), reads=[b_p1], writes=[b_mean])
            S.op("dve", I("tensor_tensor", out=msq[:, :W], in0=mean[:, :W], in1=mean[:, :W], op=ALU.mult),
                 reads=[b_mean], writes=[b_msq])
            S.op("dve", I("scalar_tensor_tensor", out=var[:, :W], in0=p2[:, :W], scalar=1.0 / CW, in1=msq[:, :W],
                          op0=ALU.mult, op1=ALU.subtract), reads=[b_p2, b_msq], writes=[b_var])
            S.op("act", I("activation", out=var[:, :W], in_=var[:, :W], func=AF.Sqrt, bias=EPS, scale=1.0),
                 reads=[b_var], writes=[b_var])
            S.op("dve", I("reciprocal", out=var[:, :W], in_=var[:, :W]), reads=[b_var], writes=[b_var])
            for c in range(4):
                y, b_y = f32t.next()
                ob, b_ob = bft.next()
                eng = "dve" if c % 2 == 0 else "pool"
                S.op(eng, I("tensor_tensor", out=y[:, :W], in0=cv[:, c, ts], in1=mean[:, :W], op=ALU.subtract),
                     reads=[b_cvc[c], b_mean], writes=[b_y])
                S.op(eng, I("tensor_tensor", out=y[:, :W], in0=y[:, :W], in1=var[:, :W], op=ALU.mult),
                     reads=[b_y, b_var], writes=[b_y])
                S.op("act", I("activation", out=ob[:, :W], in_=y[:, :W], func=AF.Silu, scale=pp[:, 1, c:c + 1],
                              bias=pp[:, 2, c:c + 1]), reads=[b_y, b_pp], writes=[b_ob])
                S.dma("sp", P.SBT[s][c * 128:(c + 1) * 128, ts], ob[:, :W], reads=[b_ob], writes=[P.b("SBT%d" % s)])
        S.barrier()
        for c in range(4):
            u, b_u = ups[c % 2]
            eng = "dve"
            S.dma("sp", u[:, 15:15 + n], P.CX[s][c * 128:(c + 1) * 128, :], reads=[P.b("CX%d" % s)], writes=[b_u])
            S.dma("sp", cv[:, 3 - c, :], P.SCB[s][c * 128:(c + 1) * 128, :], reads=[P.b("SCB%d" % s)],
                  writes=[b_cvc[3 - c]])
            S.op(eng, I("tensor_scalar", out=cv[:, c, :], in0=u[:, 14:14 + n], scalar1=scw[:, c, 0:1], scalar2=None,
                        op0=ALU.mult), reads=[b_u, b_scw], writes=[b_cvc[c]])
            for k in (1, 2):
                S.op(eng, I("scalar_tensor_tensor", out=cv[:, c, :], in0=u[:, 14 + k:14 + k + n],
                            scalar=scw[:, c, k:k + 1], in1=cv[:, c, :], op0=ALU.mult, op1=ALU.add),
                     reads=[b_u, b_cvc[c]], writes=[b_cvc[c]])
            for tt in range(n // W):
                ts = slice(tt * W, (tt + 1) * W)
                ob, b_ob = bft.next()
                S.op(eng, I("tensor_tensor", out=ob[:, :W], in0=cv[:, c, ts], in1=cv[:, 3 - c, ts], op=ALU.mult),
                     reads=[b_cvc[c], b_cvc[3 - c]], writes=[b_ob])
                S.dma("sp", P.SCT[s][c * 128:(c + 1) * 128, ts], ob[:, :W], reads=[b_ob], writes=[P.b("SCT%d" % s)])


def stage_merge(P, l, s, xsrc):
    S = P.S
    n = P.n[s]
    W = min(256, n)
    b_X = P.b_X[s]
    with Stage(S, "mg") as st:
        wao, b_wao = st.sb([128, 8, D], BF16)
        wco, b_wco = st.sb([128, 4, D], BF16)
        wso, b_wso = st.sb([128, 4, D], BF16)
        wo, b_wo = st.sb([128, 16, D], BF16)
        S.dma("pool", wao[:], P.w_attn_o[l].rearrange("(kc p) f -> p kc f", p=128), writes=[b_wao])
        S.dma("pool", wco[:], P.w_conv_o[l].rearrange("(kc p) f -> p kc f", p=128), writes=[b_wco])
        S.dma("pool", wso[:], P.w_sc_o[l].rearrange("(kc p) f -> p kc f", p=128), writes=[b_wso])
        for hf in range(2):
            S.dma("pool", wo[:, hf * 8:(hf + 1) * 8, :],
                  P.w_o[l, hf * 1024:(hf + 1) * 1024, :].rearrange("(kc p) f -> p kc f", p=128), writes=[b_wo])
        gbc, b_gbc = st.sb([128, D], F32)
        S.dma("sp", gbc[:], P.MOD[l, s, 2 * D:3 * D].partition_broadcast(128), reads=[P.b("MOD")], writes=[b_gbc])
        idf, b_idf = st.sb([128, 128], F32)
        idb, b_idb = st.sb([128, 128], BF16)
        S.dma("sp", idf[:], P.c_ident, writes=[b_idf])
        S.op("dve", I("tensor_copy", out=idb[:], in_=idf[:]), reads=[b_idf], writes=[b_idb])
        att, b_att = st.sb([128, AW], BF16)
        attT, b_attT = st.sb([128, 8, W], BF16)
        sbT, b_sbT = st.sb([128, 4, W], BF16)
        scT, b_scT = st.sb([128, 4, W], BF16)
        gT, b_gT = st.sb([128, 48, W], BF16)
        zT, b_zT = st.sb([128, 16, W], BF16)
        xts = Rot([st.sb([128, D], F32) for _ in range(2)])
        f32t = Rot([st.sb([128, 512], F32) for _ in range(6)])
        pT, b_pT = st.ps([128, 8, 128], BF16)
        pst = Rot([st.ps([128, 512], F32) for _ in range(6)])
        for tt in range(n // W):
            t0 = tt * W
            for ti in range(W // 128):
                S.dma("sp", att[:], P.ATT[s][t0 + ti * 128:t0 + (ti + 1) * 128, :], reads=[P.b("ATT%d" % s)],
                      writes=[b_att])
                S.op("pe", [I("transpose", out=pT[:, kc, :], in_=att[:, kc * 128:(kc + 1) * 128], identity=idb[:])
                            for kc in range(8)], reads=[b_att, b_idb], writes=[b_pT])
                S.op("act", I("copy", out=attT[:, :, ti * 128:(ti + 1) * 128], in_=pT[:]), reads=[b_pT],
                     writes=[b_attT])
            S.dma("sp", sbT[:], P.SBT[s][:, t0:t0 + W].rearrange("(c p) t -> p c t", p=128), reads=[P.b("SBT%d" % s)],
                  writes=[b_sbT])
            S.dma("sp", scT[:], P.SCT[s][:, t0:t0 + W].rearrange("(c p) t -> p c t", p=128), reads=[P.b("SCT%d" % s)],
                  writes=[b_scT])
            S.dma("sp", gT[:], P.GT[s][:, t0:t0 + W].rearrange("(c p) t -> p c t", p=128), reads=[P.b("GT%d" % s)],
                  writes=[b_gT])
            for fo in range(16):
                fs = slice(fo * 128, (fo + 1) * 128)
                pA, b_pA = pst.next()
                pB, b_pB = pst.next()
                pC, b_pC = pst.next()
                S.op("pe", [I("matmul", out=pA[:, :W], lhsT=wao[:, kc, fs], rhs=attT[:, kc, :], start=(kc == 0),
                               stop=(kc == 7)) for kc in range(8)], reads=[b_wao, b_attT], writes=[b_pA])
                S.op("pe", [I("matmul", out=pB[:, :W], lhsT=wco[:, kc, fs], rhs=sbT[:, kc, :], start=(kc == 0),
                               stop=(kc == 3)) for kc in range(4)], reads=[b_wco, b_sbT], writes=[b_pB])
                S.op("pe", [I("matmul", out=pC[:, :W], lhsT=wso[:, kc, fs], rhs=scT[:, kc, :], start=(kc == 0),
                               stop=(kc == 3)) for kc in range(4)], reads=[b_wso, b_scT], writes=[b_pC])
                t1, b_t1 = f32t.next()
                t2, b_t2 = f32t.next()
                t3, b_t3 = f32t.next()
                S.op("dve", I("tensor_tensor", out=t1[:, :W], in0=pA[:, :W], in1=gT[:, fo, :], op=ALU.mult),
                     reads=[b_pA, b_gT], writes=[b_t1])
                S.op("dve", I("tensor_tensor", out=t2[:, :W], in0=pB[:, :W], in1=gT[:, 16 + fo, :], op=ALU.mult),
                     reads=[b_pB, b_gT], writes=[b_t2])
                S.op("dve", I("tensor_tensor", out=t3[:, :W], in0=pC[:, :W], in1=gT[:, 32 + fo, :], op=ALU.mult),
                     reads=[b_pC, b_gT], writes=[b_t3])
                S.op("pool", I("tensor_tensor", out=t1[:, :W], in0=t1[:, :W], in1=t2[:, :W], op=ALU.add),
                     reads=[b_t1, b_t2], writes=[b_t1])
                S.op("pool", I("tensor_tensor", out=zT[:, fo, :], in0=t1[:, :W], in1=t3[:, :W], op=ALU.add),
                     reads=[b_t1, b_t3], writes=[b_zT])
            for ti in range(W // 128):
                r0 = t0 + ti * 128
                xt, b_xt = xts.next()
                S.dma("sp", xt[:], xsrc[r0:r0 + 128, :], reads=[b_X], writes=[b_xt])
                for cg in range(4):
                    cs = slice(cg * 512, (cg + 1) * 512)
                    pm, b_pm = pst.next()
                    S.op("pe", [I("matmul", out=pm[:], lhsT=zT[:, kc, ti * 128:(ti + 1) * 128], rhs=wo[:, kc, cs],
                                   start=(kc == 0), stop=(kc == 15)) for kc in range(16)],
                         reads=[b_zT, b_wo], writes=[b_pm])
                    t1, b_t1 = f32t.next()
                    S.op("dve", I("tensor_tensor", out=t1[:], in0=pm[:], in1=gbc[:, cs], op=ALU.mult),
                         reads=[b_pm, b_gbc], writes=[b_t1])
                    S.op("pool", I("tensor_tensor", out=xt[:, cs], in0=xt[:, cs], in1=t1[:], op=ALU.add),
                         reads=[b_t1, b_xt], writes=[b_xt])
                S.dma("sp", P.X[s][r0:r0 + 128, :], xt[:], reads=[b_xt], writes=[b_X])


RW = 2068
CAPS = [512, 32]
SLOT0 = [0, 512]


def stage_moe_prep(P, l, s):
    S = P.S
    n = P.n[s]
    cap = CAPS[s]
    nt = n // 128
    b_X = P.b_X[s]
    X = P.X[s]
    with Stage(S, "mp") as st:
        A2, b_A2 = st.sb([128, D], F32)
        B2, b_B2 = st.sb([128, D], F32)
        g2, b_g2 = st.sb([128, D], F32)
        S.dma("sp", A2[:], P.MOD[l, s, 4 * D:5 * D].partition_broadcast(128), reads=[P.b("MOD")], writes=[b_A2])
        S.dma("sp", B2[:], P.MOD[l, s, 3 * D:4 * D].partition_broadcast(128), reads=[P.b("MOD")], writes=[b_B2])
        S.dma("sp", g2[:], P.g_norm2[l, :].partition_broadcast(128), writes=[b_g2])
        S.op("dve", I("scalar_tensor_tensor", out=A2[:], in0=A2[:], scalar=1.0, in1=g2[:], op0=ALU.add, op1=ALU.mult),
             reads=[b_A2, b_g2], writes=[b_A2])
        wr, b_wr = st.sb([128, 16, NE], BF16)
        S.dma("pool", wr[:], P.w_router[l].rearrange("(kc p) e -> p kc e", p=128), writes=[b_wr])
        idf, b_idf = st.sb([128, 128], F32)
        idb, b_idb = st.sb([128, 128], BF16)
        trib, b_trib = st.sb([128, 128], BF16)
        oneb, b_oneb = st.sb([128, 128], BF16)
        tokid, b_tokid = st.sb([128, 32], F32)
        S.dma("sp", idf[:], P.c_ident, writes=[b_idf])
        S.op("dve", I("tensor_copy", out=idb[:], in_=idf[:]), reads=[b_idf], writes=[b_idb])
        S.dma("pool", trib[:], P.c_tri, writes=[b_trib])
        S.dma("pool", oneb[:], P.c_ones, writes=[b_oneb])
        S.dma("sp", tokid[:], P.c_tokid, writes=[b_tokid])
        ebase, b_ebase = st.sb([128, NE], F32)
        S.dma("sp", ebase[:], P.c_ebase, writes=[b_ebase])
        dumpi, b_dumpi = st.sb([128, 1], F32)
        S.dma("sp", dumpi[:], P.c_dumpidx, writes=[b_dumpi])
        affT, b_affT = st.sb([NE, n], F32)
        maskT, b_maskT = st.sb([NE, n], BF16)
        junkT, b_junkT = st.sb([NE, n], BF16)
        affs, b_affs = st.sb([128, nt, NE], F32)
        xts = Rot([st.sb([128, D], F32) for _ in range(2)])
        h2xs = Rot([st.sb([128, RW], F32) for _ in range(2)])
        h2bs = Rot([st.sb([128, D], BF16) for _ in range(2)])
        h2Ts = Rot([st.sb([128, 16, 128], BF16) for _ in range(2)])
        junk, b_junk = st.sb([128, D], BF16)
        sm = Rot([st.sb([128, NE], F32) for _ in range(8)])
        smb = Rot([st.sb([128, NE], BF16) for _ in range(2)])
        smi = Rot([st.sb([128, NE], I32) for _ in range(2)])
        c1 = Rot([st.sb([128, 1], F32) for _ in range(6)])
        pT, b_pT = st.ps([128, 16, 128], BF16)
        pl = Rot([st.ps([128, NE], F32) for _ in range(2)])
        pA = Rot([st.ps([NE, 128], F32) for _ in range(2)])
        pm = Rot([st.ps([128, NE], BF16) for _ in range(1)])
        for ti in range(nt):
            r0 = ti * 128
            xt, b_xt = xts.next()
            h2x, b_h2x = h2xs.next()
            h2b, b_h2b = h2bs.next()
            h2T, b_h2T = h2Ts.next()
            ss, b_ss = c1.next()
            rs, b_rs = c1.next()
            se, b_se = c1.next()
            S.dma("sp", xt[:], X[r0:r0 + 128, :], reads=[b_X], writes=[b_xt])
            S.op("act", I("activation", out=junk[:], in_=xt[:], func=AF.Square, accum_out=ss[:]), reads=[b_xt],
                 writes=[b_junk, b_ss])
            S.op("act", I("activation", out=rs[:], in_=ss[:], func=AF.Sqrt, scale=1.0 / D, bias=EPS), reads=[b_ss],
                 writes=[b_rs])
            S.op("dve", I("reciprocal", out=rs[:], in_=rs[:]), reads=[b_rs], writes=[b_rs])
            S.op("dve", I("scalar_tensor_tensor", out=h2x[:, 0:D], in0=xt[:], scalar=rs[:, 0:1], in1=A2[:],
                          op0=ALU.mult, op1=ALU.mult), reads=[b_xt, b_rs, b_A2], writes=[b_h2x])
            S.op("pool", I("tensor_tensor", out=h2x[:, 0:D], in0=h2x[:, 0:D], in1=B2[:], op=ALU.add),
                 reads=[b_h2x, b_B2], writes=[b_h2x])
            S.op("act", I("copy", out=h2b[:], in_=h2x[:, 0:D]), reads=[b_h2x], writes=[b_h2b])
            S.op("pe", [I("transpose", out=pT[:, kc, :], in_=h2b[:, kc * 128:(kc + 1) * 128], identity=idb[:])
                        for kc in range(16)], reads=[b_h2b, b_idb], writes=[b_pT])
            S.op("dve", I("tensor_copy", out=h2T[:], in_=pT[:]), reads=[b_pT], writes=[b_h2T])
            S.dma("sp", P.H2[s][r0:r0 + 128, :], h2b[:], reads=[b_h2b], writes=[P.b("H2%d" % s)])
            plg, b_plg = pl.next()
            S.op("pe", [I("matmul", out=plg[:], lhsT=h2T[:, kc, :], rhs=wr[:, kc, :], start=(kc == 0), stop=(kc == 15))
                        for kc in range(16)], reads=[b_h2T, b_wr], writes=[b_plg])
            ex, b_ex = sm.next()
            S.op("act", I("activation", out=ex[:], in_=plg[:], func=AF.Exp, accum_out=se[:]), reads=[b_plg],
                 writes=[b_ex, b_se])
            S.op("dve", I("reciprocal", out=se[:], in_=se[:]), reads=[b_se], writes=[b_se])
            S.op("dve", I("tensor_scalar", out=affs[:, ti, :], in0=ex[:], scalar1=se[:, 0:1], scalar2=None,
                          op0=ALU.mult), reads=[b_ex, b_se], writes=[b_affs])
            pa, b_pa = pA.next()
            S.op("pe", I("transpose", out=pa[:], in_=affs[:, ti, :], identity=idf[:]), reads=[b_affs, b_idf],
                 writes=[b_pa])
            S.op("act", I("copy", out=affT[:, r0:r0 + 128], in_=pa[:]), reads=[b_pa], writes=[b_affT])
        lo, b_lo = st.sb([128, 1], F32)
        mid, b_mid = st.sb([128, 1], F32)
        cnt, b_cnt = st.sb([128, 1], F32)
        inc, b_inc = st.sb([128, 1], F32)
        S.op("dve", I("memset", ap=lo[0:NE, :], constant=0.0), writes=[b_lo])
        for it in range(26):
            step = 0.5 ** (it + 1)
            S.op("dve", I("tensor_scalar", out=mid[0:NE, :], in0=lo[0:NE, :], scalar1=step, scalar2=None,
                          op0=ALU.add), reads=[b_lo], writes=[b_mid])
            S.op("dve", I("tensor_scalar", out=junkT[:], in0=affT[:], scalar1=mid[0:NE, 0:1], scalar2=None,
                          op0=ALU.is_ge, op1=ALU.add, accum_out=cnt[0:NE, :]), reads=[b_affT, b_mid],
                 writes=[b_junkT, b_cnt])
            S.op("dve", I("tensor_scalar", out=inc[0:NE, :], in0=cnt[0:NE, :], scalar1=cap - 0.5, scalar2=step,
                          op0=ALU.is_ge, op1=ALU.mult), reads=[b_cnt], writes=[b_inc])
            S.op("dve", I("tensor_tensor", out=lo[0:NE, :], in0=lo[0:NE, :], in1=inc[0:NE, :], op=ALU.add),
                 reads=[b_lo, b_inc], writes=[b_lo])
        S.op("dve", I("tensor_scalar", out=maskT[:], in0=affT[:], scalar1=lo[0:NE, 0:1], scalar2=None, op0=ALU.is_ge),
             reads=[b_affT, b_lo], writes=[b_maskT])
        carry, b_carry = st.sb([128, NE], F32)
        S.op("dve", I("memset", ap=carry[:], constant=float(SLOT0[s])), writes=[b_carry])
        metas = Rot([st.sb([128, MW], F32) for _ in range(2)])
        bMETA = P.b("META")
        for ti in range(nt):
            r0 = ti * 128
            mt, b_mt = metas.next()
            pmk, b_pmk = pm.next()
            mk, b_mk = sm.next()
            mkb, b_mkb = smb.next()
            pos, b_pos = sm.next()
            t1, b_t1 = sm.next()
            t2, b_t2 = sm.next()
            idx, b_idx = smi.next()
            pp, b_pp = pl.next()
            pc, b_pc = pl.next()
            S.op("dve", I("memset", ap=mt[:], constant=0.0), writes=[b_mt])
            S.op("act", I("copy", out=mt[:, 0:1], in_=tokid[:, ti:ti + 1]), reads=[b_tokid], writes=[b_mt])
            S.op("act", I("copy", out=mt[:, 1:1 + NE], in_=affs[:, ti, :]), reads=[b_affs], writes=[b_mt])
            S.op("pe", I("transpose", out=pmk[:], in_=maskT[:, r0:r0 + 128], identity=idb[0:NE, 0:NE]),
                 reads=[b_maskT, b_idb], writes=[b_pmk])
            S.op("dve", I("tensor_copy", out=mk[:], in_=pmk[:]), reads=[b_pmk], writes=[b_mk])
            S.op("act", I("copy", out=mkb[:], in_=pmk[:]), reads=[b_pmk], writes=[b_mkb])
            S.op("pe", I("matmul", out=pp[:], lhsT=trib[:], rhs=mkb[:], start=True, stop=True), reads=[b_trib, b_mkb],
                 writes=[b_pp])
            S.op("pe", I("matmul", out=pc[:], lhsT=oneb[:], rhs=mkb[:], start=True, stop=True), reads=[b_oneb, b_mkb],
                 writes=[b_pc])
            S.op("dve", I("tensor_tensor", out=pos[:], in0=pp[:], in1=carry[:], op=ALU.add), reads=[b_pp, b_carry],
                 writes=[b_pos])
            S.op("dve", I("tensor_scalar", out=t1[:], in0=pos[:], scalar1=SLOT0[s] + cap - 0.5, scalar2=None,
                          op0=ALU.is_lt), reads=[b_pos], writes=[b_t1])
            S.op("dve", I("tensor_tensor", out=t1[:], in0=t1[:], in1=mk[:], op=ALU.mult), reads=[b_t1, b_mk],
                 writes=[b_t1])
            S.op("dve", I("tensor_tensor", out=pos[:], in0=pos[:], in1=ebase[:], op=ALU.add), reads=[b_pos, b_ebase],
                 writes=[b_pos])
            S.op("dve", I("tensor_scalar", out=pos[:], in0=pos[:], scalar1=dumpi[:, 0:1], scalar2=None,
                          op0=ALU.subtract), reads=[b_pos, b_dumpi], writes=[b_pos])
            S.op("dve", I("tensor_tensor", out=t2[:], in0=pos[:], in1=t1[:], op=ALU.mult), reads=[b_pos, b_t1],
                 writes=[b_t2])
            S.op("dve", I("tensor_scalar", out=t2[:], in0=t2[:], scalar1=dumpi[:, 0:1], scalar2=None, op0=ALU.add),
                 reads=[b_t2, b_dumpi], writes=[b_t2])
            S.op("dve", I("tensor_copy", out=idx[:], in_=t2[:]), reads=[b_t2], writes=[b_idx])
            S.op("dve", I("tensor_tensor", out=carry[:], in0=carry[:], in1=pc[:], op=ALU.add), reads=[b_carry, b_pc],
                 writes=[b_carry])
            for e in range(NE):
                S.dma_fn("pool", I("indirect_dma_start", out=P.META,
                                   out_offset=bass.IndirectOffsetOnAxis(ap=idx[:, e:e + 1], axis=0),
                                   in_=mt[:, :], in_offset=None, bounds_check=None),
                         reads=[b_mt, b_idx], writes=[bMETA])


def stage_experts_dense(P, l, streams):
    S = P.S
    with Stage(S, "ex") as st:
        w1, b_w1 = st.sb([128, 16, FF], BF16)
        w3, b_w3 = st.sb([128, 16, FF], BF16)
        w2, b_w2 = st.sb([128, 8, D], BF16)
        h2Ts = Rot([st.sb([128, 16, 512], BF16) for _ in range(2)])
        hT, b_hT = st.sb([128, 8, 512], BF16)
        m5 = {}
        gt = {}
        bxt = {}
        for s in streams:
            nt = P.n[s] // 128
            m5[s] = st.sb([128, D], F32)
            S.dma("sp", m5[s][0][:], P.MOD[l, s, 5 * D:6 * D].partition_broadcast(128), reads=[P.b("MOD")],
                  writes=[m5[s][1]])
            gt[s] = st.sb([128, nt, NE], F32)
            S.dma("sp", gt[s][0][:], P.GATE[s].rearrange("(t p) e -> p t e", p=128), reads=[P.b("GATE%d" % s)],
                  writes=[gt[s][1]])
            bxt[s] = [S.buf() for _ in range(nt)]
        yos = Rot([st.sb([128, D], F32) for _ in range(2)])
        xts = Rot([st.sb([128, D], F32) for _ in range(2)])
        f32t = Rot([st.sb([128, 512], F32) for _ in range(3)])
        pst = Rot([st.ps([128, 512], F32) for _ in range(6)])
        for e in range(NE):
            S.dma("pool", w1[:], P.w_e1[l, e].rearrange("(kc p) f -> p kc f", p=128), writes=[b_w1])
            S.dma("pool", w3[:], P.w_e3[l, e].rearrange("(kc p) f -> p kc f", p=128), writes=[b_w3])
            S.dma("pool", w2[:], P.w_e2[l, e].rearrange("(kc p) f -> p kc f", p=128), writes=[b_w2])
            for s in streams:
                n = P.n[s]
                W = min(512, n)
                for tt in range(n // W):
                    t0 = tt * W
                    h2T, b_h2T = h2Ts.next()
                    S.dma("sp", h2T[:, :, 0:W], P.H2T[s][:, t0:t0 + W].rearrange("(kc p) t -> p kc t", p=128),
                          reads=[P.b("H2T%d" % s)], writes=[b_h2T])
                    for ffc in range(8):
                        fs = slice(ffc * 128, (ffc + 1) * 128)
                        pa, b_pa = pst.next()
                        pb, b_pb = pst.next()
                        S.op("pe", [I("matmul", out=pa[:, 0:W], lhsT=w1[:, kc, fs], rhs=h2T[:, kc, 0:W],
                                       start=(kc == 0), stop=(kc == 15)) for kc in range(16)],
                             reads=[b_w1, b_h2T], writes=[b_pa])
                        S.op("pe", [I("matmul", out=pb[:, 0:W], lhsT=w3[:, kc, fs], rhs=h2T[:, kc, 0:W],
                                       start=(kc == 0), stop=(kc == 15)) for kc in range(16)],
                             reads=[b_w3, b_h2T], writes=[b_pb])
                        sa, b_sa = f32t.next()
                        S.op("act", I("activation", out=sa[:, 0:W], in_=pa[:, 0:W], func=AF.Silu), reads=[b_pa],
                             writes=[b_sa])
                        S.op("dve", I("tensor_tensor", out=hT[:, ffc, 0:W], in0=sa[:, 0:W], in1=pb[:, 0:W],
                                      op=ALU.mult), reads=[b_sa, b_pb], writes=[b_hT])
                    for ti in range(W // 128):
                        tix = (t0 // 128) + ti
                        r0 = t0 + ti * 128
                        yo, b_yo = yos.next()
                        xt, b_xt = xts.next()
                        S.dma("sp", xt[:], P.X[s][r0:r0 + 128, :], reads=[bxt[s][tix]], writes=[b_xt])
                        for cg in range(4):
                            cs = slice(cg * 512, (cg + 1) * 512)
                            py, b_py = pst.next()
                            S.op("pe", [I("matmul", out=py[:], lhsT=hT[:, ffc, ti * 128:(ti + 1) * 128],
                                           rhs=w2[:, ffc, cs], start=(ffc == 0), stop=(ffc == 7))
                                        for ffc in range(8)], reads=[b_hT, b_w2], writes=[b_py])
                            S.op("dve", I("scalar_tensor_tensor", out=yo[:, cs], in0=py[:],
                                          scalar=gt[s][0][:, tix, e:e + 1], in1=m5[s][0][:, cs], op0=ALU.mult,
                                          op1=ALU.mult), reads=[b_py, gt[s][1], m5[s][1]], writes=[b_yo])
                            S.op("pool", I("tensor_tensor", out=xt[:, cs], in0=xt[:, cs], in1=yo[:, cs], op=ALU.add),
                                 reads=[b_yo, b_xt], writes=[b_xt])
                        S.dma("sp", P.X[s][r0:r0 + 128, :], xt[:], reads=[b_xt], writes=[bxt[s][tix]])


def stage_experts(P, l, streams):
    S = P.S
    tiles = []
    for s in streams:
        cap = CAPS[s]
        for r in range(0, cap, 128):
            tiles.append((s, SLOT0[s] + r, min(128, cap - r)))
    groups = [(SLOT0[s], CAPS[s]) for s in streams]
    NS = 544
    bMETA = P.b("META")
    with Stage(S, "ex") as st:
        w1, b_w1 = st.sb([128, 16, FF], BF16)
        w3, b_w3 = st.sb([128, 16, FF], BF16)
        w2, b_w2 = st.sb([128, 8, D], BF16)
        xsT, b_xsT = st.sb([128, 16, NS], BF16)
        hT, b_hT = st.sb([128, 8, NS], BF16)
        m5 = {}
        for s in streams:
            m5[s] = st.sb([128, D], F32)
            S.dma("sp", m5[s][0][:], P.MOD[l, s, 5 * D:6 * D].partition_broadcast(128), reads=[P.b("MOD")],
                  writes=[m5[s][1]])
        idf, b_idf = st.sb([128, 128], F32)
        idb, b_idb = st.sb([128, 128], BF16)
        S.dma("sp", idf[:], P.c_ident, writes=[b_idf])
        S.op("dve", I("tensor_copy", out=idb[:], in_=idf[:]), reads=[b_idf], writes=[b_idb])
        mts = Rot([st.sb([128, MW], F32) for _ in range(16)])
        xbs = Rot([st.sb([128, D], BF16) for _ in range(8)])
        yos = Rot([st.sb([128, D], F32) for _ in range(2)])
        tks = Rot([st.sb([128, 1], I32) for _ in range(16)])
        f32t = Rot([st.sb([128, 512], F32) for _ in range(3)])
        pT, b_pT = st.ps([128, 16, 128], BF16)
        pst = Rot([st.ps([128, 512], F32) for _ in range(5)])

        def load13(e):
            S.dma("pool", w1[:], P.w_e1[l, e].rearrange("(kc p) f -> p kc f", p=128), writes=[b_w1])
            S.dma("pool", w3[:], P.w_e3[l, e].rearrange("(kc p) f -> p kc f", p=128), writes=[b_w3])

        def load2(e):
            S.dma("pool", w2[:], P.w_e2[l, e].rearrange("(kc p) f -> p kc f", p=128), writes=[b_w2])

        def gather(e):
            meta = []
            for (s, r0, nr) in tiles:
                mt, b_mt = mts.next()
                xb, b_xb = xbs.next()
                tk, b_tk = tks.next()
                S.dma("sp", mt[0:nr, :], P.META[e * 544 + r0:e * 544 + r0 + nr, :], reads=[bMETA], writes=[b_mt])
                S.op("dve", I("tensor_copy", out=tk[0:nr, :], in_=mt[0:nr, 0:1]), reads=[b_mt], writes=[b_tk])
                S.dma_fn("pool", I("indirect_dma_start", out=xb[0:nr, :], out_offset=None, in_=P.H2[s],
                                   in_offset=bass.IndirectOffsetOnAxis(ap=tk[0:nr, 0:1], axis=0), bounds_check=None),
                         reads=[b_tk, P.b("H2%d" % s)], writes=[b_xb])
                meta.append((tk, b_tk, mt, b_mt, xb, b_xb))
            return meta

        load13(0)
        load2(0)
        meta = gather(0)
        for e in range(NE):
            for ti_, (s, r0, nr) in enumerate(tiles):
                tk, b_tk, mt, b_mt, xb, b_xb = meta[ti_]
                S.op("pe", [I("transpose", out=pT[:, kc, 0:nr], in_=xb[0:nr, kc * 128:(kc + 1) * 128],
                               identity=idb[0:nr, 0:nr]) for kc in range(16)], reads=[b_xb, b_idb], writes=[b_pT])
                S.op("dve", I("tensor_copy", out=xsT[:, :, r0:r0 + nr], in_=pT[:, :, 0:nr]), reads=[b_pT],
                     writes=[b_xsT])
            for ffc in range(8):
                fs = slice(ffc * 128, (ffc + 1) * 128)
                for (c0, cn) in groups:
                    pa, b_pa = pst.next()
                    pb, b_pb = pst.next()
                    S.op("pe", [I("matmul", out=pa[:, 0:cn], lhsT=w1[:, kc, fs], rhs=xsT[:, kc, c0:c0 + cn],
                                   start=(kc == 0), stop=(kc == 15)) for kc in range(16)],
                         reads=[b_w1, b_xsT], writes=[b_pa])
                    S.op("pe", [I("matmul", out=pb[:, 0:cn], lhsT=w3[:, kc, fs], rhs=xsT[:, kc, c0:c0 + cn],
                                   start=(kc == 0), stop=(kc == 15)) for kc in range(16)],
                         reads=[b_w3, b_xsT], writes=[b_pb])
                    sa, b_sa = f32t.next()
                    S.op("act", I("activation", out=sa[:, 0:cn], in_=pa[:, 0:cn], func=AF.Silu), reads=[b_pa],
                         writes=[b_sa])
                    S.op("dve", I("tensor_tensor", out=hT[:, ffc, c0:c0 + cn], in0=sa[:, 0:cn], in1=pb[:, 0:cn],
                                  op=ALU.mult), reads=[b_sa, b_pb], writes=[b_hT])
            if e + 1 < NE:
                load13(e + 1)
                meta_next = gather(e + 1)
            for ti_, (s, r0, nr) in enumerate(tiles):
                tk, b_tk, mt, b_mt, xb, b_xb = meta[ti_]
                yo, b_yo = yos.next()
                for cg in range(4):
                    cs = slice(cg * 512, (cg + 1) * 512)
                    py, b_py = pst.next()
                    S.op("pe", [I("matmul", out=py[0:nr, :], lhsT=hT[:, ffc, r0:r0 + nr], rhs=w2[:, ffc, cs],
                                   start=(ffc == 0), stop=(ffc == 7)) for ffc in range(8)],
                         reads=[b_hT, b_w2], writes=[b_py])
                    S.op("dve", I("scalar_tensor_tensor", out=yo[0:nr, cs], in0=py[0:nr, :],
                                  scalar=mt[0:nr, 1 + e:2 + e], in1=m5[s][0][0:nr, cs], op0=ALU.mult, op1=ALU.mult),
                         reads=[b_py, b_mt, m5[s][1]], writes=[b_yo])
                S.dma_fn("pool", I("indirect_dma_start", out=P.X[s],
                                   out_offset=bass.IndirectOffsetOnAxis(ap=tk[0:nr, 0:1], axis=0),
                                   in_=yo[0:nr, :], in_offset=None, bounds_check=None, compute_op=ALU.add),
                         reads=[b_yo, b_tk, P.b_X[s]], writes=[P.b_X[s]])
            if e + 1 < NE:
                load2(e + 1)
                meta = meta_next


def stage_meta_reset(P):
    S = P.S
    with Stage(S, "xr") as st:
        fill, b_fill = st.sb([128, 68 * MW], F32)
        S.dma("sp", fill[:], P.c_metafill, writes=[b_fill])
        S.dma("sp", P.META[0:NE * 544, :].rearrange("(p a) w -> p (a w)", p=128), fill[:], reads=[b_fill],
              writes=[P.b("META")])


def stage_init(P):
    S = P.S
    with Stage(S, "in") as st:
        z, b_z = st.sb([128, D], F32)
        zb, b_zb = st.sb([128, D], BF16)
        S.op("dve", I("memset", ap=z[:], constant=0.0), writes=[b_z])
        S.op("pool", I("memset", ap=zb[:], constant=0.0), writes=[b_zb])
        for s in range(2):
            n = P.n[s]
            S.dma("sp", P.X[s][n:n + 128, :], z[:], reads=[b_z], writes=[P.b_X[s]])
            S.dma("sp", P.H2[s][n:n + 128, :], zb[:], reads=[b_zb], writes=[P.b("H2%d" % s)])


def stage_final(P):
    S = P.S
    with Stage(S, "fi") as st:
        xts = Rot([st.sb([128, 4, D], F32) for _ in range(3)])
        bo = S.buf()
        for i in range(N // 512):
            xt, b_xt = xts.next()
            S.dma("sp", xt[:], P.X[0][i * 512:(i + 1) * 512, :].rearrange("(a p) f -> p a f", p=128),
                  reads=[P.b_X[0]], writes=[b_xt])
            S.dma("act", P.out[i * 512:(i + 1) * 512, :].rearrange("(a p) f -> p a f", p=128), xt[:], reads=[b_xt],
                  writes=[bo])


def build_program(dbg=()):
    P = Prog(dbg=dbg)
    stage_init(P)
    stage_mod(P)
    for l in range(2):
        last = (l == 1)
        xl = P.x if l == 0 else P.X[0]
        xc = P.ctx if l == 0 else P.X[1]
        stage_inproj(P, l, 1, xsrc=xc)
        stage_inproj(P, l, 0, xsrc=xl)
        if not last:
            stage_attn(P, l, 1)
        stage_attn(P, l, 0)
        if not last:
            stage_conv(P, l, 1)
        stage_conv(P, l, 0)
        if not last:
            stage_merge(P, l, 1, xc)
        stage_merge(P, l, 0, xl)
        stage_meta_reset(P)
        streams = [0] if last else [0, 1]
        for s in streams:
            stage_moe_prep(P, l, s)
        stage_experts(P, l, streams)
    stage_final(P)
    P.S.finish()
    return P


_PROG = {}


def kernel(**inputs):
    if "p" not in _PROG:
        _PROG["p"] = build_program()
    P = _PROG["p"]
    sh = prep_shared(inputs)
    in_maps = [prep_core(inputs, b % 4, sh) for b in range(8)]
    res = run_bass_kernel_spmd(P.nc, in_maps, core_ids=list(range(8)))
    out = np.stack([np.asarray(res.results[b]["out"], dtype=np.float32) for b in range(4)], axis=0)
    return out
```

```python
import numpy as np
import concourse.bass as bass
import concourse.mybir as mybir
from concourse.bass_utils import run_bass_kernel_spmd
from contextlib import ExitStack

F32 = mybir.dt.float32
BF16 = mybir.dt.bfloat16
I32 = mybir.dt.int32
ALU = mybir.AluOpType
AF = mybir.ActivationFunctionType

N = 4096
NCX = 256
D = 2048
NH = 16
DH = 64
AW = 1024
CW = 512
PW = 11776
NE = 16
FF = 1024
GW = 64
EPS = 1e-6
NEG = -30000.0
MW = 20


def I(name, **kw):
    return (name, kw)


class Buf:
    __slots__ = ("name", "w", "r")

    def __init__(self, name):
        self.name = name
        self.w = None
        self.r = {}


class Sched:
    ENG = ("pe", "act", "dve", "pool", "sp")
    DQ = ("sp", "pool", "act")

    def __init__(self, nc, es, ndma=8):
        self.nc = nc
        self.es = es
        self.prog = {e: [] for e in self.ENG}
        self.sems = {}
        self.cnt = {}
        for e in self.ENG:
            self.sems[e] = es.enter_context(nc.semaphore("s_" + e))
            self.cnt[e] = 0
        self.ndma = ndma
        self.dcnt = {}
        for q in self.DQ:
            self.dcnt[q] = 0
            for i in range(ndma):
                self.sems[("d", q, i)] = es.enter_context(nc.semaphore("d_%s_%d" % (q, i)))
        self.seen = {e: {} for e in self.ENG}
        self.nbuf = 0
        self.ninst = 0

    def buf(self, name=None):
        self.nbuf += 1
        return Buf(name or "b%d" % self.nbuf)

    def _wait(self, e, ev):
        key, val = ev
        if self.seen[e].get(key, 0) >= val:
            return
        self.seen[e][key] = val
        sem = self.sems[key]
        self.prog[e].append(lambda eng, sem=sem, val=val: eng.wait_ge(sem, val))

    def _deps(self, e, reads, writes):
        for b in reads:
            if b.w is not None:
                if not (b.w[0] == e and e == "pe"):
                    self._wait(e, b.w)
        for b in writes:
            if b.w is not None and b.w[0] != e:
                self._wait(e, b.w)
            for k, v in b.r.items():
                if k != e:
                    self._wait(e, (k, v))

    def _mark(self, ev, reads, writes):
        k, v = ev
        for b in reads:
            if b.r.get(k, 0) < v:
                b.r[k] = v
        for b in writes:
            b.w = ev
            b.r = {}

    def op(self, e, insts, reads=(), writes=()):
        if isinstance(insts, tuple):
            insts = [insts]
        self._deps(e, reads, writes)
        self.cnt[e] += 1
        val = self.cnt[e]
        sem = self.sems[e]
        self.ninst += len(insts)

        def run(eng, insts=insts, sem=sem):
            r = None
            for name, kw in insts:
                r = getattr(eng, name)(**kw)
            r.then_inc(sem, 1)
        self.prog[e].append(run)
        self._mark((e, val), reads, writes)

    def dma(self, q, out, in_, reads=(), writes=(), **kw):
        self.dma_fn(q, I("dma_start", out=out, in_=in_, **kw), reads, writes)

    def dma_fn(self, q, inst, reads=(), writes=()):
        self._deps(q, reads, writes)
        i = self.dcnt[q]
        self.dcnt[q] += 1
        slot = i % self.ndma
        val = 16 * (i // self.ndma + 1)
        key = ("d", q, slot)
        if val > 16:
            self._wait(q, (key, val - 16))
        sem = self.sems[key]
        self.ninst += 1
        self.prog[q].append(lambda eng, inst=inst, sem=sem: getattr(eng, inst[0])(**inst[1]).then_inc(sem, 16))
        self._mark((key, val), reads, writes)

    def all_events(self):
        evs = [(e, self.cnt[e]) for e in self.ENG if self.cnt[e] > 0]
        for q in self.DQ:
            for s in range(self.ndma):
                n = (self.dcnt[q] - 1 - s) // self.ndma + 1 if self.dcnt[q] > s else 0
                if n > 0:
                    evs.append((("d", q, s), 16 * n))
        return evs

    def barrier(self, engines=None):
        evs = self.all_events()
        for e in (engines or self.ENG):
            for ev in evs:
                if ev[0] != e:
                    self._wait(e, ev)

    def flush(self):
        nc = self.nc
        prog = self.prog
        if not any(prog[e] for e in self.ENG):
            return
        with nc.Block() as block:
            @block.tensor
            def _(eng):
                for f in prog["pe"]:
                    f(eng)

            @block.scalar
            def _(eng):
                for f in prog["act"]:
                    f(eng)

            @block.vector
            def _(eng):
                for f in prog["dve"]:
                    f(eng)

            @block.gpsimd
            def _(eng):
                for f in prog["pool"]:
                    f(eng)

            @block.sync
            def _(eng):
                for f in prog["sp"]:
                    f(eng)
        self.prog = {e: [] for e in self.ENG}

    def finish(self):
        for ev in self.all_events():
            if ev[0] != "sp":
                self._wait("sp", ev)
        self.flush()


class Stage:
    _uid = [0]

    def __init__(self, S, name):
        self.S = S
        Stage._uid[0] += 1
        self.name = "%s%d" % (name, Stage._uid[0])
        self.k = 0

    def __enter__(self):
        self.S.barrier()
        self.st = ExitStack()
        self.st.__enter__()
        return self

    def __exit__(self, *a):
        if a[0] is None:
            self.S.barrier()
            self.S.flush()
        return self.st.__exit__(*a)

    def sb(self, shape, dt, name=None):
        self.k += 1
        t = self.st.enter_context(self.S.nc.sbuf_tensor("%s_%s%d" % (self.name, name or "t", self.k), list(shape), dt))
        return t, self.S.buf()

    def ps(self, shape, dt, name=None):
        self.k += 1
        t = self.st.enter_context(self.S.nc.psum_tensor("%s_%s%d" % (self.name, name or "p", self.k), list(shape), dt))
        return t, self.S.buf()


def _rope_tables():
    quarter = 16
    freqs = 1.0 / (10000.0 ** (np.arange(quarter, dtype=np.float32) / quarter))
    t = np.arange(N)
    row, col = t // GW, t % GW
    cos = np.zeros((128, N), np.float32)
    sin = np.zeros((128, N), np.float32)
    for p in range(128):
        d = p % 64
        hh, i = d // 32, d % 32
        pos = (row if hh == 0 else col).astype(np.float32)
        ang = pos * freqs[i % 16]
        cos[p] = np.cos(ang)
        sin[p] = np.sin(ang)
    rperm = np.zeros((128, 128), np.float32)
    for m in range(128):
        i = (m % 64) % 32
        if i < 16:
            rperm[m + 16, m] = -1.0
        else:
            rperm[m - 16, m] = 1.0
    return cos, sin, rperm


ACASE_B = [0, 1, 2, 30, 31]


def _acase(b):
    return 0 if b == 0 else 1 if b == 1 else 3 if b == 30 else 4 if b == 31 else 2


def _attn_geometry():
    ro = np.zeros((5, 128, 5, 128), np.int64)
    co = np.zeros((5, 128, 5, 128), np.int64)
    va = np.zeros((5, 128, 5, 128), bool)
    kcol = np.arange(64)
    qcol = np.arange(64)
    qcs = np.clip(qcol - 8, 0, 48)
    cv = (kcol[:, None] >= qcs[None, :]) & (kcol[:, None] < qcs[None, :] + 16)
    cof = np.clip(kcol[:, None] - qcol[None, :] + 15, 0, 30)
    for ci, b in enumerate(ACASE_B):
        kstart = int(np.clip(2 * b - 4, 0, 54))
        for c in range(5):
            for kk in range(2):
                krow = kstart + 2 * c + kk
                for qr in range(2):
                    r = 2 * b + qr
                    rs = int(np.clip(r - 4, 0, 56))
                    rv = rs <= krow < rs + 8
                    ps = slice(kk * 64, kk * 64 + 64)
                    qs = slice(qr * 64, qr * 64 + 64)
                    ro[ci, ps, c, qs] = int(np.clip(krow - r + 7, 0, 14))
                    co[ci, ps, c, qs] = cof
                    va[ci, ps, c, qs] = cv & rv
    return ro, co, va


def _metafill():
    m = np.zeros((NE * 544, MW), np.float32)
    row = np.arange(NE * 544)
    slot = row % 544
    m[:, 0] = np.where(slot < 512, N, NCX) + (row % 128)
    return m.reshape(128, 68 * MW)


_CONST = {}


def _consts():
    if _CONST:
        return _CONST
    cos, sin, rperm = _rope_tables()
    ro, co, va = _attn_geometry()
    blk = np.zeros((128, 128), np.float32)
    blk[:64, :64] = 1.0 / 64
    blk[64:, 64:] = 1.0 / 64
    tri = np.triu(np.ones((128, 128), np.float32), 1)
    _CONST.update(dict(
        cosT=cos, sinT=sin, rperm=rperm, blk64=blk, ident=np.eye(128, dtype=np.float32),
        ones=np.ones((128, 128), np.float32), tri=tri,
        tokid=(np.arange(32)[None, :] * 128 + np.arange(128)[:, None]).astype(np.float32),
        ebase=np.tile((np.arange(16) * 544).astype(np.float32)[None, :], (128, 1)),
        metafill=_metafill(), dumpidx=(NE * 544 + np.arange(128)).astype(np.float32).reshape(128, 1),
        amask=np.where(va, 0.0, NEG).astype(np.float32).reshape(5, 128, 640),
        _ro=ro, _co=co))
    return _CONST


CONST_SHAPES = dict(cosT=[128, N], sinT=[128, N], rperm=[128, 128], blk64=[128, 128], ident=[128, 128],
                    ones=[128, 128], tri=[128, 128], tokid=[128, 32], amask=[5, 128, 640], ebase=[128, 16],
                    metafill=[128, 68 * MW], dumpidx=[128, 1])

IN_SHAPES = dict(
    x=[N, D], ctx=[NCX, D], cc=[2, D],
    w_ada=[2, D, 6 * D], b_ada=[2, 6 * D], g_norm1=[2, D], g_norm2=[2, D], w_in=[2, D, PW], b_in=[2, PW],
    g_q=[2, DH], g_k=[2, DH], rpbx=[2, NH, 5, 128, 640], w_attn_o=[2, AW, D], conv_dw_w=[2, 31, CW],
    conv_dw_b=[2, CW], conv_ln_g=[2, CW], conv_ln_b=[2, CW], w_conv_o=[2, CW, D], sc_w=[2, 3, CW],
    w_sc_o=[2, CW, D], w_o=[2, D, D], w_router=[2, D, NE], w_e1=[2, NE, D, FF], w_e3=[2, NE, D, FF],
    w_e2=[2, NE, FF, D])


class Rot:
    def __init__(self, items):
        self.items = items
        self.i = 0

    def next(self):
        it = self.items[self.i % len(self.items)]
        self.i += 1
        return it


class Prog:
    def __init__(self, dbg=()):
        self.dbg = set(dbg)
        nc = self.nc = bass.Bass("TRN2", target_bir_lowering=False)
        self.es = ExitStack()
        self.S = Sched(nc, self.es)
        self.n = [N, NCX]
        for k, shp in IN_SHAPES.items():
            setattr(self, k, nc.dram_tensor(k, list(shp), F32, kind="ExternalInput").ap())
        for k, shp in CONST_SHAPES.items():
            setattr(self, "c_" + k, nc.dram_tensor("c_" + k, list(shp), F32, kind="ExternalInput").ap())
        self.out = nc.dram_tensor("out", [N, D], F32, kind="ExternalOutput").ap()
        self.bufs = {}
        S = self.S
        self.MOD = self.scr("MOD", [2, 2, 6 * D], F32)
        self.XC = self.scr("XC", [NCX + 128, D], F32)
        self.XE = self.scr("XE", [N + 128, D], F32)
        self.X = [self.XE, self.XC]
        self.b_X = [S.buf(), S.buf()]
        self.QP = [self.scr("QP%d" % s, [AW, self.n[s]], BF16) for s in range(2)]
        self.QR = self.scr("QR", [AW, N], BF16)
        self.KR = self.scr("KR", [AW, N], BF16)
        self.KC = self.scr("KC", [AW, NCX], BF16)
        self.V = [self.scr("V%d" % s, [self.n[s], AW], BF16) for s in range(2)]
        self.UT = [self.scr("UT%d" % s, [CW, self.n[s]], BF16) for s in range(2)]
        self.SCB = [self.scr("SCB%d" % s, [CW, self.n[s]], F32) for s in range(2)]
        self.CX = [self.scr("CX%d" % s, [CW, self.n[s]], F32) for s in range(2)]
        self.GT = [self.scr("GT%d" % s, [3 * D, self.n[s]], BF16) for s in range(2)]
        self.ATT = [self.scr("ATT%d" % s, [self.n[s], AW], BF16) for s in range(2)]
        self.SBT = [self.scr("SBT%d" % s, [CW, self.n[s]], BF16) for s in range(2)]
        self.SCT = [self.scr("SCT%d" % s, [CW, self.n[s]], BF16) for s in range(2)]
        self.H2 = [self.scr("H2%d" % s, [self.n[s] + 128, D], BF16) for s in range(2)]
        self.META = self.scr("META", [NE * 544 + 128, MW], F32)

    def scr(self, name, shape, dt):
        kind = "ExternalOutput" if name in self.dbg else "Internal"
        t = self.nc.dram_tensor(name, list(shape), dt, kind=kind).ap()
        self.bufs[name] = self.S.buf(name)
        return t

    def b(self, name):
        return self.bufs[name]


def stage_mod(P):
    S = P.S
    with Stage(S, "mod") as st:
        cT32, b_c32 = st.sb([128, 16, 2], F32)
        cT, b_cT = st.sb([128, 16, 2], BF16)
        for j in range(2):
            S.dma("sp", cT32[:, :, j], P.cc[j, :].rearrange("(kc p) -> p kc", p=128), writes=[b_c32],
                  allow_slow_non_contiguous=True)
        S.op("act", I("activation", out=cT[:], in_=cT32[:], func=AF.Silu), reads=[b_c32], writes=[b_cT])
        wbs = Rot([st.sb([128, 16, 512], BF16) for _ in range(2)])
        pss = Rot([st.ps([128, 512], F32) for _ in range(2)])
        bias2, b_bias2 = st.sb([2, 6 * D], F32)
        res, b_res = st.sb([2, 6 * D], F32)
        for l in range(2):
            for j in range(2):
                S.dma("sp", bias2[j:j + 1, :], P.b_ada[l:l + 1, :], writes=[b_bias2])
            for ch in range(24):
                wb, b_wb = wbs.next()
                pm, b_pm = pss.next()
                S.dma("pool", wb[:], P.w_ada[l, :, ch * 512:(ch + 1) * 512].rearrange("(kc p) n -> p kc n", p=128),
                      writes=[b_wb])
                S.op("pe", [I("matmul", out=pm[0:2, :], lhsT=cT[:, kc, :], rhs=wb[:, kc, :], start=(kc == 0),
                               stop=(kc == 15)) for kc in range(16)], reads=[b_cT, b_wb], writes=[b_pm])
                S.op("dve", I("tensor_tensor", out=res[0:2, ch * 512:(ch + 1) * 512], in0=pm[0:2, :],
                              in1=bias2[0:2, ch * 512:(ch + 1) * 512], op=ALU.add),
                     reads=[b_pm, b_bias2], writes=[b_res])
            S.dma("sp", P.MOD[l], res[:], reads=[b_res], writes=[P.b("MOD")])


def load_pp(S, q, tile_ap, b_tile, src_row, ncol, reads=()):
    S.dma(q, tile_ap, src_row.rearrange("(c p) -> p c", p=128), reads=list(reads), writes=[b_tile],
          allow_slow_non_contiguous=True)


def stage_inproj(P, l, s, xsrc=None):
    S = P.S
    n = P.n[s]
    lat = (s == 0)
    X = xsrc if xsrc is not None else P.X[s]
    b_X = P.b_X[s]
    G = min(n, 2048)
    npass = n // G
    W = min(512, G)
    ntt = G // W
    chunks = list(range(23)) if not (s == 1 and l == 1) else [2, 3, 4, 5]
    with Stage(S, "ip") as st:
        hT, b_hT = st.sb([128, 16, G], BF16)
        wbs = [st.sb([128, 16, 512], BF16) for _ in range(3)]
        xts = Rot([st.sb([128, D], F32) for _ in range(2)])
        xss = Rot([st.sb([128, D], BF16) for _ in range(2)])
        junk, b_junk = st.sb([128, D], BF16)
        sss = Rot([st.sb([128, 1], F32) for _ in range(2)])
        rss = Rot([st.sb([128, 1], F32) for _ in range(2)])
        idf, b_idf = st.sb([128, 128], F32)
        idb, b_idb = st.sb([128, 128], BF16)
        blk, b_blk = st.sb([128, 128], BF16)
        rperm, b_rperm = st.sb([128, 128], BF16)
        A1, b_A1 = st.sb([128, 16], F32)
        B1, b_B1 = st.sb([128, 16], F32)
        g1, b_g1 = st.sb([128, 16], F32)
        bP, b_bP = st.sb([128, 92], F32)
        bvbc, b_bvbc = st.sb([128, AW], F32)
        gq, b_gq = st.sb([128, 1], F32)
        gk, b_gk = st.sb([128, 1], F32)
        cstabs = Rot([(st.sb([128, 512], F32), st.sb([128, 512], F32)) for _ in range(2)])
        cs_cur = None
        pend = []

        def flush_pend():
            if len(pend) >= 2:
                pend[-2][1]()
                pend[-1][0]()
                pend[-1][1]()
            elif len(pend) == 1:
                pend[-1][0]()
                pend[-1][1]()
            del pend[:]
        pT, b_pT = st.ps([128, 16, 128], BF16)
        paccs = Rot([st.ps([128, 512], F32) for _ in range(3)])
        pauxs = Rot([st.ps([128, 512], F32) for _ in range(3)])
        f32t = Rot([st.sb([128, 512], F32) for _ in range(18)])
        bft = Rot([st.sb([128, 512], BF16) for _ in range(14)])

        S.dma("sp", idf[:], P.c_ident, writes=[b_idf])
        S.op("dve", I("tensor_copy", out=idb[:], in_=idf[:]), reads=[b_idf], writes=[b_idb])
        S.dma("pool", blk[:], P.c_blk64, writes=[b_blk])
        S.dma("pool", rperm[:], P.c_rperm, writes=[b_rperm])
        load_pp(S, "sp", B1[:], b_B1, P.MOD[l, s, 0:D], 16, reads=[P.b("MOD")])
        load_pp(S, "sp", A1[:], b_A1, P.MOD[l, s, D:2 * D], 16, reads=[P.b("MOD")])
        load_pp(S, "sp", g1[:], b_g1, P.g_norm1[l, :], 16)
        S.op("dve", I("scalar_tensor_tensor", out=A1[:], in0=A1[:], scalar=1.0, in1=g1[:], op0=ALU.add,
                      op1=ALU.mult), reads=[b_A1, b_g1], writes=[b_A1])
        load_pp(S, "sp", bP[:], b_bP, P.b_in[l, :], 92)
        S.dma("sp", bvbc[:], P.b_in[l, 2 * AW:3 * AW].partition_broadcast(128), writes=[b_bvbc])
        for (gt, b_gt, src, sc) in ((gq, b_gq, P.g_q, 0.125), (gk, b_gk, P.g_k, 1.0)):
            for hh in range(2):
                S.dma("sp", gt[hh * 64:(hh + 1) * 64, 0:1], src[l, :].rearrange("(p o) -> p o", o=1),
                      writes=[b_gt], allow_slow_non_contiguous=True)
            S.op("act", I("mul", out=gt[:], in_=gt[:], mul=sc), reads=[b_gt], writes=[b_gt])

        def load_w(c):
            wb, b_wb = wbs[c % 3]
            S.dma("pool", wb[:], P.w_in[l, :, c * 512:(c + 1) * 512].rearrange("(kc p) n -> p kc n", p=128),
                  writes=[b_wb])

        def mm_fm(c, i, tt, pa, b_pa):
            wb, b_wb = wbs[c % 3]
            S.op("pe", [I("matmul", out=pa[:, :W], lhsT=wb[:, kc, i * 128:(i + 1) * 128],
                           rhs=hT[:, kc, tt * W:(tt + 1) * W], start=(kc == 0), stop=(kc == 15))
                        for kc in range(16)], reads=[b_wb, b_hT], writes=[b_pa])

        for ps_ in range(npass):
            for ti in range(G // 128):
                t0 = ps_ * G + ti * 128
                xt, b_xt = xts.next()
                xs, b_xs = xss.next()
                ss, b_ss = sss.next()
                rs, b_rs = rss.next()
                S.dma("sp", xt[:], X[t0:t0 + 128, :], reads=[b_X], writes=[b_xt])
                S.op("act", I("activation", out=junk[:], in_=xt[:], func=AF.Square, accum_out=ss[:]),
                     reads=[b_xt], writes=[b_junk, b_ss])
                S.op("act", I("activation", out=rs[:], in_=ss[:], func=AF.Sqrt, scale=1.0 / D, bias=EPS),
                     reads=[b_ss], writes=[b_rs])
                S.op("dve", I("reciprocal", out=rs[:], in_=rs[:]), reads=[b_rs], writes=[b_rs])
                S.op("dve", I("tensor_scalar", out=xs[:], in0=xt[:], scalar1=rs[:, 0:1], scalar2=None,
                              op0=ALU.mult), reads=[b_xt, b_rs], writes=[b_xs])
                S.op("pe", [I("transpose", out=pT[:, kc, :], in_=xs[:, kc * 128:(kc + 1) * 128], identity=idb[:])
                            for kc in range(16)], reads=[b_xs, b_idb], writes=[b_pT])
                S.op("dve", [I("tensor_scalar", out=hT[:, kc, ti * 128:(ti + 1) * 128], in0=pT[:, kc, :],
                               scalar1=A1[:, kc:kc + 1], scalar2=B1[:, kc:kc + 1], op0=ALU.mult, op1=ALU.add)
                             for kc in range(0, 8)], reads=[b_pT, b_A1, b_B1], writes=[b_hT])
                S.op("act", [I("activation", out=hT[:, kc, ti * 128:(ti + 1) * 128], in_=pT[:, kc, :],
                               func=AF.Identity, scale=A1[:, kc:kc + 1], bias=B1[:, kc:kc + 1])
                             for kc in range(8, 16)], reads=[b_pT, b_A1, b_B1], writes=[b_hT])
            load_w(chunks[0])
            for ci, c in enumerate(chunks):
                if ci + 1 < len(chunks):
                    load_w(chunks[ci + 1])
                wb, b_wb = wbs[c % 3]
                if c >= 4 and pend:
                    flush_pend()
                if c in (4, 5):
                    for ti in range(G // 128):
                        t0 = ps_ * G + ti * 128
                        pa, b_pa = paccs.next()
                        S.op("pe", [I("matmul", out=pa[:], lhsT=hT[:, kc, ti * 128:(ti + 1) * 128], rhs=wb[:, kc, :],
                                       start=(kc == 0), stop=(kc == 15)) for kc in range(16)],
                             reads=[b_wb, b_hT], writes=[b_pa])
                        ob, b_ob = bft.next()
                        S.op("dve", I("tensor_tensor", out=ob[:], in0=pa[:], in1=bvbc[:, (c - 4) * 512:(c - 3) * 512],
                                      op=ALU.add), reads=[b_pa, b_bvbc], writes=[b_ob])
                        S.dma("sp", P.V[s][t0:t0 + 128, (c - 4) * 512:(c - 3) * 512], ob[:], reads=[b_ob],
                              writes=[P.b("V%d" % s)])
                    continue
                if c in (6, 9):
                    continue
                for tt in range(ntt):
                    t0 = ps_ * G + tt * W
                    if c < 4 and lat:
                        cs_cur = cstabs.next()
                        S.dma("sp", cs_cur[0][0][:, :W], P.c_cosT[:, t0:t0 + W], writes=[cs_cur[0][1]])
                        S.dma("sp", cs_cur[1][0][:, :W], P.c_sinT[:, t0:t0 + W], writes=[cs_cur[1][1]])
                    for i in range(4):
                        fc = 4 * c + i
                        pa, b_pa = paccs.next()
                        mm_fm(c, i, tt, pa, b_pa)
                        if c < 4:
                            isq = c < 2
                            gt, b_gt = (gq, b_gq) if isq else (gk, b_gk)
                            raw, b_raw = f32t.next()
                            sq, b_sq = bft.next()
                            rstd, b_rstd = f32t.next()
                            qn, b_qn = f32t.next()
                            qb, b_qb = bft.next()
                            S.op("act", I("activation", out=raw[:, :W], in_=pa[:, :W], func=AF.Identity,
                                          bias=bP[:, fc:fc + 1], scale=1.0), reads=[b_pa, b_bP], writes=[b_raw])
                            S.op("act", I("activation", out=sq[:, :W], in_=pa[:, :W], func=AF.Square,
                                          bias=bP[:, fc:fc + 1], scale=1.0), reads=[b_pa, b_bP], writes=[b_sq])
                            rows = slice((fc % 8) * 128, (fc % 8 + 1) * 128)

                            def step2(isq=isq, gt=gt, b_gt=b_gt, raw=raw, b_raw=b_raw, sq=sq, b_sq=b_sq, rstd=rstd,
                                      b_rstd=b_rstd, qn=qn, b_qn=b_qn, rows=rows, t0=t0, qb=qb, b_qb=b_qb):
                                px, b_px = pauxs.next()
                                S.op("pe", I("matmul", out=px[:, :W], lhsT=blk[:], rhs=sq[:, :W], start=True,
                                             stop=True), reads=[b_sq, b_blk], writes=[b_px])
                                S.op("act", I("activation", out=rstd[:, :W], in_=px[:, :W], func=AF.Sqrt, bias=EPS,
                                              scale=1.0), reads=[b_px], writes=[b_rstd])
                                S.op("dve", I("reciprocal", out=rstd[:, :W], in_=rstd[:, :W]), reads=[b_rstd],
                                     writes=[b_rstd])
                                S.op("dve", I("scalar_tensor_tensor", out=qn[:, :W], in0=raw[:, :W],
                                              scalar=gt[:, 0:1], in1=rstd[:, :W], op0=ALU.mult, op1=ALU.mult),
                                     reads=[b_raw, b_rstd, b_gt], writes=[b_qn])
                                S.op("act", I("copy", out=qb[:, :W], in_=qn[:, :W]), reads=[b_qn], writes=[b_qb])
                                if isq:
                                    S.dma("sp", P.QP[s][rows, t0:t0 + W], qb[:, :W], reads=[b_qb],
                                          writes=[P.b("QP%d" % s)])
                                elif not lat:
                                    S.dma("sp", P.KC[rows, t0:t0 + W], qb[:, :W], reads=[b_qb], writes=[P.b("KC")])

                            def step3(isq=isq, qn=qn, b_qn=b_qn, rows=rows, t0=t0, cs=cs_cur, qb=qb, b_qb=b_qb):
                                if not lat:
                                    return
                                (cos_t, b_cos), (sin_t, b_sin) = cs
                                py, b_py = pauxs.next()
                                t1, b_t1 = f32t.next()
                                t2, b_t2 = f32t.next()
                                ob, b_ob = bft.next()
                                S.op("pe", I("matmul", out=py[:, :W], lhsT=rperm[:], rhs=qb[:, :W], start=True,
                                             stop=True), reads=[b_qb, b_rperm], writes=[b_py])
                                S.op("pool", I("tensor_tensor", out=t1[:, :W], in0=qn[:, :W], in1=cos_t[:, :W],
                                               op=ALU.mult), reads=[b_qn, b_cos], writes=[b_t1])
                                S.op("dve", I("tensor_tensor", out=t2[:, :W], in0=py[:, :W], in1=sin_t[:, :W],
                                              op=ALU.mult), reads=[b_py, b_sin], writes=[b_t2])
                                S.op("dve", I("tensor_tensor", out=ob[:, :W], in0=t1[:, :W], in1=t2[:, :W],
                                              op=ALU.add), reads=[b_t1, b_t2], writes=[b_ob])
                                dst, nm = (P.QR, "QR") if isq else (P.KR, "KR")
                                S.dma("sp", dst[rows, t0:t0 + W], ob[:, :W], reads=[b_ob], writes=[P.b(nm)])

                            pend.append([step2, step3])
                            if len(pend) >= 2:
                                pend[-2][0]()
                            if len(pend) >= 3:
                                pend[-3][1]()
                                pend.pop(0)
                        elif c == 7:
                            pb, b_pb = paccs.next()
                            mm_fm(6, i, tt, pb, b_pb)
                            a, b_a = f32t.next()
                            sg, b_sg = f32t.next()
                            u, b_u = bft.next()
                            S.op("act", I("activation", out=a[:, :W], in_=pb[:, :W], func=AF.Identity,
                                          bias=bP[:, 24 + i:25 + i], scale=1.0), reads=[b_pb, b_bP], writes=[b_a])
                            S.op("act", I("activation", out=sg[:, :W], in_=pa[:, :W], func=AF.Sigmoid,
                                          bias=bP[:, fc:fc + 1], scale=1.0), reads=[b_pa, b_bP], writes=[b_sg])
                            S.op("dve", I("tensor_tensor", out=u[:, :W], in0=a[:, :W], in1=sg[:, :W], op=ALU.mult),
                                 reads=[b_a, b_sg], writes=[b_u])
                            S.dma("sp", P.UT[s][i * 128:(i + 1) * 128, t0:t0 + W], u[:, :W], reads=[b_u],
                                  writes=[P.b("UT%d" % s)])
                        elif c == 8:
                            a, b_a = f32t.next()
                            S.op("act", I("activation", out=a[:, :W], in_=pa[:, :W], func=AF.Identity,
                                          bias=bP[:, fc:fc + 1], scale=1.0), reads=[b_pa, b_bP], writes=[b_a])
                            S.dma("sp", P.SCB[s][i * 128:(i + 1) * 128, t0:t0 + W], a[:, :W], reads=[b_a],
                                  writes=[P.b("SCB%d" % s)])
                        elif c == 10:
                            pb, b_pb = paccs.next()
                            mm_fm(9, i, tt, pb, b_pb)
                            a, b_a = f32t.next()
                            u, b_u = f32t.next()
                            S.op("act", I("activation", out=a[:, :W], in_=pb[:, :W], func=AF.Identity,
                                          bias=bP[:, 36 + i:37 + i], scale=1.0), reads=[b_pb, b_bP], writes=[b_a])
                            S.op("dve", I("scalar_tensor_tensor", out=u[:, :W], in0=pa[:, :W],
                                          scalar=bP[:, fc:fc + 1], in1=a[:, :W], op0=ALU.add, op1=ALU.mult),
                                 reads=[b_pa, b_a, b_bP], writes=[b_u])
                            S.dma("sp", P.CX[s][i * 128:(i + 1) * 128, t0:t0 + W], u[:, :W], reads=[b_u],
                                  writes=[P.b("CX%d" % s)])
                        else:
                            ob, b_ob = bft.next()
                            S.op("act", I("activation", out=ob[:, :W], in_=pa[:, :W], func=AF.Sigmoid,
                                          bias=bP[:, fc:fc + 1], scale=1.0), reads=[b_pa, b_bP], writes=[b_ob])
                            r0 = (fc - 44) * 128
                            S.dma("sp", P.GT[s][r0:r0 + 128, t0:t0 + W], ob[:, :W], reads=[b_ob],
                                  writes=[P.b("GT%d" % s)])


_SHARED = {}


def prep_shared(inputs):
    C = _consts()
    sh = {}
    for k in IN_SHAPES:
        if k in ("x", "ctx", "cc", "rpbx"):
            continue
        sh[k] = np.ascontiguousarray(np.asarray(inputs[k], dtype=np.float32))
    rpb = np.asarray(inputs["rpb"], dtype=np.float32)
    ro = C["_ro"].reshape(5, 128, 640)
    co = C["_co"].reshape(5, 128, 640)
    sh["rpbx"] = np.ascontiguousarray(rpb[:, :, ro, co])
    for k in CONST_SHAPES:
        sh["c_" + k] = np.ascontiguousarray(C[k])
    return sh


def prep_core(inputs, b, sh):
    m = dict(sh)
    m["x"] = np.ascontiguousarray(np.asarray(inputs["x"][b], dtype=np.float32))
    m["ctx"] = np.ascontiguousarray(np.asarray(inputs["ctx"][b], dtype=np.float32))
    m["cc"] = np.ascontiguousarray(np.stack([np.asarray(inputs["c"][b]), np.asarray(inputs["c_ctx"])]).astype(np.float32))
    return m


def stage_attn(P, l, s):
    if s == 1:
        return stage_attn_ctx(P, l)
    S = P.S
    with Stage(S, "at") as st:
        Es = Rot([st.sb([128, 7, 128], BF16) for _ in range(4)])
        sbts = Rot([st.sb([128, 640], F32) for _ in range(4)])
        recs = Rot([st.sb([128, 1], F32) for _ in range(4)])
        pS1s = Rot([st.ps([128, 4, 128], F32) for _ in range(3)])
        pS2s = Rot([st.ps([128, 3, 128], F32) for _ in range(3)])
        pOs = Rot([st.ps([128, 65], F32) for _ in range(2)])
        amask, b_amask = st.sb([128, 5, 640], F32)
        S.dma("sp", amask[:], P.c_amask.rearrange("c p f -> p c f"), writes=[b_amask])
        sets = []
        for i in range(2):
            d = dict(kcT=st.sb([64, NCX], BF16), vca=st.sb([128, 2, 65], BF16), qr=st.sb([64, N], BF16),
                     qp=st.sb([64, N], BF16), kr=st.sb([64, N], BF16), va=st.sb([128, 32, 65], BF16),
                     bias=st.sb([128, 5, 640], F32), osb=st.sb([128, 32, 64], BF16))
            S.op("pool", I("memset", ap=d["vca"][0][:, :, 64:65], constant=1.0), writes=[d["vca"][1]])
            S.op("pool", I("memset", ap=d["va"][0][:, :, 64:65], constant=1.0), writes=[d["va"][1]])
            sets.append(d)

        def load(h):
            d = sets[h % 2]
            hs = slice(h * 64, (h + 1) * 64)
            S.dma("sp", d["kcT"][0][:], P.KC[hs, :], reads=[P.b("KC")], writes=[d["kcT"][1]])
            S.dma("sp", d["vca"][0][:, :, 0:64], P.V[1][:, hs].rearrange("(c p) d -> p c d", p=128),
                  reads=[P.b("V1")], writes=[d["vca"][1]])
            S.dma("sp", d["kr"][0][:], P.KR[hs, :], reads=[P.b("KR")], writes=[d["kr"][1]])
            S.dma("sp", d["qr"][0][:], P.QR[hs, :], reads=[P.b("QR")], writes=[d["qr"][1]])
            S.dma("sp", d["qp"][0][:], P.QP[0][hs, :], reads=[P.b("QP0")], writes=[d["qp"][1]])
            S.dma("sp", d["va"][0][:, :, 0:64], P.V[0][:, hs].rearrange("(t p) d -> p t d", p=128),
                  reads=[P.b("V0")], writes=[d["va"][1]])
            S.dma("sp", d["bias"][0][:], P.rpbx[l, h].rearrange("c p f -> p c f"), writes=[d["bias"][1]])
            S.op("pool", I("tensor_tensor", out=d["bias"][0][:], in0=d["bias"][0][:], in1=amask[:], op=ALU.add),
                 reads=[d["bias"][1], b_amask], writes=[d["bias"][1]])

        load(0)
        for h in range(NH):
            if h + 1 < NH:
                load(h + 1)
            d = sets[h % 2]
            hs = slice(h * 64, (h + 1) * 64)
            kcT, b_kcT = d["kcT"]
            vca, b_vca = d["vca"]
            qr, b_qr = d["qr"]
            qp, b_qp = d["qp"]
            kr, b_kr = d["kr"]
            va, b_va = d["va"]
            bias, b_bias = d["bias"]
            osb, b_osb = d["osb"]
            def qk(b):
                pS1, b_pS1 = pS1s.next()
                pS2, b_pS2 = pS2s.next()
                ks = int(np.clip(2 * b - 4, 0, 54))
                qs = slice(b * 128, (b + 1) * 128)
                S.op("pe", [I("matmul", out=pS1[:, c, :], lhsT=kr[:, (ks + 2 * c) * 64:(ks + 2 * c + 2) * 64],
                               rhs=qr[:, qs], start=True, stop=True) for c in range(4)],
                     reads=[b_kr, b_qr], writes=[b_pS1])
                S.op("pe", [I("matmul", out=pS2[:, 0, :], lhsT=kr[:, (ks + 8) * 64:(ks + 10) * 64], rhs=qr[:, qs],
                               start=True, stop=True)] +
                           [I("matmul", out=pS2[:, 1 + c, :], lhsT=kcT[:, c * 128:(c + 1) * 128], rhs=qp[:, qs],
                              start=True, stop=True) for c in range(2)],
                     reads=[b_kr, b_qr, b_kcT, b_qp], writes=[b_pS2])
                return pS1, b_pS1, pS2, b_pS2

            ahead = [qk(0), qk(1)]
            for b in range(32):
                pS1, b_pS1, pS2, b_pS2 = ahead.pop(0)
                E, b_E = Es.next()
                sbt, b_sbt = sbts.next()
                rec, b_rec = recs.next()
                pO, b_pO = pOs.next()
                case = _acase(b)
                ks = int(np.clip(2 * b - 4, 0, 54))
                S.op("dve", I("tensor_tensor", out=sbt[:, 0:512], in0=pS1[:].rearrange("p c q -> p (c q)"),
                              in1=bias[:, case, 0:512], op=ALU.add), reads=[b_pS1, b_bias], writes=[b_sbt])
                S.op("dve", I("tensor_tensor", out=sbt[:, 512:640], in0=pS2[:, 0, :], in1=bias[:, case, 512:640],
                              op=ALU.add), reads=[b_pS2, b_bias], writes=[b_sbt])
                S.op("act", I("activation", out=E[:, 0:5, :].rearrange("p c q -> p (c q)"), in_=sbt[:], func=AF.Exp),
                     reads=[b_sbt], writes=[b_E])
                S.op("act", I("activation", out=E[:, 5:7, :], in_=pS2[:, 1:3, :], func=AF.Exp), reads=[b_pS2],
                     writes=[b_E])
                if b + 2 < 32:
                    ahead.append(qk(b + 2))
                mm = [I("matmul", out=pO[:], lhsT=E[:, c, :], rhs=va[:, ks // 2 + c, :], start=(c == 0), stop=False)
                      for c in range(5)]
                mm += [I("matmul", out=pO[:], lhsT=E[:, 5 + c, :], rhs=vca[:, c, :], start=False, stop=(c == 1))
                       for c in range(2)]
                S.op("pe", mm, reads=[b_E, b_va, b_vca], writes=[b_pO])
                S.op("dve", I("reciprocal", out=rec[:], in_=pO[:, 64:65]), reads=[b_pO], writes=[b_rec])
                S.op("dve", I("tensor_scalar", out=osb[:, b, :], in0=pO[:, 0:64], scalar1=rec[:, 0:1], scalar2=None,
                              op0=ALU.mult), reads=[b_pO, b_rec], writes=[b_osb])
            S.dma("pool", P.ATT[0][:, hs].rearrange("(b p) f -> p b f", p=128), osb[:], reads=[b_osb],
                  writes=[P.b("ATT0")])


def stage_attn_ctx(P, l):
    S = P.S
    with Stage(S, "ac") as st:
        kcT, b_kcT = st.sb([64, NCX], BF16)
        vca, b_vca = st.sb([128, 2, 65], BF16)
        S.op("pool", I("memset", ap=vca[:, :, 64:65], constant=1.0), writes=[b_vca])
        Es = Rot([st.sb([128, 2, 128], BF16) for _ in range(2)])
        recs = Rot([st.sb([128, 1], F32) for _ in range(2)])
        pCs = Rot([st.ps([128, 2, 128], F32) for _ in range(2)])
        pOs = Rot([st.ps([128, 65], F32) for _ in range(2)])
        qp, b_qp = st.sb([64, NCX], BF16)
        osb, b_osb = st.sb([128, 2, 64], BF16)
        for h in range(NH):
            hs = slice(h * 64, (h + 1) * 64)
            S.dma("sp", kcT[:], P.KC[hs, :], reads=[P.b("KC")], writes=[b_kcT])
            S.dma("sp", vca[:, :, 0:64], P.V[1][:, hs].rearrange("(c p) d -> p c d", p=128), reads=[P.b("V1")],
                  writes=[b_vca])
            S.dma("sp", qp[:], P.QP[1][hs, :], reads=[P.b("QP1")], writes=[b_qp])
            for rb in range(2):
                E, b_E = Es.next()
                pC, b_pC = pCs.next()
                pO, b_pO = pOs.next()
                rec, b_rec = recs.next()
                qpsl = qp[:, rb * 128:(rb + 1) * 128]
                S.op("pe", [I("matmul", out=pC[:, c, :], lhsT=kcT[:, c * 128:(c + 1) * 128], rhs=qpsl, start=True,
                               stop=True) for c in range(2)], reads=[b_kcT, b_qp], writes=[b_pC])
                S.op("act", I("activation", out=E[:], in_=pC[:], func=AF.Exp), reads=[b_pC], writes=[b_E])
                S.op("pe", [I("matmul", out=pO[:], lhsT=E[:, c, :], rhs=vca[:, c, :], start=(c == 0), stop=(c == 1))
                            for c in range(2)], reads=[b_E, b_vca], writes=[b_pO])
                S.op("dve", I("reciprocal", out=rec[:], in_=pO[:, 64:65]), reads=[b_pO], writes=[b_rec])
                S.op("dve", I("tensor_scalar", out=osb[:, rb, :], in0=pO[:, 0:64], scalar1=rec[:, 0:1], scalar2=None,
                              op0=ALU.mult), reads=[b_pO, b_rec], writes=[b_osb])
            S.dma("sp", P.ATT[1][:, hs].rearrange("(b p) f -> p b f", p=128), osb[:], reads=[b_osb],
                  writes=[P.b("ATT1")])


def stage_conv(P, l, s):
    S = P.S
    n = P.n[s]
    W = min(512, n)
    with Stage(S, "cv") as st:
        dw, b_dw = st.sb([128, 4, 31], F32)
        scw, b_scw = st.sb([128, 4, 3], F32)
        pp, b_pp = st.sb([128, 4, 4], F32)
        for k in range(31):
            S.dma("sp", dw[:, :, k], P.conv_dw_w[l, k, :].rearrange("(c p) -> p c", p=128), writes=[b_dw],
                  allow_slow_non_contiguous=True)
        for k in range(3):
            S.dma("sp", scw[:, :, k], P.sc_w[l, k, :].rearrange("(c p) -> p c", p=128), writes=[b_scw],
                  allow_slow_non_contiguous=True)
        for i, src in enumerate((P.conv_dw_b, P.conv_ln_g, P.conv_ln_b)):
            S.dma("sp", pp[:, i, :], src[l, :].rearrange("(c p) -> p c", p=128), writes=[b_pp],
                  allow_slow_non_contiguous=True)
        ones, b_ones = st.sb([128, 128], F32)
        S.dma("sp", ones[:], P.c_ones, writes=[b_ones])
        ups = [st.sb([128, n + 30], F32) for _ in range(2)]
        for (u, b_u) in ups:
            S.op("pool", I("memset", ap=u[:], constant=0.0), writes=[b_u])
        ubs = [st.sb([128, n + 30], BF16) for _ in range(2)]
        for (u, b_u) in ubs:
            S.op("pool", I("memset", ap=u[:], constant=0.0), writes=[b_u])
        idf, b_idf = st.sb([128, 128], F32)
        S.dma("sp", idf[:], P.c_ident, writes=[b_idf])
        dg, b_dg = st.sb([128, 4, 31, 128], BF16)
        S.op("dve", [I("tensor_scalar", out=dg[:, c, k, :], in0=idf[:], scalar1=dw[:, c, k:k + 1], scalar2=None,
                       op0=ALU.mult) for c in range(4) for k in range(31)], reads=[b_idf, b_dw], writes=[b_dg])
        cv, b_cv = st.sb([128, 4, n], F32)
        b_cvc = [S.buf() for _ in range(4)]
        pcs = Rot([st.ps([128, 512], F32) for _ in range(3)])
        for c in range(4):
            ub, b_ub = ubs[c % 2]
            S.dma("sp", ub[:, 15:15 + n], P.UT[s][c * 128:(c + 1) * 128, :], reads=[P.b("UT%d" % s)], writes=[b_ub])
            for tt in range(n // W):
                pc, b_pc = pcs.next()
                S.op("pe", [I("matmul", out=pc[:, :W], lhsT=dg[:, c, k, :], rhs=ub[:, tt * W + k:tt * W + k + W],
                               start=(k == 0), stop=(k == 30)) for k in range(31)], reads=[b_dg, b_ub], writes=[b_pc])
                S.op("act", I("activation", out=cv[:, c, tt * W:(tt + 1) * W], in_=pc[:, :W], func=AF.Identity,
                              bias=pp[:, 0, c:c + 1], scale=1.0), reads=[b_pc, b_pp], writes=[b_cvc[c]])
        f32t = Rot([st.sb([128, 512], F32) for _ in range(10)])
        bft = Rot([st.sb([128, 512], BF16) for _ in range(3)])
        pst = Rot([st.ps([128, 512], F32) for _ in range(4)])
        for tt in range(n // W):
            ts = slice(tt * W, (tt + 1) * W)
            p1, b_p1 = pst.next()
            p2, b_p2 = pst.next()
            S.op("pe", [I("matmul", out=p1[:, :W], lhsT=ones[:], rhs=cv[:, c, ts], start=(c == 0), stop=(c == 3))
                        for c in range(4)], reads=b_cvc + [b_ones], writes=[b_p1])
            sqs = []
            for c in range(4):
                sq, b_sq = f32t.next()
                S.op("act", I("activation", out=sq[:, :W], in_=cv[:, c, ts], func=AF.Square), reads=[b_cvc[c]],
                     writes=[b_sq])
                sqs.append((sq, b_sq))
            S.op("pe", [I("matmul", out=p2[:, :W], lhsT=ones[:], rhs=sqs[c][0][:, :W], start=(c == 0), stop=(c == 3))
                        for c in range(4)], reads=[q[1] for q in sqs] + [b_ones], writes=[b_p2])
            mean, b_mean = f32t.next()
            msq, b_msq = f32t.next()
            var, b_var = f32t.next()
            S.op("act", I("mul", out=mean[:, :W], in_=p1[:, :W], mul=1.0 / CW), reads=[b_p1], writes=[b_mean])
            S.op("dve", I("tensor_tensor", out=msq[:, :W], in0=mean[:, :W], in1=mean[:, :W], op=ALU.mult),
                 reads=[b_mean], writes=[b_msq])
            S.op("dve", I("scalar_tensor_tensor", out=var[:, :W], in0=p2[:, :W], scalar=1.0 / CW, in1=msq[:, :W],
                          op0=ALU.mult, op1=ALU.subtract), reads=[b_p2, b_msq], writes=[b_var])
            S.op("act", I("activation", out=var[:, :W], in_=var[:, :W], func=AF.Sqrt, bias=EPS, scale=1.0),
                 reads=[b_var], writes=[b_var])
            S.op("dve", I("reciprocal", out=var[:, :W], in_=var[:, :W]), reads=[b_var], writes=[b_var])
            for c in range(4):
                y, b_y = f32t.next()
                ob, b_ob = bft.next()
                eng = "dve" if c % 2 == 0 else "pool"
                S.op(eng, I("tensor_tensor", out=y[:, :W], in0=cv[:, c, ts], in1=mean[:, :W], op=ALU.subtract),
                     reads=[b_cvc[c], b_mean], writes=[b_y])
                S.op(eng, I("tensor_tensor", out=y[:, :W], in0=y[:, :W], in1=var[:, :W], op=ALU.mult),
                     reads=[b_y, b_var], writes=[b_y])
                S.op("act", I("activation", out=ob[:, :W], in_=y[:, :W], func=AF.Silu, scale=pp[:, 1, c:c + 1],
                              bias=pp[:, 2, c:c + 1]), reads=[b_y, b_pp], writes=[b_ob])
                S.dma("sp", P.SBT[s][c * 128:(c + 1) * 128, ts], ob[:, :W], reads=[b_ob], writes=[P.b("SBT%d" % s)])
        S.barrier()
        for c in range(4):
            u, b_u = ups[c % 2]
            eng = "dve"
            S.dma("sp", u[:, 15:15 + n], P.CX[s][c * 128:(c + 1) * 128, :], reads=[P.b("CX%d" % s)], writes=[b_u])
            S.dma("sp", cv[:, 3 - c, :], P.SCB[s][c * 128:(c + 1) * 128, :], reads=[P.b("SCB%d" % s)],
                  writes=[b_cvc[3 - c]])
            S.op(eng, I("tensor_scalar", out=cv[:, c, :], in0=u[:, 14:14 + n], scalar1=scw[:, c, 0:1], scalar2=None,
                        op0=ALU.mult), reads=[b_u, b_scw], writes=[b_cvc[c]])
            for k in (1, 2):
                S.op(eng, I("scalar_tensor_tensor", out=cv[:, c, :], in0=u[:, 14 + k:14 + k + n],
                            scalar=scw[:, c, k:k + 1], in1=cv[:, c, :], op0=ALU.mult, op1=ALU.add),
                     reads=[b_u, b_cvc[c]], writes=[b_cvc[c]])
            for tt in range(n // W):
                ts = slice(tt * W, (tt + 1) * W)
                ob, b_ob = bft.next()
                S.op(eng, I("tensor_tensor", out=ob[:, :W], in0=cv[:, c, ts], in1=cv[:, 3 - c, ts], op=ALU.mult),
                     reads=[b_cvc[c], b_cvc[3 - c]], writes=[b_ob])
                S.dma("sp", P.SCT[s][c * 128:(c + 1) * 128, ts], ob[:, :W], reads=[b_ob], writes=[P.b("SCT%d" % s)])


def stage_merge(P, l, s, xsrc):
    S = P.S
    n = P.n[s]
    W = min(256, n)
    b_X = P.b_X[s]
    with Stage(S, "mg") as st:
        wao, b_wao = st.sb([128, 8, D], BF16)
        wco, b_wco = st.sb([128, 4, D], BF16)
        wso, b_wso = st.sb([128, 4, D], BF16)
        wo, b_wo = st.sb([128, 16, D], BF16)
        S.dma("pool", wao[:], P.w_attn_o[l].rearrange("(kc p) f -> p kc f", p=128), writes=[b_wao])
        S.dma("pool", wco[:], P.w_conv_o[l].rearrange("(kc p) f -> p kc f", p=128), writes=[b_wco])
        S.dma("pool", wso[:], P.w_sc_o[l].rearrange("(kc p) f -> p kc f", p=128), writes=[b_wso])
        for hf in range(2):
            S.dma("pool", wo[:, hf * 8:(hf + 1) * 8, :],
                  P.w_o[l, hf * 1024:(hf + 1) * 1024, :].rearrange("(kc p) f -> p kc f", p=128), writes=[b_wo])
        gbc, b_gbc = st.sb([128, D], F32)
        S.dma("sp", gbc[:], P.MOD[l, s, 2 * D:3 * D].partition_broadcast(128), reads=[P.b("MOD")], writes=[b_gbc])
        idf, b_idf = st.sb([128, 128], F32)
        idb, b_idb = st.sb([128, 128], BF16)
        S.dma("sp", idf[:], P.c_ident, writes=[b_idf])
        S.op("dve", I("tensor_copy", out=idb[:], in_=idf[:]), reads=[b_idf], writes=[b_idb])
        att, b_att = st.sb([128, AW], BF16)
        attT, b_attT = st.sb([128, 8, W], BF16)
        sbT, b_sbT = st.sb([128, 4, W], BF16)
        scT, b_scT = st.sb([128, 4, W], BF16)
        gT, b_gT = st.sb([128, 48, W], BF16)
        zT, b_zT = st.sb([128, 16, W], BF16)
        xts = Rot([st.sb([128, D], F32) for _ in range(2)])
        f32t = Rot([st.sb([128, 512], F32) for _ in range(6)])
        pT, b_pT = st.ps([128, 8, 128], BF16)
        pst = Rot([st.ps([128, 512], F32) for _ in range(6)])
        for tt in range(n // W):
            t0 = tt * W
            for ti in range(W // 128):
                S.dma("sp", att[:], P.ATT[s][t0 + ti * 128:t0 + (ti + 1) * 128, :], reads=[P.b("ATT%d" % s)],
                      writes=[b_att])
                S.op("pe", [I("transpose", out=pT[:, kc, :], in_=att[:, kc * 128:(kc + 1) * 128], identity=idb[:])
                            for kc in range(8)], reads=[b_att, b_idb], writes=[b_pT])
                S.op("act", I("copy", out=attT[:, :, ti * 128:(ti + 1) * 128], in_=pT[:]), reads=[b_pT],
                     writes=[b_attT])
            S.dma("sp", sbT[:], P.SBT[s][:, t0:t0 + W].rearrange("(c p) t -> p c t", p=128), reads=[P.b("SBT%d" % s)],
                  writes=[b_sbT])
            S.dma("sp", scT[:], P.SCT[s][:, t0:t0 + W].rearrange("(c p) t -> p c t", p=128), reads=[P.b("SCT%d" % s)],
                  writes=[b_scT])
            S.dma("sp", gT[:], P.GT[s][:, t0:t0 + W].rearrange("(c p) t -> p c t", p=128), reads=[P.b("GT%d" % s)],
                  writes=[b_gT])
            for fo in range(16):
                fs = slice(fo * 128, (fo + 1) * 128)
                pA, b_pA = pst.next()
                pB, b_pB = pst.next()
                pC, b_pC = pst.next()
                S.op("pe", [I("matmul", out=pA[:, :W], lhsT=wao[:, kc, fs], rhs=attT[:, kc, :], start=(kc == 0),
                               stop=(kc == 7)) for kc in range(8)], reads=[b_wao, b_attT], writes=[b_pA])
                S.op("pe", [I("matmul", out=pB[:, :W], lhsT=wco[:, kc, fs], rhs=sbT[:, kc, :], start=(kc == 0),
                               stop=(kc == 3)) for kc in range(4)], reads=[b_wco, b_sbT], writes=[b_pB])
                S.op("pe", [I("matmul", out=pC[:, :W], lhsT=wso[:, kc, fs], rhs=scT[:, kc, :], start=(kc == 0),
                               stop=(kc == 3)) for kc in range(4)], reads=[b_wso, b_scT], writes=[b_pC])
                t1, b_t1 = f32t.next()
                t2, b_t2 = f32t.next()
                t3, b_t3 = f32t.next()
                S.op("dve", I("tensor_tensor", out=t1[:, :W], in0=pA[:, :W], in1=gT[:, fo, :], op=ALU.mult),
                     reads=[b_pA, b_gT], writes=[b_t1])
                S.op("dve", I("tensor_tensor", out=t2[:, :W], in0=pB[:, :W], in1=gT[:, 16 + fo, :], op=ALU.mult),
                     reads=[b_pB, b_gT], writes=[b_t2])
                S.op("dve", I("tensor_tensor", out=t3[:, :W], in0=pC[:, :W], in1=gT[:, 32 + fo, :], op=ALU.mult),
                     reads=[b_pC, b_gT], writes=[b_t3])
                S.op("pool", I("tensor_tensor", out=t1[:, :W], in0=t1[:, :W], in1=t2[:, :W], op=ALU.add),
                     reads=[b_t1, b_t2], writes=[b_t1])
                S.op("pool", I("tensor_tensor", out=zT[:, fo, :], in0=t1[:, :W], in1=t3[:, :W], op=ALU.add),
                     reads=[b_t1, b_t3], writes=[b_zT])
            for ti in range(W // 128):
                r0 = t0 + ti * 128
                xt, b_xt = xts.next()
                S.dma("sp", xt[:], xsrc[r0:r0 + 128, :], reads=[b_X], writes=[b_xt])
                for cg in range(4):
                    cs = slice(cg * 512, (cg + 1) * 512)
                    pm, b_pm = pst.next()
                    S.op("pe", [I("matmul", out=pm[:], lhsT=zT[:, kc, ti * 128:(ti + 1) * 128], rhs=wo[:, kc, cs],
                                   start=(kc == 0), stop=(kc == 15)) for kc in range(16)],
                         reads=[b_zT, b_wo], writes=[b_pm])
                    t1, b_t1 = f32t.next()
                    S.op("dve", I("tensor_tensor", out=t1[:], in0=pm[:], in1=gbc[:, cs], op=ALU.mult),
                         reads=[b_pm, b_gbc], writes=[b_t1])
                    S.op("pool", I("tensor_tensor", out=xt[:, cs], in0=xt[:, cs], in1=t1[:], op=ALU.add),
                         reads=[b_t1, b_xt], writes=[b_xt])
                S.dma("sp", P.X[s][r0:r0 + 128, :], xt[:], reads=[b_xt], writes=[b_X])


RW = 2068
CAPS = [512, 32]
SLOT0 = [0, 512]


def stage_moe_prep(P, l, s):
    S = P.S
    n = P.n[s]
    cap = CAPS[s]
    nt = n // 128
    b_X = P.b_X[s]
    X = P.X[s]
    with Stage(S, "mp") as st:
        A2, b_A2 = st.sb([128, D], F32)
        B2, b_B2 = st.sb([128, D], F32)
        g2, b_g2 = st.sb([128, D], F32)
        S.dma("sp", A2[:], P.MOD[l, s, 4 * D:5 * D].partition_broadcast(128), reads=[P.b("MOD")], writes=[b_A2])
        S.dma("sp", B2[:], P.MOD[l, s, 3 * D:4 * D].partition_broadcast(128), reads=[P.b("MOD")], writes=[b_B2])
        S.dma("sp", g2[:], P.g_norm2[l, :].partition_broadcast(128), writes=[b_g2])
        S.op("dve", I("scalar_tensor_tensor", out=A2[:], in0=A2[:], scalar=1.0, in1=g2[:], op0=ALU.add, op1=ALU.mult),
             reads=[b_A2, b_g2], writes=[b_A2])
        wr, b_wr = st.sb([128, 16, NE], BF16)
        S.dma("pool", wr[:], P.w_router[l].rearrange("(kc p) e -> p kc e", p=128), writes=[b_wr])
        idf, b_idf = st.sb([128, 128], F32)
        idb, b_idb = st.sb([128, 128], BF16)
        trib, b_trib = st.sb([128, 128], BF16)
        oneb, b_oneb = st.sb([128, 128], BF16)
        tokid, b_tokid = st.sb([128, 32], F32)
        S.dma("sp", idf[:], P.c_ident, writes=[b_idf])
        S.op("dve", I("tensor_copy", out=idb[:], in_=idf[:]), reads=[b_idf], writes=[b_idb])
        S.dma("pool", trib[:], P.c_tri, writes=[b_trib])
        S.dma("pool", oneb[:], P.c_ones, writes=[b_oneb])
        S.dma("sp", tokid[:], P.c_tokid, writes=[b_tokid])
        ebase, b_ebase = st.sb([128, NE], F32)
        S.dma("sp", ebase[:], P.c_ebase, writes=[b_ebase])
        dumpi, b_dumpi = st.sb([128, 1], F32)
        S.dma("sp", dumpi[:], P.c_dumpidx, writes=[b_dumpi])
        affT, b_affT = st.sb([NE, n], F32)
        maskT, b_maskT = st.sb([NE, n], BF16)
        junkT, b_junkT = st.sb([NE, n], BF16)
        affs, b_affs = st.sb([128, nt, NE], F32)
        xts = Rot([st.sb([128, D], F32) for _ in range(2)])
        h2xs = Rot([st.sb([128, RW], F32) for _ in range(2)])
        h2bs = Rot([st.sb([128, D], BF16) for _ in range(2)])
        h2Ts = Rot([st.sb([128, 16, 128], BF16) for _ in range(2)])
        junk, b_junk = st.sb([128, D], BF16)
        sm = Rot([st.sb([128, NE], F32) for _ in range(8)])
        smb = Rot([st.sb([128, NE], BF16) for _ in range(2)])
        smi = Rot([st.sb([128, NE], I32) for _ in range(2)])
        c1 = Rot([st.sb([128, 1], F32) for _ in range(6)])
        pT, b_pT = st.ps([128, 16, 128], BF16)
        pl = Rot([st.ps([128, NE], F32) for _ in range(2)])
        pA = Rot([st.ps([NE, 128], F32) for _ in range(2)])
        pm = Rot([st.ps([128, NE], BF16) for _ in range(1)])
        for ti in range(nt):
            r0 = ti * 128
            xt, b_xt = xts.next()
            h2x, b_h2x = h2xs.next()
            h2b, b_h2b = h2bs.next()
            h2T, b_h2T = h2Ts.next()
            ss, b_ss = c1.next()
            rs, b_rs = c1.next()
            se, b_se = c1.next()
            S.dma("sp", xt[:], X[r0:r0 + 128, :], reads=[b_X], writes=[b_xt])
            S.op("act", I("activation", out=junk[:], in_=xt[:], func=AF.Square, accum_out=ss[:]), reads=[b_xt],
                 writes=[b_junk, b_ss])
            S.op("act", I("activation", out=rs[:], in_=ss[:], func=AF.Sqrt, scale=1.0 / D, bias=EPS), reads=[b_ss],
                 writes=[b_rs])
            S.op("dve", I("reciprocal", out=rs[:], in_=rs[:]), reads=[b_rs], writes=[b_rs])
            S.op("dve", I("scalar_tensor_tensor", out=h2x[:, 0:D], in0=xt[:], scalar=rs[:, 0:1], in1=A2[:],
                          op0=ALU.mult, op1=ALU.mult), reads=[b_xt, b_rs, b_A2], writes=[b_h2x])
            S.op("pool", I("tensor_tensor", out=h2x[:, 0:D], in0=h2x[:, 0:D], in1=B2[:], op=ALU.add),
                 reads=[b_h2x, b_B2], writes=[b_h2x])
            S.op("act", I("copy", out=h2b[:], in_=h2x[:, 0:D]), reads=[b_h2x], writes=[b_h2b])
            S.op("pe", [I("transpose", out=pT[:, kc, :], in_=h2b[:, kc * 128:(kc + 1) * 128], identity=idb[:])
                        for kc in range(16)], reads=[b_h2b, b_idb], writes=[b_pT])
            S.op("dve", I("tensor_copy", out=h2T[:], in_=pT[:]), reads=[b_pT], writes=[b_h2T])
            S.dma("sp", P.H2[s][r0:r0 + 128, :], h2b[:], reads=[b_h2b], writes=[P.b("H2%d" % s)])
            plg, b_plg = pl.next()
            S.op("pe", [I("matmul", out=plg[:], lhsT=h2T[:, kc, :], rhs=wr[:, kc, :], start=(kc == 0), stop=(kc == 15))
                        for kc in range(16)], reads=[b_h2T, b_wr], writes=[b_plg])
            ex, b_ex = sm.next()
            S.op("act", I("activation", out=ex[:], in_=plg[:], func=AF.Exp, accum_out=se[:]), reads=[b_plg],
                 writes=[b_ex, b_se])
            S.op("dve", I("reciprocal", out=se[:], in_=se[:]), reads=[b_se], writes=[b_se])
            S.op("dve", I("tensor_scalar", out=affs[:, ti, :], in0=ex[:], scalar1=se[:, 0:1], scalar2=None,
                          op0=ALU.mult), reads=[b_ex, b_se], writes=[b_affs])
            pa, b_pa = pA.next()
            S.op("pe", I("transpose", out=pa[:], in_=affs[:, ti, :], identity=idf[:]), reads=[b_affs, b_idf],
                 writes=[b_pa])
            S.op("act", I("copy", out=affT[:, r0:r0 + 128], in_=pa[:]), reads=[b_pa], writes=[b_affT])
        lo, b_lo = st.sb([128, 1], F32)
        mid, b_mid = st.sb([128, 1], F32)
        cnt, b_cnt = st.sb([128, 1], F32)
        inc, b_inc = st.sb([128, 1], F32)
        S.op("dve", I("memset", ap=lo[0:NE, :], constant=0.0), writes=[b_lo])
        for it in range(26):
            step = 0.5 ** (it + 1)
            S.op("dve", I("tensor_scalar", out=mid[0:NE, :], in0=lo[0:NE, :], scalar1=step, scalar2=None,
                          op0=ALU.add), reads=[b_lo], writes=[b_mid])
            S.op("dve", I("tensor_scalar", out=junkT[:], in0=affT[:], scalar1=mid[0:NE, 0:1], scalar2=None,
                          op0=ALU.is_ge, op1=ALU.add, accum_out=cnt[0:NE, :]), reads=[b_affT, b_mid],
                 writes=[b_junkT, b_cnt])
            S.op("dve", I("tensor_scalar", out=inc[0:NE, :], in0=cnt[0:NE, :], scalar1=cap - 0.5, scalar2=step,
                          op0=ALU.is_ge, op1=ALU.mult), reads=[b_cnt], writes=[b_inc])
            S.op("dve", I("tensor_tensor", out=lo[0:NE, :], in0=lo[0:NE, :], in1=inc[0:NE, :], op=ALU.add),
                 reads=[b_lo, b_inc], writes=[b_lo])
        S.op("dve", I("tensor_scalar", out=maskT[:], in0=affT[:], scalar1=lo[0:NE, 0:1], scalar2=None, op0=ALU.is_ge),
             reads=[b_affT, b_lo], writes=[b_maskT])
        carry, b_carry = st.sb([128, NE], F32)
        S.op("dve", I("memset", ap=carry[:], constant=float(SLOT0[s])), writes=[b_carry])
        metas = Rot([st.sb([128, MW], F32) for _ in range(2)])
        bMETA = P.b("META")
        for ti in range(nt):
            r0 = ti * 128
            mt, b_mt = metas.next()
            pmk, b_pmk = pm.next()
            mk, b_mk = sm.next()
            mkb, b_mkb = smb.next()
            pos, b_pos = sm.next()
            t1, b_t1 = sm.next()
            t2, b_t2 = sm.next()
            idx, b_idx = smi.next()
            pp, b_pp = pl.next()
            pc, b_pc = pl.next()
            S.op("dve", I("memset", ap=mt[:], constant=0.0), writes=[b_mt])
            S.op("act", I("copy", out=mt[:, 0:1], in_=tokid[:, ti:ti + 1]), reads=[b_tokid], writes=[b_mt])
            S.op("act", I("copy", out=mt[:, 1:1 + NE], in_=affs[:, ti, :]), reads=[b_affs], writes=[b_mt])
            S.op("pe", I("transpose", out=pmk[:], in_=maskT[:, r0:r0 + 128], identity=idb[0:NE, 0:NE]),
                 reads=[b_maskT, b_idb], writes=[b_pmk])
            S.op("dve", I("tensor_copy", out=mk[:], in_=pmk[:]), reads=[b_pmk], writes=[b_mk])
            S.op("act", I("copy", out=mkb[:], in_=pmk[:]), reads=[b_pmk], writes=[b_mkb])
            S.op("pe", I("matmul", out=pp[:], lhsT=trib[:], rhs=mkb[:], start=True, stop=True), reads=[b_trib, b_mkb],
                 writes=[b_pp])
            S.op("pe", I("matmul", out=pc[:], lhsT=oneb[:], rhs=mkb[:], start=True, stop=True), reads=[b_oneb, b_mkb],
                 writes=[b_pc])
            S.op("dve", I("tensor_tensor", out=pos[:], in0=pp[:], in1=carry[:], op=ALU.add), reads=[b_pp, b_carry],
                 writes=[b_pos])
            S.op("dve", I("tensor_scalar", out=t1[:], in0=pos[:], scalar1=SLOT0[s] + cap - 0.5, scalar2=None,
                          op0=ALU.is_lt), reads=[b_pos], writes=[b_t1])
            S.op("dve", I("tensor_tensor", out=t1[:], in0=t1[:], in1=mk[:], op=ALU.mult), reads=[b_t1, b_mk],
                 writes=[b_t1])
            S.op("dve", I("tensor_tensor", out=pos[:], in0=pos[:], in1=ebase[:], op=ALU.add), reads=[b_pos, b_ebase],
                 writes=[b_pos])
            S.op("dve", I("tensor_scalar", out=pos[:], in0=pos[:], scalar1=dumpi[:, 0:1], scalar2=None,
                          op0=ALU.subtract), reads=[b_pos, b_dumpi], writes=[b_pos])
            S.op("dve", I("tensor_tensor", out=t2[:], in0=pos[:], in1=t1[:], op=ALU.mult), reads=[b_pos, b_t1],
                 writes=[b_t2])
            S.op("dve", I("tensor_scalar", out=t2[:], in0=t2[:], scalar1=dumpi[:, 0:1], scalar2=None, op0=ALU.add),
                 reads=[b_t2, b_dumpi], writes=[b_t2])
            S.op("dve", I("tensor_copy", out=idx[:], in_=t2[:]), reads=[b_t2], writes=[b_idx])
            S.op("dve", I("tensor_tensor", out=carry[:], in0=carry[:], in1=pc[:], op=ALU.add), reads=[b_carry, b_pc],
                 writes=[b_carry])
            for e in range(NE):
                S.dma_fn("pool", I("indirect_dma_start", out=P.META,
                                   out_offset=bass.IndirectOffsetOnAxis(ap=idx[:, e:e + 1], axis=0),
                                   in_=mt[:, :], in_offset=None, bounds_check=None),
                         reads=[b_mt, b_idx], writes=[bMETA])


def stage_experts_dense(P, l, streams):
    S = P.S
    with Stage(S, "ex") as st:
        w1, b_w1 = st.sb([128, 16, FF], BF16)
        w3, b_w3 = st.sb([128, 16, FF], BF16)
        w2, b_w2 = st.sb([128, 8, D], BF16)
        h2Ts = Rot([st.sb([128, 16, 512], BF16) for _ in range(2)])
        hT, b_hT = st.sb([128, 8, 512], BF16)
        m5 = {}
        gt = {}
        bxt = {}
        for s in streams:
            nt = P.n[s] // 128
            m5[s] = st.sb([128, D], F32)
            S.dma("sp", m5[s][0][:], P.MOD[l, s, 5 * D:6 * D].partition_broadcast(128), reads=[P.b("MOD")],
                  writes=[m5[s][1]])
            gt[s] = st.sb([128, nt, NE], F32)
            S.dma("sp", gt[s][0][:], P.GATE[s].rearrange("(t p) e -> p t e", p=128), reads=[P.b("GATE%d" % s)],
                  writes=[gt[s][1]])
            bxt[s] = [S.buf() for _ in range(nt)]
        yos = Rot([st.sb([128, D], F32) for _ in range(2)])
        xts = Rot([st.sb([128, D], F32) for _ in range(2)])
        f32t = Rot([st.sb([128, 512], F32) for _ in range(3)])
        pst = Rot([st.ps([128, 512], F32) for _ in range(6)])
        for e in range(NE):
            S.dma("pool", w1[:], P.w_e1[l, e].rearrange("(kc p) f -> p kc f", p=128), writes=[b_w1])
            S.dma("pool", w3[:], P.w_e3[l, e].rearrange("(kc p) f -> p kc f", p=128), writes=[b_w3])
            S.dma("pool", w2[:], P.w_e2[l, e].rearrange("(kc p) f -> p kc f", p=128), writes=[b_w2])
            for s in streams:
                n = P.n[s]
                W = min(512, n)
                for tt in range(n // W):
                    t0 = tt * W
                    h2T, b_h2T = h2Ts.next()
                    S.dma("sp", h2T[:, :, 0:W], P.H2T[s][:, t0:t0 + W].rearrange("(kc p) t -> p kc t", p=128),
                          reads=[P.b("H2T%d" % s)], writes=[b_h2T])
                    for ffc in range(8):
                        fs = slice(ffc * 128, (ffc + 1) * 128)
                        pa, b_pa = pst.next()
                        pb, b_pb = pst.next()
                        S.op("pe", [I("matmul", out=pa[:, 0:W], lhsT=w1[:, kc, fs], rhs=h2T[:, kc, 0:W],
                                       start=(kc == 0), stop=(kc == 15)) for kc in range(16)],
                             reads=[b_w1, b_h2T], writes=[b_pa])
                        S.op("pe", [I("matmul", out=pb[:, 0:W], lhsT=w3[:, kc, fs], rhs=h2T[:, kc, 0:W],
                                       start=(kc == 0), stop=(kc == 15)) for kc in range(16)],
                             reads=[b_w3, b_h2T], writes=[b_pb])
                        sa, b_sa = f32t.next()
                        S.op("act", I("activation", out=sa[:, 0:W], in_=pa[:, 0:W], func=AF.Silu), reads=[b_pa],
                             writes=[b_sa])
                        S.op("dve", I("tensor_tensor", out=hT[:, ffc, 0:W], in0=sa[:, 0:W], in1=pb[:, 0:W],
                                      op=ALU.mult), reads=[b_sa, b_pb], writes=[b_hT])
                    for ti in range(W // 128):
                        tix = (t0 // 128) + ti
                        r0 = t0 + ti * 128
                        yo, b_yo = yos.next()
                        xt, b_xt = xts.next()
                        S.dma("sp", xt[:], P.X[s][r0:r0 + 128, :], reads=[bxt[s][tix]], writes=[b_xt])
                        for cg in range(4):
                            cs = slice(cg * 512, (cg + 1) * 512)
                            py, b_py = pst.next()
                            S.op("pe", [I("matmul", out=py[:], lhsT=hT[:, ffc, ti * 128:(ti + 1) * 128],
                                           rhs=w2[:, ffc, cs], start=(ffc == 0), stop=(ffc == 7))
                                        for ffc in range(8)], reads=[b_hT, b_w2], writes=[b_py])
                            S.op("dve", I("scalar_tensor_tensor", out=yo[:, cs], in0=py[:],
                                          scalar=gt[s][0][:, tix, e:e + 1], in1=m5[s][0][:, cs], op0=ALU.mult,
                                          op1=ALU.mult), reads=[b_py, gt[s][1], m5[s][1]], writes=[b_yo])
                            S.op("pool", I("tensor_tensor", out=xt[:, cs], in0=xt[:, cs], in1=yo[:, cs], op=ALU.add),
                                 reads=[b_yo, b_xt], writes=[b_xt])
                        S.dma("sp", P.X[s][r0:r0 + 128, :], xt[:], reads=[b_xt], writes=[bxt[s][tix]])


def stage_experts(P, l, streams):
    S = P.S
    tiles = []
    for s in streams:
        cap = CAPS[s]
        for r in range(0, cap, 128):
            tiles.append((s, SLOT0[s] + r, min(128, cap - r)))
    groups = [(SLOT0[s], CAPS[s]) for s in streams]
    NS = 544
    bMETA = P.b("META")
    with Stage(S, "ex") as st:
        w1h = [st.sb([128, 16, FF // 2], BF16) for _ in range(2)]
        w3h = [st.sb([128, 16, FF // 2], BF16) for _ in range(2)]
        w2, b_w2 = st.sb([128, 8, D], BF16)
        xsT, b_xsT = st.sb([128, 16, NS], BF16)
        hT, b_hT = st.sb([128, 8, NS], BF16)
        m5 = {}
        for s in streams:
            m5[s] = st.sb([128, D], F32)
            S.dma("sp", m5[s][0][:], P.MOD[l, s, 5 * D:6 * D].partition_broadcast(128), reads=[P.b("MOD")],
                  writes=[m5[s][1]])
        idf, b_idf = st.sb([128, 128], F32)
        idb, b_idb = st.sb([128, 128], BF16)
        S.dma("sp", idf[:], P.c_ident, writes=[b_idf])
        S.op("dve", I("tensor_copy", out=idb[:], in_=idf[:]), reads=[b_idf], writes=[b_idb])
        mts = Rot([st.sb([128, MW], F32) for _ in range(16)])
        xbs = Rot([st.sb([128, D], BF16) for _ in range(6)])
        yos = Rot([st.sb([128, D], F32) for _ in range(4)])
        tks = Rot([st.sb([128, 1], I32) for _ in range(16)])
        f32t = Rot([st.sb([128, 512], F32) for _ in range(3)])
        pT, b_pT = st.ps([128, 16, 128], BF16)
        pst = Rot([st.ps([128, 512], F32) for _ in range(5)])

        def load13(e, hf):
            fsl = slice(hf * (FF // 2), (hf + 1) * (FF // 2))
            S.dma("pool", w1h[hf][0][:], P.w_e1[l, e][:, fsl].rearrange("(kc p) f -> p kc f", p=128),
                  writes=[w1h[hf][1]])
            S.dma("pool", w3h[hf][0][:], P.w_e3[l, e][:, fsl].rearrange("(kc p) f -> p kc f", p=128),
                  writes=[w3h[hf][1]])

        def load2(e):
            S.dma("pool", w2[:], P.w_e2[l, e].rearrange("(kc p) f -> p kc f", p=128), writes=[b_w2])

        def gather(e):
            meta = []
            for (s, r0, nr) in tiles:
                mt, b_mt = mts.next()
                xb, b_xb = xbs.next()
                tk, b_tk = tks.next()
                S.dma("sp", mt[0:nr, :], P.META[e * 544 + r0:e * 544 + r0 + nr, :], reads=[bMETA], writes=[b_mt])
                S.op("dve", I("tensor_copy", out=tk[0:nr, :], in_=mt[0:nr, 0:1]), reads=[b_mt], writes=[b_tk])
                S.dma_fn("pool", I("indirect_dma_start", out=xb[0:nr, :], out_offset=None, in_=P.H2[s],
                                   in_offset=bass.IndirectOffsetOnAxis(ap=tk[0:nr, 0:1], axis=0), bounds_check=None),
                         reads=[b_tk, P.b("H2%d" % s)], writes=[b_xb])
                meta.append((tk, b_tk, mt, b_mt, xb, b_xb))
            return meta

        load13(0, 0)
        load13(0, 1)
        load2(0)
        meta = gather(0)
        for e in range(NE):
            for ti_, (s, r0, nr) in enumerate(tiles):
                tk, b_tk, mt, b_mt, xb, b_xb = meta[ti_]
                S.op("pe", [I("transpose", out=pT[:, kc, 0:nr], in_=xb[0:nr, kc * 128:(kc + 1) * 128],
                               identity=idb[0:nr, 0:nr]) for kc in range(16)], reads=[b_xb, b_idb], writes=[b_pT])
                S.op("dve", I("tensor_copy", out=xsT[:, :, r0:r0 + nr], in_=pT[:, :, 0:nr]), reads=[b_pT],
                     writes=[b_xsT])
            for ffc in range(8):
                hf = ffc // 4
                fs = slice((ffc % 4) * 128, (ffc % 4 + 1) * 128)
                w1, b_w1 = w1h[hf]
                w3, b_w3 = w3h[hf]
                if ffc == 4 and e + 1 < NE:
                    load13(e + 1, 0)
                    meta_next = gather(e + 1)
                for (c0, cn) in groups:
                    pa, b_pa = pst.next()
                    pb, b_pb = pst.next()
                    S.op("pe", [I("matmul", out=pa[:, 0:cn], lhsT=w1[:, kc, fs], rhs=xsT[:, kc, c0:c0 + cn],
                                   start=(kc == 0), stop=(kc == 15)) for kc in range(16)],
                         reads=[b_w1, b_xsT], writes=[b_pa])
                    S.op("pe", [I("matmul", out=pb[:, 0:cn], lhsT=w3[:, kc, fs], rhs=xsT[:, kc, c0:c0 + cn],
                                   start=(kc == 0), stop=(kc == 15)) for kc in range(16)],
                         reads=[b_w3, b_xsT], writes=[b_pb])
                    sa, b_sa = f32t.next()
                    S.op("act", I("activation", out=sa[:, 0:cn], in_=pa[:, 0:cn], func=AF.Silu), reads=[b_pa],
                         writes=[b_sa])
                    S.op("dve", I("tensor_tensor", out=hT[:, ffc, c0:c0 + cn], in0=sa[:, 0:cn], in1=pb[:, 0:cn],
                                  op=ALU.mult), reads=[b_sa, b_pb], writes=[b_hT])
            if e + 1 < NE:
                load13(e + 1, 1)
            for ti_, (s, r0, nr) in enumerate(tiles):
                tk, b_tk, mt, b_mt, xb, b_xb = meta[ti_]
                yo, b_yo = yos.next()
                for cg in range(4):
                    cs = slice(cg * 512, (cg + 1) * 512)
                    py, b_py = pst.next()
                    S.op("pe", [I("matmul", out=py[0:nr, :], lhsT=hT[:, ffc, r0:r0 + nr], rhs=w2[:, ffc, cs],
                                   start=(ffc == 0), stop=(ffc == 7)) for ffc in range(8)],
                         reads=[b_hT, b_w2], writes=[b_py])
                    S.op("dve", I("scalar_tensor_tensor", out=yo[0:nr, cs], in0=py[0:nr, :],
                                  scalar=mt[0:nr, 1 + e:2 + e], in1=m5[s][0][0:nr, cs], op0=ALU.mult, op1=ALU.mult),
                         reads=[b_py, b_mt, m5[s][1]], writes=[b_yo])
                S.dma_fn("pool", I("indirect_dma_start", out=P.X[s],
                                   out_offset=bass.IndirectOffsetOnAxis(ap=tk[0:nr, 0:1], axis=0),
                                   in_=yo[0:nr, :], in_offset=None, bounds_check=None, compute_op=ALU.add),
                         reads=[b_yo, b_tk, P.b_X[s]], writes=[P.b_X[s]])
            if e + 1 < NE:
                load2(e + 1)
                meta = meta_next


def stage_meta_reset(P):
    S = P.S
    with Stage(S, "xr") as st:
        fill, b_fill = st.sb([128, 68 * MW], F32)
        S.dma("sp", fill[:], P.c_metafill, writes=[b_fill])
        S.dma("sp", P.META[0:NE * 544, :].rearrange("(p a) w -> p (a w)", p=128), fill[:], reads=[b_fill],
              writes=[P.b("META")])


def stage_init(P):
    S = P.S
    with Stage(S, "in") as st:
        z, b_z = st.sb([128, D], F32)
        zb, b_zb = st.sb([128, D], BF16)
        S.op("dve", I("memset", ap=z[:], constant=0.0), writes=[b_z])
        S.op("pool", I("memset", ap=zb[:], constant=0.0), writes=[b_zb])
        for s in range(2):
            n = P.n[s]
            S.dma("sp", P.X[s][n:n + 128, :], z[:], reads=[b_z], writes=[P.b_X[s]])
            S.dma("sp", P.H2[s][n:n + 128, :], zb[:], reads=[b_zb], writes=[P.b("H2%d" % s)])


def stage_final(P):
    S = P.S
    with Stage(S, "fi") as st:
        xts = Rot([st.sb([128, 4, D], F32) for _ in range(3)])
        bo = S.buf()
        for i in range(N // 512):
            xt, b_xt = xts.next()
            S.dma("sp", xt[:], P.X[0][i * 512:(i + 1) * 512, :].rearrange("(a p) f -> p a f", p=128),
                  reads=[P.b_X[0]], writes=[b_xt])
            S.dma("act", P.out[i * 512:(i + 1) * 512, :].rearrange("(a p) f -> p a f", p=128), xt[:], reads=[b_xt],
                  writes=[bo])


def build_program(dbg=()):
    P = Prog(dbg=dbg)
    stage_init(P)
    stage_mod(P)
    for l in range(2):
        last = (l == 1)
        xl = P.x if l == 0 else P.X[0]
        xc = P.ctx if l == 0 else P.X[1]
        stage_inproj(P, l, 1, xsrc=xc)
        stage_inproj(P, l, 0, xsrc=xl)
        if not last:
            stage_attn(P, l, 1)
        stage_attn(P, l, 0)
        if not last:
            stage_conv(P, l, 1)
        stage_conv(P, l, 0)
        if not last:
            stage_merge(P, l, 1, xc)
        stage_merge(P, l, 0, xl)
        stage_meta_reset(P)
        streams = [0] if last else [0, 1]
        for s in streams:
            stage_moe_prep(P, l, s)
        stage_experts(P, l, streams)
    stage_final(P)
    P.S.finish()
    return P


_PROG = {}


def kernel(**inputs):
    if "p" not in _PROG:
        _PROG["p"] = build_program()
    P = _PROG["p"]
    sh = prep_shared(inputs)
    in_maps = [prep_core(inputs, b % 4, sh) for b in range(8)]
    res = run_bass_kernel_spmd(P.nc, in_maps, core_ids=list(range(8)))
    out = np.stack([np.asarray(res.results[b]["out"], dtype=np.float32) for b in range(4)], axis=0)
    return out
```

```python
import numpy as np
import concourse.bass as bass
import concourse.mybir as mybir
from concourse.bass_utils import run_bass_kernel_spmd
from contextlib import ExitStack

F32 = mybir.dt.float32
BF16 = mybir.dt.bfloat16
I32 = mybir.dt.int32
ALU = mybir.AluOpType
AF = mybir.ActivationFunctionType

N = 4096
NCX = 256
D = 2048
NH = 16
DH = 64
AW = 1024
CW = 512
PW = 11776
NE = 16
FF = 1024
GW = 64
EPS = 1e-6
NEG = -30000.0
MW = 20


def I(name, **kw):
    return (name, kw)


class Buf:
    __slots__ = ("name", "w", "r")

    def __init__(self, name):
        self.name = name
        self.w = None
        self.r = {}


class Sched:
    ENG = ("pe", "act", "dve", "pool", "sp")
    DQ = ("sp", "pool", "act")

    def __init__(self, nc, es, ndma=8):
        self.nc = nc
        self.es = es
        self.prog = {e: [] for e in self.ENG}
        self.sems = {}
        self.cnt = {}
        for e in self.ENG:
            self.sems[e] = es.enter_context(nc.semaphore("s_" + e))
            self.cnt[e] = 0
        self.ndma = ndma
        self.dcnt = {}
        for q in self.DQ:
            self.dcnt[q] = 0
            for i in range(ndma):
                self.sems[("d", q, i)] = es.enter_context(nc.semaphore("d_%s_%d" % (q, i)))
        self.seen = {e: {} for e in self.ENG}
        self.nbuf = 0
        self.ninst = 0

    def buf(self, name=None):
        self.nbuf += 1
        return Buf(name or "b%d" % self.nbuf)

    def _wait(self, e, ev):
        key, val = ev
        if self.seen[e].get(key, 0) >= val:
            return
        self.seen[e][key] = val
        sem = self.sems[key]
        self.prog[e].append(lambda eng, sem=sem, val=val: eng.wait_ge(sem, val))

    def _deps(self, e, reads, writes):
        for b in reads:
            if b.w is not None:
                if not (b.w[0] == e and e == "pe"):
                    self._wait(e, b.w)
        for b in writes:
            if b.w is not None and b.w[0] != e:
                self._wait(e, b.w)
            for k, v in b.r.items():
                if k != e:
                    self._wait(e, (k, v))

    def _mark(self, ev, reads, writes):
        k, v = ev
        for b in reads:
            if b.r.get(k, 0) < v:
                b.r[k] = v
        for b in writes:
            b.w = ev
            b.r = {}

    def op(self, e, insts, reads=(), writes=()):
        if isinstance(insts, tuple):
            insts = [insts]
        self._deps(e, reads, writes)
        self.cnt[e] += 1
        val = self.cnt[e]
        sem = self.sems[e]
        self.ninst += len(insts)

        def run(eng, insts=insts, sem=sem):
            r = None
            for name, kw in insts:
                r = getattr(eng, name)(**kw)
            r.then_inc(sem, 1)
        self.prog[e].append(run)
        self._mark((e, val), reads, writes)

    def dma(self, q, out, in_, reads=(), writes=(), **kw):
        self.dma_fn(q, I("dma_start", out=out, in_=in_, **kw), reads, writes)

    def dma_fn(self, q, inst, reads=(), writes=()):
        self._deps(q, reads, writes)
        i = self.dcnt[q]
        self.dcnt[q] += 1
        slot = i % self.ndma
        val = 16 * (i // self.ndma + 1)
        key = ("d", q, slot)
        if val > 16:
            self._wait(q, (key, val - 16))
        sem = self.sems[key]
        self.ninst += 1
        self.prog[q].append(lambda eng, inst=inst, sem=sem: getattr(eng, inst[0])(**inst[1]).then_inc(sem, 16))
        self._mark((key, val), reads, writes)

    def all_events(self):
        evs = [(e, self.cnt[e]) for e in self.ENG if self.cnt[e] > 0]
        for q in self.DQ:
            for s in range(self.ndma):
                n = (self.dcnt[q] - 1 - s) // self.ndma + 1 if self.dcnt[q] > s else 0
                if n > 0:
                    evs.append((("d", q, s), 16 * n))
        return evs

    def barrier(self, engines=None):
        evs = self.all_events()
        for e in (engines or self.ENG):
            for ev in evs:
                if ev[0] != e:
                    self._wait(e, ev)

    def flush(self):
        nc = self.nc
        prog = self.prog
        if not any(prog[e] for e in self.ENG):
            return
        with nc.Block() as block:
            @block.tensor
            def _(eng):
                for f in prog["pe"]:
                    f(eng)

            @block.scalar
            def _(eng):
                for f in prog["act"]:
                    f(eng)

            @block.vector
            def _(eng):
                for f in prog["dve"]:
                    f(eng)

            @block.gpsimd
            def _(eng):
                for f in prog["pool"]:
                    f(eng)

            @block.sync
            def _(eng):
                for f in prog["sp"]:
                    f(eng)
        self.prog = {e: [] for e in self.ENG}

    def finish(self):
        for ev in self.all_events():
            if ev[0] != "sp":
                self._wait("sp", ev)
        self.flush()


class Stage:
    _uid = [0]

    def __init__(self, S, name):
        self.S = S
        Stage._uid[0] += 1
        self.name = "%s%d" % (name, Stage._uid[0])
        self.k = 0

    def __enter__(self):
        self.S.barrier()
        self.st = ExitStack()
        self.st.__enter__()
        return self

    def __exit__(self, *a):
        if a[0] is None:
            self.S.barrier()
            self.S.flush()
        return self.st.__exit__(*a)

    def sb(self, shape, dt, name=None):
        self.k += 1
        t = self.st.enter_context(self.S.nc.sbuf_tensor("%s_%s%d" % (self.name, name or "t", self.k), list(shape), dt))
        return t, self.S.buf()

    def ps(self, shape, dt, name=None):
        self.k += 1
        t = self.st.enter_context(self.S.nc.psum_tensor("%s_%s%d" % (self.name, name or "p", self.k), list(shape), dt))
        return t, self.S.buf()


def _rope_tables():
    quarter = 16
    freqs = 1.0 / (10000.0 ** (np.arange(quarter, dtype=np.float32) / quarter))
    t = np.arange(N)
    row, col = t // GW, t % GW
    cos = np.zeros((128, N), np.float32)
    sin = np.zeros((128, N), np.float32)
    for p in range(128):
        d = p % 64
        hh, i = d // 32, d % 32
        pos = (row if hh == 0 else col).astype(np.float32)
        ang = pos * freqs[i % 16]
        cos[p] = np.cos(ang)
        sin[p] = np.sin(ang)
    rperm = np.zeros((128, 128), np.float32)
    for m in range(128):
        i = (m % 64) % 32
        if i < 16:
            rperm[m + 16, m] = -1.0
        else:
            rperm[m - 16, m] = 1.0
    return cos, sin, rperm


ACASE_B = [0, 1, 2, 30, 31]


def _acase(b):
    return 0 if b == 0 else 1 if b == 1 else 3 if b == 30 else 4 if b == 31 else 2


def _attn_geometry():
    ro = np.zeros((5, 128, 5, 128), np.int64)
    co = np.zeros((5, 128, 5, 128), np.int64)
    va = np.zeros((5, 128, 5, 128), bool)
    kcol = np.arange(64)
    qcol = np.arange(64)
    qcs = np.clip(qcol - 8, 0, 48)
    cv = (kcol[:, None] >= qcs[None, :]) & (kcol[:, None] < qcs[None, :] + 16)
    cof = np.clip(kcol[:, None] - qcol[None, :] + 15, 0, 30)
    for ci, b in enumerate(ACASE_B):
        kstart = int(np.clip(2 * b - 4, 0, 54))
        for c in range(5):
            for kk in range(2):
                krow = kstart + 2 * c + kk
                for qr in range(2):
                    r = 2 * b + qr
                    rs = int(np.clip(r - 4, 0, 56))
                    rv = rs <= krow < rs + 8
                    ps = slice(kk * 64, kk * 64 + 64)
                    qs = slice(qr * 64, qr * 64 + 64)
                    ro[ci, ps, c, qs] = int(np.clip(krow - r + 7, 0, 14))
                    co[ci, ps, c, qs] = cof
                    va[ci, ps, c, qs] = cv & rv
    return ro, co, va


def _metafill():
    m = np.zeros((NE * 544, MW), np.float32)
    row = np.arange(NE * 544)
    slot = row % 544
    m[:, 0] = np.where(slot < 512, N, NCX) + (row % 128)
    return m.reshape(128, 68 * MW)


_CONST = {}


def _consts():
    if _CONST:
        return _CONST
    cos, sin, rperm = _rope_tables()
    ro, co, va = _attn_geometry()
    blk = np.zeros((128, 128), np.float32)
    blk[:64, :64] = 1.0 / 64
    blk[64:, 64:] = 1.0 / 64
    tri = np.triu(np.ones((128, 128), np.float32), 1)
    _CONST.update(dict(
        cosT=cos, sinT=sin, rperm=rperm, blk64=blk, ident=np.eye(128, dtype=np.float32),
        ones=np.ones((128, 128), np.float32), tri=tri,
        tokid=(np.arange(32)[None, :] * 128 + np.arange(128)[:, None]).astype(np.float32),
        ebase=np.tile((np.arange(16) * 544).astype(np.float32)[None, :], (128, 1)),
        metafill=_metafill(), dumpidx=(NE * 544 + np.arange(128)).astype(np.float32).reshape(128, 1),
        amask=np.where(va, 0.0, NEG).astype(np.float32).reshape(5, 128, 640),
        _ro=ro, _co=co))
    return _CONST


CONST_SHAPES = dict(cosT=[128, N], sinT=[128, N], rperm=[128, 128], blk64=[128, 128], ident=[128, 128],
                    ones=[128, 128], tri=[128, 128], tokid=[128, 32], amask=[5, 128, 640], ebase=[128, 16],
                    metafill=[128, 68 * MW], dumpidx=[128, 1])

IN_SHAPES = dict(
    x=[N, D], ctx=[NCX, D], cc=[2, D],
    w_ada=[2, D, 6 * D], b_ada=[2, 6 * D], g_norm1=[2, D], g_norm2=[2, D], w_in=[2, D, PW], b_in=[2, PW],
    g_q=[2, DH], g_k=[2, DH], rpbx=[2, NH, 5, 128, 640], w_attn_o=[2, AW, D], conv_dw_w=[2, 31, CW],
    conv_dw_b=[2, CW], conv_ln_g=[2, CW], conv_ln_b=[2, CW], w_conv_o=[2, CW, D], sc_w=[2, 3, CW],
    w_sc_o=[2, CW, D], w_o=[2, D, D], w_router=[2, D, NE], w_e1=[2, NE, D, FF], w_e3=[2, NE, D, FF],
    w_e2=[2, NE, FF, D])


class Rot:
    def __init__(self, items):
        self.items = items
        self.i = 0

    def next(self):
        it = self.items[self.i % len(self.items)]
        self.i += 1
        return it


class Prog:
    def __init__(self, dbg=()):
        self.dbg = set(dbg)
        nc = self.nc = bass.Bass("TRN2", target_bir_lowering=False)
        self.es = ExitStack()
        self.S = Sched(nc, self.es)
        self.n = [N, NCX]
        for k, shp in IN_SHAPES.items():
            setattr(self, k, nc.dram_tensor(k, list(shp), F32, kind="ExternalInput").ap())
        for k, shp in CONST_SHAPES.items():
            setattr(self, "c_" + k, nc.dram_tensor("c_" + k, list(shp), F32, kind="ExternalInput").ap())
        self.out = nc.dram_tensor("out", [N, D], F32, kind="ExternalOutput").ap()
        self.bufs = {}
        S = self.S
        self.MOD = self.scr("MOD", [2, 2, 6 * D], F32)
        self.XC = self.scr("XC", [NCX + 128, D], F32)
        self.XE = self.scr("XE", [N + 128, D], F32)
        self.X = [self.XE, self.XC]
        self.b_X = [S.buf(), S.buf()]
        self.QP = [self.scr("QP%d" % s, [AW, self.n[s]], BF16) for s in range(2)]
        self.QR = self.scr("QR", [AW, N], BF16)
        self.KR = self.scr("KR", [AW, N], BF16)
        self.KC = self.scr("KC", [AW, NCX], BF16)
        self.V = [self.scr("V%d" % s, [self.n[s], AW], BF16) for s in range(2)]
        self.UT = [self.scr("UT%d" % s, [CW, self.n[s]], BF16) for s in range(2)]
        self.SCB = [self.scr("SCB%d" % s, [CW, self.n[s]], F32) for s in range(2)]
        self.CX = [self.scr("CX%d" % s, [CW, self.n[s]], F32) for s in range(2)]
        self.GT = [self.scr("GT%d" % s, [3 * D, self.n[s]], BF16) for s in range(2)]
        self.ATT = [self.scr("ATT%d" % s, [self.n[s], AW], BF16) for s in range(2)]
        self.SBT = [self.scr("SBT%d" % s, [CW, self.n[s]], BF16) for s in range(2)]
        self.SCT = [self.scr("SCT%d" % s, [CW, self.n[s]], BF16) for s in range(2)]
        self.H2 = [self.scr("H2%d" % s, [self.n[s] + 128, D], BF16) for s in range(2)]
        self.META = self.scr("META", [NE * 544 + 128, MW], F32)

    def scr(self, name, shape, dt):
        kind = "ExternalOutput" if name in self.dbg else "Internal"
        t = self.nc.dram_tensor(name, list(shape), dt, kind=kind).ap()
        self.bufs[name] = self.S.buf(name)
        return t

    def b(self, name):
        return self.bufs[name]


def stage_mod(P):
    S = P.S
    with Stage(S, "mod") as st:
        cT32, b_c32 = st.sb([128, 16, 2], F32)
        cT, b_cT = st.sb([128, 16, 2], BF16)
        for j in range(2):
            S.dma("sp", cT32[:, :, j], P.cc[j, :].rearrange("(kc p) -> p kc", p=128), writes=[b_c32],
                  allow_slow_non_contiguous=True)
        S.op("act", I("activation", out=cT[:], in_=cT32[:], func=AF.Silu), reads=[b_c32], writes=[b_cT])
        wbs = Rot([st.sb([128, 16, 512], BF16) for _ in range(2)])
        pss = Rot([st.ps([128, 512], F32) for _ in range(2)])
        bias2, b_bias2 = st.sb([2, 6 * D], F32)
        res, b_res = st.sb([2, 6 * D], F32)
        for l in range(2):
            for j in range(2):
                S.dma("sp", bias2[j:j + 1, :], P.b_ada[l:l + 1, :], writes=[b_bias2])
            for ch in range(24):
                wb, b_wb = wbs.next()
                pm, b_pm = pss.next()
                S.dma("pool", wb[:], P.w_ada[l, :, ch * 512:(ch + 1) * 512].rearrange("(kc p) n -> p kc n", p=128),
                      writes=[b_wb])
                S.op("pe", [I("matmul", out=pm[0:2, :], lhsT=cT[:, kc, :], rhs=wb[:, kc, :], start=(kc == 0),
                               stop=(kc == 15)) for kc in range(16)], reads=[b_cT, b_wb], writes=[b_pm])
                S.op("dve", I("tensor_tensor", out=res[0:2, ch * 512:(ch + 1) * 512], in0=pm[0:2, :],
                              in1=bias2[0:2, ch * 512:(ch + 1) * 512], op=ALU.add),
                     reads=[b_pm, b_bias2], writes=[b_res])
            S.dma("sp", P.MOD[l], res[:], reads=[b_res], writes=[P.b("MOD")])


def load_pp(S, q, tile_ap, b_tile, src_row, ncol, reads=()):
    S.dma(q, tile_ap, src_row.rearrange("(c p) -> p c", p=128), reads=list(reads), writes=[b_tile],
          allow_slow_non_contiguous=True)


def stage_inproj(P, l, s, xsrc=None):
    S = P.S
    n = P.n[s]
    lat = (s == 0)
    X = xsrc if xsrc is not None else P.X[s]
    b_X = P.b_X[s]
    G = min(n, 2048)
    npass = n // G
    W = min(512, G)
    ntt = G // W
    chunks = list(range(23)) if not (s == 1 and l == 1) else [2, 3, 4, 5]
    with Stage(S, "ip") as st:
        hT, b_hT = st.sb([128, 16, G], BF16)
        wbs = [st.sb([128, 16, 512], BF16) for _ in range(3)]
        xts = Rot([st.sb([128, D], F32) for _ in range(2)])
        xss = Rot([st.sb([128, D], BF16) for _ in range(2)])
        junk, b_junk = st.sb([128, D], BF16)
        sss = Rot([st.sb([128, 1], F32) for _ in range(2)])
        rss = Rot([st.sb([128, 1], F32) for _ in range(2)])
        idf, b_idf = st.sb([128, 128], F32)
        idb, b_idb = st.sb([128, 128], BF16)
        blk, b_blk = st.sb([128, 128], BF16)
        rperm, b_rperm = st.sb([128, 128], BF16)
        A1, b_A1 = st.sb([128, 16], F32)
        B1, b_B1 = st.sb([128, 16], F32)
        g1, b_g1 = st.sb([128, 16], F32)
        bP, b_bP = st.sb([128, 92], F32)
        bvbc, b_bvbc = st.sb([128, AW], F32)
        gq, b_gq = st.sb([128, 1], F32)
        gk, b_gk = st.sb([128, 1], F32)
        cstabs = Rot([(st.sb([128, 512], F32), st.sb([128, 512], F32)) for _ in range(2)])
        cs_cur = None
        pend = []

        def flush_pend():
            if len(pend) >= 2:
                pend[-2][1]()
                pend[-1][0]()
                pend[-1][1]()
            elif len(pend) == 1:
                pend[-1][0]()
                pend[-1][1]()
            del pend[:]
        pT, b_pT = st.ps([128, 16, 128], BF16)
        paccs = Rot([st.ps([128, 512], F32) for _ in range(3)])
        pauxs = Rot([st.ps([128, 512], F32) for _ in range(3)])
        f32t = Rot([st.sb([128, 512], F32) for _ in range(18)])
        bft = Rot([st.sb([128, 512], BF16) for _ in range(14)])

        S.dma("sp", idf[:], P.c_ident, writes=[b_idf])
        S.op("dve", I("tensor_copy", out=idb[:], in_=idf[:]), reads=[b_idf], writes=[b_idb])
        S.dma("pool", blk[:], P.c_blk64, writes=[b_blk])
        S.dma("pool", rperm[:], P.c_rperm, writes=[b_rperm])
        load_pp(S, "sp", B1[:], b_B1, P.MOD[l, s, 0:D], 16, reads=[P.b("MOD")])
        load_pp(S, "sp", A1[:], b_A1, P.MOD[l, s, D:2 * D], 16, reads=[P.b("MOD")])
        load_pp(S, "sp", g1[:], b_g1, P.g_norm1[l, :], 16)
        S.op("dve", I("scalar_tensor_tensor", out=A1[:], in0=A1[:], scalar=1.0, in1=g1[:], op0=ALU.add,
                      op1=ALU.mult), reads=[b_A1, b_g1], writes=[b_A1])
        load_pp(S, "sp", bP[:], b_bP, P.b_in[l, :], 92)
        S.dma("sp", bvbc[:], P.b_in[l, 2 * AW:3 * AW].partition_broadcast(128), writes=[b_bvbc])
        for (gt, b_gt, src, sc) in ((gq, b_gq, P.g_q, 0.125), (gk, b_gk, P.g_k, 1.0)):
            for hh in range(2):
                S.dma("sp", gt[hh * 64:(hh + 1) * 64, 0:1], src[l, :].rearrange("(p o) -> p o", o=1),
                      writes=[b_gt], allow_slow_non_contiguous=True)
            S.op("act", I("mul", out=gt[:], in_=gt[:], mul=sc), reads=[b_gt], writes=[b_gt])

        def load_w(c):
            wb, b_wb = wbs[c % 3]
            S.dma("pool", wb[:], P.w_in[l, :, c * 512:(c + 1) * 512].rearrange("(kc p) n -> p kc n", p=128),
                  writes=[b_wb])

        def mm_fm(c, i, tt, pa, b_pa):
            wb, b_wb = wbs[c % 3]
            S.op("pe", [I("matmul", out=pa[:, :W], lhsT=wb[:, kc, i * 128:(i + 1) * 128],
                           rhs=hT[:, kc, tt * W:(tt + 1) * W], start=(kc == 0), stop=(kc == 15))
                        for kc in range(16)], reads=[b_wb, b_hT], writes=[b_pa])

        for ps_ in range(npass):
            for ti in range(G // 128):
                t0 = ps_ * G + ti * 128
                xt, b_xt = xts.next()
                xs, b_xs = xss.next()
                ss, b_ss = sss.next()
                rs, b_rs = rss.next()
                S.dma("sp", xt[:], X[t0:t0 + 128, :], reads=[b_X], writes=[b_xt])
                S.op("act", I("activation", out=junk[:], in_=xt[:], func=AF.Square, accum_out=ss[:]),
                     reads=[b_xt], writes=[b_junk, b_ss])
                S.op("act", I("activation", out=rs[:], in_=ss[:], func=AF.Sqrt, scale=1.0 / D, bias=EPS),
                     reads=[b_ss], writes=[b_rs])
                S.op("dve", I("reciprocal", out=rs[:], in_=rs[:]), reads=[b_rs], writes=[b_rs])
                S.op("dve", I("tensor_scalar", out=xs[:], in0=xt[:], scalar1=rs[:, 0:1], scalar2=None,
                              op0=ALU.mult), reads=[b_xt, b_rs], writes=[b_xs])
                S.op("pe", [I("transpose", out=pT[:, kc, :], in_=xs[:, kc * 128:(kc + 1) * 128], identity=idb[:])
                            for kc in range(16)], reads=[b_xs, b_idb], writes=[b_pT])
                S.op("dve", [I("tensor_scalar", out=hT[:, kc, ti * 128:(ti + 1) * 128], in0=pT[:, kc, :],
                               scalar1=A1[:, kc:kc + 1], scalar2=B1[:, kc:kc + 1], op0=ALU.mult, op1=ALU.add)
                             for kc in range(0, 8)], reads=[b_pT, b_A1, b_B1], writes=[b_hT])
                S.op("act", [I("activation", out=hT[:, kc, ti * 128:(ti + 1) * 128], in_=pT[:, kc, :],
                               func=AF.Identity, scale=A1[:, kc:kc + 1], bias=B1[:, kc:kc + 1])
                             for kc in range(8, 16)], reads=[b_pT, b_A1, b_B1], writes=[b_hT])
            load_w(chunks[0])
            for ci, c in enumerate(chunks):
                if ci + 1 < len(chunks):
                    load_w(chunks[ci + 1])
                wb, b_wb = wbs[c % 3]
                if c >= 4 and pend:
                    flush_pend()
                if c in (4, 5):
                    for ti in range(G // 128):
                        t0 = ps_ * G + ti * 128
                        pa, b_pa = paccs.next()
                        S.op("pe", [I("matmul", out=pa[:], lhsT=hT[:, kc, ti * 128:(ti + 1) * 128], rhs=wb[:, kc, :],
                                       start=(kc == 0), stop=(kc == 15)) for kc in range(16)],
                             reads=[b_wb, b_hT], writes=[b_pa])
                        ob, b_ob = bft.next()
                        S.op("dve", I("tensor_tensor", out=ob[:], in0=pa[:], in1=bvbc[:, (c - 4) * 512:(c - 3) * 512],
                                      op=ALU.add), reads=[b_pa, b_bvbc], writes=[b_ob])
                        S.dma("sp", P.V[s][t0:t0 + 128, (c - 4) * 512:(c - 3) * 512], ob[:], reads=[b_ob],
                              writes=[P.b("V%d" % s)])
                    continue
                if c in (6, 9):
                    continue
                for tt in range(ntt):
                    t0 = ps_ * G + tt * W
                    if c < 4 and lat:
                        cs_cur = cstabs.next()
                        S.dma("sp", cs_cur[0][0][:, :W], P.c_cosT[:, t0:t0 + W], writes=[cs_cur[0][1]])
                        S.dma("sp", cs_cur[1][0][:, :W], P.c_sinT[:, t0:t0 + W], writes=[cs_cur[1][1]])
                    for i in range(4):
                        fc = 4 * c + i
                        pa, b_pa = paccs.next()
                        mm_fm(c, i, tt, pa, b_pa)
                        if c < 4:
                            isq = c < 2
                            gt, b_gt = (gq, b_gq) if isq else (gk, b_gk)
                            raw, b_raw = f32t.next()
                            sq, b_sq = bft.next()
                            rstd, b_rstd = f32t.next()
                            qn, b_qn = f32t.next()
                            qb, b_qb = bft.next()
                            S.op("act", I("activation", out=raw[:, :W], in_=pa[:, :W], func=AF.Identity,
                                          bias=bP[:, fc:fc + 1], scale=1.0), reads=[b_pa, b_bP], writes=[b_raw])
                            S.op("act", I("activation", out=sq[:, :W], in_=pa[:, :W], func=AF.Square,
                                          bias=bP[:, fc:fc + 1], scale=1.0), reads=[b_pa, b_bP], writes=[b_sq])
                            rows = slice((fc % 8) * 128, (fc % 8 + 1) * 128)

                            def step2(isq=isq, gt=gt, b_gt=b_gt, raw=raw, b_raw=b_raw, sq=sq, b_sq=b_sq, rstd=rstd,
                                      b_rstd=b_rstd, qn=qn, b_qn=b_qn, rows=rows, t0=t0, qb=qb, b_qb=b_qb):
                                px, b_px = pauxs.next()
                                S.op("pe", I("matmul", out=px[:, :W], lhsT=blk[:], rhs=sq[:, :W], start=True,
                                             stop=True), reads=[b_sq, b_blk], writes=[b_px])
                                S.op("act", I("activation", out=rstd[:, :W], in_=px[:, :W], func=AF.Sqrt, bias=EPS,
                                              scale=1.0), reads=[b_px], writes=[b_rstd])
                                S.op("dve", I("reciprocal", out=rstd[:, :W], in_=rstd[:, :W]), reads=[b_rstd],
                                     writes=[b_rstd])
                                S.op("dve", I("scalar_tensor_tensor", out=qn[:, :W], in0=raw[:, :W],
                                              scalar=gt[:, 0:1], in1=rstd[:, :W], op0=ALU.mult, op1=ALU.mult),
                                     reads=[b_raw, b_rstd, b_gt], writes=[b_qn])
                                S.op("act", I("copy", out=qb[:, :W], in_=qn[:, :W]), reads=[b_qn], writes=[b_qb])
                                if isq:
                                    S.dma("sp", P.QP[s][rows, t0:t0 + W], qb[:, :W], reads=[b_qb],
                                          writes=[P.b("QP%d" % s)])
                                elif not lat:
                                    S.dma("sp", P.KC[rows, t0:t0 + W], qb[:, :W], reads=[b_qb], writes=[P.b("KC")])

                            def step3(isq=isq, qn=qn, b_qn=b_qn, rows=rows, t0=t0, cs=cs_cur, qb=qb, b_qb=b_qb):
                                if not lat:
                                    return
                                (cos_t, b_cos), (sin_t, b_sin) = cs
                                py, b_py = pauxs.next()
                                t1, b_t1 = f32t.next()
                                t2, b_t2 = f32t.next()
                                ob, b_ob = bft.next()
                                S.op("pe", I("matmul", out=py[:, :W], lhsT=rperm[:], rhs=qb[:, :W], start=True,
                                             stop=True), reads=[b_qb, b_rperm], writes=[b_py])
                                S.op("pool", I("tensor_tensor", out=t1[:, :W], in0=qn[:, :W], in1=cos_t[:, :W],
                                               op=ALU.mult), reads=[b_qn, b_cos], writes=[b_t1])
                                S.op("dve", I("tensor_tensor", out=t2[:, :W], in0=py[:, :W], in1=sin_t[:, :W],
                                              op=ALU.mult), reads=[b_py, b_sin], writes=[b_t2])
                                S.op("dve", I("tensor_tensor", out=ob[:, :W], in0=t1[:, :W], in1=t2[:, :W],
                                              op=ALU.add), reads=[b_t1, b_t2], writes=[b_ob])
                                dst, nm = (P.QR, "QR") if isq else (P.KR, "KR")
                                S.dma("sp", dst[rows, t0:t0 + W], ob[:, :W], reads=[b_ob], writes=[P.b(nm)])

                            pend.append([step2, step3])
                            if len(pend) >= 2:
                                pend[-2][0]()
                            if len(pend) >= 3:
                                pend[-3][1]()
                                pend.pop(0)
                        elif c == 7:
                            pb, b_pb = paccs.next()
                            mm_fm(6, i, tt, pb, b_pb)
                            a, b_a = f32t.next()
                            sg, b_sg = f32t.next()
                            u, b_u = bft.next()
                            S.op("act", I("activation", out=a[:, :W], in_=pb[:, :W], func=AF.Identity,
                                          bias=bP[:, 24 + i:25 + i], scale=1.0), reads=[b_pb, b_bP], writes=[b_a])
                            S.op("act", I("activation", out=sg[:, :W], in_=pa[:, :W], func=AF.Sigmoid,
                                          bias=bP[:, fc:fc + 1], scale=1.0), reads=[b_pa, b_bP], writes=[b_sg])
                            S.op("dve", I("tensor_tensor", out=u[:, :W], in0=a[:, :W], in1=sg[:, :W], op=ALU.mult),
                                 reads=[b_a, b_sg], writes=[b_u])
                            S.dma("sp", P.UT[s][i * 128:(i + 1) * 128, t0:t0 + W], u[:, :W], reads=[b_u],
                                  writes=[P.b("UT%d" % s)])
                        elif c == 8:
                            a, b_a = f32t.next()
                            S.op("act", I("activation", out=a[:, :W], in_=pa[:, :W], func=AF.Identity,
                                          bias=bP[:, fc:fc + 1], scale=1.0), reads=[b_pa, b_bP], writes=[b_a])
                            S.dma("sp", P.SCB[s][i * 128:(i + 1) * 128, t0:t0 + W], a[:, :W], reads=[b_a],
                                  writes=[P.b("SCB%d" % s)])
                        elif c == 10:
                            pb, b_pb = paccs.next()
                            mm_fm(9, i, tt, pb, b_pb)
                            a, b_a = f32t.next()
                            u, b_u = f32t.next()
                            S.op("act", I("activation", out=a[:, :W], in_=pb[:, :W], func=AF.Identity,
                                          bias=bP[:, 36 + i:37 + i], scale=1.0), reads=[b_pb, b_bP], writes=[b_a])
                            S.op("dve", I("scalar_tensor_tensor", out=u[:, :W], in0=pa[:, :W],
                                          scalar=bP[:, fc:fc + 1], in1=a[:, :W], op0=ALU.add, op1=ALU.mult),
                                 reads=[b_pa, b_a, b_bP], writes=[b_u])
                            S.dma("sp", P.CX[s][i * 128:(i + 1) * 128, t0:t0 + W], u[:, :W], reads=[b_u],
                                  writes=[P.b("CX%d" % s)])
                        else:
                            ob, b_ob = bft.next()
                            S.op("act", I("activation", out=ob[:, :W], in_=pa[:, :W], func=AF.Sigmoid,
                                          bias=bP[:, fc:fc + 1], scale=1.0), reads=[b_pa, b_bP], writes=[b_ob])
                            r0 = (fc - 44) * 128
                            S.dma("sp", P.GT[s][r0:r0 + 128, t0:t0 + W], ob[:, :W], reads=[b_ob],
                                  writes=[P.b("GT%d" % s)])


_SHARED = {}


def prep_shared(inputs):
    C = _consts()
    sh = {}
    for k in IN_SHAPES:
        if k in ("x", "ctx", "cc", "rpbx"):
            continue
        sh[k] = np.ascontiguousarray(np.asarray(inputs[k], dtype=np.float32))
    rpb = np.asarray(inputs["rpb"], dtype=np.float32)
    ro = C["_ro"].reshape(5, 128, 640)
    co = C["_co"].reshape(5, 128, 640)
    sh["rpbx"] = np.ascontiguousarray(rpb[:, :, ro, co])
    for k in CONST_SHAPES:
        sh["c_" + k] = np.ascontiguousarray(C[k])
    return sh


def prep_core(inputs, b, sh):
    m = dict(sh)
    m["x"] = np.ascontiguousarray(np.asarray(inputs["x"][b], dtype=np.float32))
    m["ctx"] = np.ascontiguousarray(np.asarray(inputs["ctx"][b], dtype=np.float32))
    m["cc"] = np.ascontiguousarray(np.stack([np.asarray(inputs["c"][b]), np.asarray(inputs["c_ctx"])]).astype(np.float32))
    return m


def stage_attn(P, l, s):
    if s == 1:
        return stage_attn_ctx(P, l)
    S = P.S
    with Stage(S, "at") as st:
        Es = Rot([st.sb([128, 7, 128], BF16) for _ in range(4)])
        sbts = Rot([st.sb([128, 640], F32) for _ in range(4)])
        recs = Rot([st.sb([128, 1], F32) for _ in range(4)])
        pS1s = Rot([st.ps([128, 4, 128], F32) for _ in range(3)])
        pS2s = Rot([st.ps([128, 3, 128], F32) for _ in range(3)])
        pOs = Rot([st.ps([128, 65], F32) for _ in range(2)])
        amask, b_amask = st.sb([128, 5, 640], F32)
        S.dma("sp", amask[:], P.c_amask.rearrange("c p f -> p c f"), writes=[b_amask])
        sets = []
        for i in range(2):
            d = dict(kcT=st.sb([64, NCX], BF16), vca=st.sb([128, 2, 65], BF16), qr=st.sb([64, N], BF16),
                     qp=st.sb([64, N], BF16), kr=st.sb([64, N], BF16), va=st.sb([128, 32, 65], BF16),
                     bias=st.sb([128, 5, 640], F32), osb=st.sb([128, 32, 64], BF16))
            S.op("pool", I("memset", ap=d["vca"][0][:, :, 64:65], constant=1.0), writes=[d["vca"][1]])
            S.op("pool", I("memset", ap=d["va"][0][:, :, 64:65], constant=1.0), writes=[d["va"][1]])
            sets.append(d)

        def load(h):
            d = sets[h % 2]
            hs = slice(h * 64, (h + 1) * 64)
            S.dma("sp", d["kcT"][0][:], P.KC[hs, :], reads=[P.b("KC")], writes=[d["kcT"][1]])
            S.dma("sp", d["vca"][0][:, :, 0:64], P.V[1][:, hs].rearrange("(c p) d -> p c d", p=128),
                  reads=[P.b("V1")], writes=[d["vca"][1]])
            S.dma("sp", d["kr"][0][:], P.KR[hs, :], reads=[P.b("KR")], writes=[d["kr"][1]])
            S.dma("sp", d["qr"][0][:], P.QR[hs, :], reads=[P.b("QR")], writes=[d["qr"][1]])
            S.dma("sp", d["qp"][0][:], P.QP[0][hs, :], reads=[P.b("QP0")], writes=[d["qp"][1]])
            S.dma("sp", d["va"][0][:, :, 0:64], P.V[0][:, hs].rearrange("(t p) d -> p t d", p=128),
                  reads=[P.b("V0")], writes=[d["va"][1]])
            S.dma("sp", d["bias"][0][:], P.rpbx[l, h].rearrange("c p f -> p c f"), writes=[d["bias"][1]])
            S.op("pool", I("tensor_tensor", out=d["bias"][0][:], in0=d["bias"][0][:], in1=amask[:], op=ALU.add),
                 reads=[d["bias"][1], b_amask], writes=[d["bias"][1]])

        load(0)
        for h in range(NH):
            if h + 1 < NH:
                load(h + 1)
            d = sets[h % 2]
            hs = slice(h * 64, (h + 1) * 64)
            kcT, b_kcT = d["kcT"]
            vca, b_vca = d["vca"]
            qr, b_qr = d["qr"]
            qp, b_qp = d["qp"]
            kr, b_kr = d["kr"]
            va, b_va = d["va"]
            bias, b_bias = d["bias"]
            osb, b_osb = d["osb"]
            def qk(b):
                pS1, b_pS1 = pS1s.next()
                pS2, b_pS2 = pS2s.next()
                ks = int(np.clip(2 * b - 4, 0, 54))
                qs = slice(b * 128, (b + 1) * 128)
                S.op("pe", [I("matmul", out=pS1[:, c, :], lhsT=kr[:, (ks + 2 * c) * 64:(ks + 2 * c + 2) * 64],
                               rhs=qr[:, qs], start=True, stop=True) for c in range(4)],
                     reads=[b_kr, b_qr], writes=[b_pS1])
                S.op("pe", [I("matmul", out=pS2[:, 0, :], lhsT=kr[:, (ks + 8) * 64:(ks + 10) * 64], rhs=qr[:, qs],
                               start=True, stop=True)] +
                           [I("matmul", out=pS2[:, 1 + c, :], lhsT=kcT[:, c * 128:(c + 1) * 128], rhs=qp[:, qs],
                              start=True, stop=True) for c in range(2)],
                     reads=[b_kr, b_qr, b_kcT, b_qp], writes=[b_pS2])
                return pS1, b_pS1, pS2, b_pS2

            ahead = [qk(0), qk(1)]
            for b in range(32):
                pS1, b_pS1, pS2, b_pS2 = ahead.pop(0)
                E, b_E = Es.next()
                sbt, b_sbt = sbts.next()
                rec, b_rec = recs.next()
                pO, b_pO = pOs.next()
                case = _acase(b)
                ks = int(np.clip(2 * b - 4, 0, 54))
                S.op("dve", I("tensor_tensor", out=sbt[:, 0:512], in0=pS1[:].rearrange("p c q -> p (c q)"),
                              in1=bias[:, case, 0:512], op=ALU.add), reads=[b_pS1, b_bias], writes=[b_sbt])
                S.op("dve", I("tensor_tensor", out=sbt[:, 512:640], in0=pS2[:, 0, :], in1=bias[:, case, 512:640],
                              op=ALU.add), reads=[b_pS2, b_bias], writes=[b_sbt])
                S.op("act", I("activation", out=E[:, 0:5, :].rearrange("p c q -> p (c q)"), in_=sbt[:], func=AF.Exp),
                     reads=[b_sbt], writes=[b_E])
                S.op("act", I("activation", out=E[:, 5:7, :], in_=pS2[:, 1:3, :], func=AF.Exp), reads=[b_pS2],
                     writes=[b_E])
                if b + 2 < 32:
                    ahead.append(qk(b + 2))
                mm = [I("matmul", out=pO[:], lhsT=E[:, c, :], rhs=va[:, ks // 2 + c, :], start=(c == 0), stop=False)
                      for c in range(5)]
                mm += [I("matmul", out=pO[:], lhsT=E[:, 5 + c, :], rhs=vca[:, c, :], start=False, stop=(c == 1))
                       for c in range(2)]
                S.op("pe", mm, reads=[b_E, b_va, b_vca], writes=[b_pO])
                S.op("dve", I("reciprocal", out=rec[:], in_=pO[:, 64:65]), reads=[b_pO], writes=[b_rec])
                S.op("dve", I("tensor_scalar", out=osb[:, b, :], in0=pO[:, 0:64], scalar1=rec[:, 0:1], scalar2=None,
                              op0=ALU.mult), reads=[b_pO, b_rec], writes=[b_osb])
            S.dma("pool", P.ATT[0][:, hs].rearrange("(b p) f -> p b f", p=128), osb[:], reads=[b_osb],
                  writes=[P.b("ATT0")])


def stage_attn_ctx(P, l):
    S = P.S
    with Stage(S, "ac") as st:
        kcT, b_kcT = st.sb([64, NCX], BF16)
        vca, b_vca = st.sb([128, 2, 65], BF16)
        S.op("pool", I("memset", ap=vca[:, :, 64:65], constant=1.0), writes=[b_vca])
        Es = Rot([st.sb([128, 2, 128], BF16) for _ in range(2)])
        recs = Rot([st.sb([128, 1], F32) for _ in range(2)])
        pCs = Rot([st.ps([128, 2, 128], F32) for _ in range(2)])
        pOs = Rot([st.ps([128, 65], F32) for _ in range(2)])
        qp, b_qp = st.sb([64, NCX], BF16)
        osb, b_osb = st.sb([128, 2, 64], BF16)
        for h in range(NH):
            hs = slice(h * 64, (h + 1) * 64)
            S.dma("sp", kcT[:], P.KC[hs, :], reads=[P.b("KC")], writes=[b_kcT])
            S.dma("sp", vca[:, :, 0:64], P.V[1][:, hs].rearrange("(c p) d -> p c d", p=128), reads=[P.b("V1")],
                  writes=[b_vca])
            S.dma("sp", qp[:], P.QP[1][hs, :], reads=[P.b("QP1")], writes=[b_qp])
            for rb in range(2):
                E, b_E = Es.next()
                pC, b_pC = pCs.next()
                pO, b_pO = pOs.next()
                rec, b_rec = recs.next()
                qpsl = qp[:, rb * 128:(rb + 1) * 128]
                S.op("pe", [I("matmul", out=pC[:, c, :], lhsT=kcT[:, c * 128:(c + 1) * 128], rhs=qpsl, start=True,
                               stop=True) for c in range(2)], reads=[b_kcT, b_qp], writes=[b_pC])
                S.op("act", I("activation", out=E[:], in_=pC[:], func=AF.Exp), reads=[b_pC], writes=[b_E])
                S.op("pe", [I("matmul", out=pO[:], lhsT=E[:, c, :], rhs=vca[:, c, :], start=(c == 0), stop=(c == 1))
                            for c in range(2)], reads=[b_E, b_vca], writes=[b_pO])
                S.op("dve", I("reciprocal", out=rec[:], in_=pO[:, 64:65]), reads=[b_pO], writes=[b_rec])
                S.op("dve", I("tensor_scalar", out=osb[:, rb, :], in0=pO[:, 0:64], scalar1=rec[:, 0:1], scalar2=None,
                              op0=ALU.mult), reads=[b_pO, b_rec], writes=[b_osb])
            S.dma("sp", P.ATT[1][:, hs].rearrange("(b p) f -> p b f", p=128), osb[:], reads=[b_osb],
                  writes=[P.b("ATT1")])


def stage_conv(P, l, s):
    S = P.S
    n = P.n[s]
    W = min(512, n)
    with Stage(S, "cv") as st:
        dw, b_dw = st.sb([128, 4, 31], F32)
        scw, b_scw = st.sb([128, 4, 3], F32)
        pp, b_pp = st.sb([128, 4, 4], F32)
        for k in range(31):
            S.dma("sp", dw[:, :, k], P.conv_dw_w[l, k, :].rearrange("(c p) -> p c", p=128), writes=[b_dw],
                  allow_slow_non_contiguous=True)
        for k in range(3):
            S.dma("sp", scw[:, :, k], P.sc_w[l, k, :].rearrange("(c p) -> p c", p=128), writes=[b_scw],
                  allow_slow_non_contiguous=True)
        for i, src in enumerate((P.conv_dw_b, P.conv_ln_g, P.conv_ln_b)):
            S.dma("sp", pp[:, i, :], src[l, :].rearrange("(c p) -> p c", p=128), writes=[b_pp],
                  allow_slow_non_contiguous=True)
        ones, b_ones = st.sb([128, 128], F32)
        S.dma("sp", ones[:], P.c_ones, writes=[b_ones])
        ups = [st.sb([128, n + 30], F32) for _ in range(2)]
        for (u, b_u) in ups:
            S.op("pool", I("memset", ap=u[:], constant=0.0), writes=[b_u])
        ubs = [st.sb([128, n + 30], BF16) for _ in range(2)]
        for (u, b_u) in ubs:
            S.op("pool", I("memset", ap=u[:], constant=0.0), writes=[b_u])
        idf, b_idf = st.sb([128, 128], F32)
        S.dma("sp", idf[:], P.c_ident, writes=[b_idf])
        dg, b_dg = st.sb([128, 4, 31, 128], BF16)
        S.op("dve", [I("tensor_scalar", out=dg[:, c, k, :], in0=idf[:], scalar1=dw[:, c, k:k + 1], scalar2=None,
                       op0=ALU.mult) for c in range(4) for k in range(31)], reads=[b_idf, b_dw], writes=[b_dg])
        cv, b_cv = st.sb([128, 4, n], F32)
        b_cvc = [S.buf() for _ in range(4)]
        pcs = Rot([st.ps([128, 512], F32) for _ in range(3)])
        for c in range(4):
            ub, b_ub = ubs[c % 2]
            S.dma("sp", ub[:, 15:15 + n], P.UT[s][c * 128:(c + 1) * 128, :], reads=[P.b("UT%d" % s)], writes=[b_ub])
            for tt in range(n // W):
                pc, b_pc = pcs.next()
                S.op("pe", [I("matmul", out=pc[:, :W], lhsT=dg[:, c, k, :], rhs=ub[:, tt * W + k:tt * W + k + W],
                               start=(k == 0), stop=(k == 30)) for k in range(31)], reads=[b_dg, b_ub], writes=[b_pc])
                S.op("act", I("activation", out=cv[:, c, tt * W:(tt + 1) * W], in_=pc[:, :W], func=AF.Identity,
                              bias=pp[:, 0, c:c + 1], scale=1.0), reads=[b_pc, b_pp], writes=[b_cvc[c]])
        f32t = Rot([st.sb([128, 512], F32) for _ in range(10)])
        bft = Rot([st.sb([128, 512], BF16) for _ in range(3)])
        pst = Rot([st.ps([128, 512], F32) for _ in range(4)])
        for tt in range(n // W):
            ts = slice(tt * W, (tt + 1) * W)
            p1, b_p1 = pst.next()
            p2, b_p2 = pst.next()
            S.op("pe", [I("matmul", out=p1[:, :W], lhsT=ones[:], rhs=cv[:, c, ts], start=(c == 0), stop=(c == 3))
                        for c in range(4)], reads=b_cvc + [b_ones], writes=[b_p1])
            sqs = []
            for c in range(4):
                sq, b_sq = f32t.next()
                S.op("act", I("activation", out=sq[:, :W], in_=cv[:, c, ts], func=AF.Square), reads=[b_cvc[c]],
                     writes=[b_sq])
                sqs.append((sq, b_sq))
            S.op("pe", [I("matmul", out=p2[:, :W], lhsT=ones[:], rhs=sqs[c][0][:, :W], start=(c == 0), stop=(c == 3))
                        for c in range(4)], reads=[q[1] for q in sqs] + [b_ones], writes=[b_p2])
            mean, b_mean = f32t.next()
            msq, b_msq = f32t.next()
            var, b_var = f32t.next()
            S.op("act", I("mul", out=mean[:, :W], in_=p1[:, :W], mul=1.0 / CW), reads=[b_p1], writes=[b_mean])
            S.op("dve", I("tensor_tensor", out=msq[:, :W], in0=mean[:, :W], in1=mean[:, :W], op=ALU.mult),
                 reads=[b_mean], writes=[b_msq])
            S.op("dve", I("scalar_tensor_tensor", out=var[:, :W], in0=p2[:, :W], scalar=1.0 / CW, in1=msq[:, :W],
                          op0=ALU.mult, op1=ALU.subtract), reads=[b_p2, b_msq], writes=[b_var])
            S.op("act", I("activation", out=var[:, :W], in_=var[:, :W], func=AF.Sqrt, bias=EPS, scale=1.0),
                 reads=[b_var], writes=[b_var])
            S.op("dve", I("reciprocal", out=var[:, :W], in_=var[:, :W]), reads=[b_var], writes=[b_var])
            for c in range(4):
                y, b_y = f32t.next()
                ob, b_ob = bft.next()
                eng = "dve" if c % 2 == 0 else "pool"
                S.op(eng, I("tensor_tensor", out=y[:, :W], in0=cv[:, c, ts], in1=mean[:, :W], op=ALU.subtract),
                     reads=[b_cvc[c], b_mean], writes=[b_y])
                S.op(eng, I("tensor_tensor", out=y[:, :W], in0=y[:, :W], in1=var[:, :W], op=ALU.mult),
                     reads=[b_y, b_var], writes=[b_y])
                S.op("act", I("activation", out=ob[:, :W], in_=y[:, :W], func=AF.Silu, scale=pp[:, 1, c:c + 1],
                              bias=pp[:, 2, c:c + 1]), reads=[b_y, b_pp], writes=[b_ob])
                S.dma("sp", P.SBT[s][c * 128:(c + 1) * 128, ts], ob[:, :W], reads=[b_ob], writes=[P.b("SBT%d" % s)])
        S.barrier()
        for c in range(4):
            u, b_u = ups[c % 2]
            eng = "dve"
            S.dma("sp", u[:, 15:15 + n], P.CX[s][c * 128:(c + 1) * 128, :], reads=[P.b("CX%d" % s)], writes=[b_u])
            S.dma("sp", cv[:, 3 - c, :], P.SCB[s][c * 128:(c + 1) * 128, :], reads=[P.b("SCB%d" % s)],
                  writes=[b_cvc[3 - c]])
            S.op(eng, I("tensor_scalar", out=cv[:, c, :], in0=u[:, 14:14 + n], scalar1=scw[:, c, 0:1], scalar2=None,
                        op0=ALU.mult), reads=[b_u, b_scw], writes=[b_cvc[c]])
            for k in (1, 2):
                S.op(eng, I("scalar_tensor_tensor", out=cv[:, c, :], in0=u[:, 14 + k:14 + k + n],
                            scalar=scw[:, c, k:k + 1], in1=cv[:, c, :], op0=ALU.mult, op1=ALU.add),
                     reads=[b_u, b_cvc[c]], writes=[b_cvc[c]])
            for tt in range(n // W):
                ts = slice(tt * W, (tt + 1) * W)
                ob, b_ob = bft.next()
                S.op(eng, I("tensor_tensor", out=ob[:, :W], in0=cv[:, c, ts], in1=cv[:, 3 - c, ts], op=ALU.mult),
                     reads=[b_cvc[c], b_cvc[3 - c]], writes=[b_ob])
                S.dma("sp", P.SCT[s][c * 128:(c + 1) * 128, ts], ob[:, :W], reads=[b_ob], writes=[P.b("SCT%d" % s)])


def stage_merge(P, l, s, xsrc):
    S = P.S
    n = P.n[s]
    W = min(256, n)
    b_X = P.b_X[s]
    with Stage(S, "mg") as st:
        wao, b_wao = st.sb([128, 8, D], BF16)
        wco, b_wco = st.sb([128, 4, D], BF16)
        wso, b_wso = st.sb([128, 4, D], BF16)
        wo, b_wo = st.sb([128, 16, D], BF16)
        S.dma("pool", wao[:], P.w_attn_o[l].rearrange("(kc p) f -> p kc f", p=128), writes=[b_wao])
        S.dma("pool", wco[:], P.w_conv_o[l].rearrange("(kc p) f -> p kc f", p=128), writes=[b_wco])
        S.dma("pool", wso[:], P.w_sc_o[l].rearrange("(kc p) f -> p kc f", p=128), writes=[b_wso])
        for hf in range(2):
            S.dma("pool", wo[:, hf * 8:(hf + 1) * 8, :],
                  P.w_o[l, hf * 1024:(hf + 1) * 1024, :].rearrange("(kc p) f -> p kc f", p=128), writes=[b_wo])
        gbc, b_gbc = st.sb([128, D], F32)
        S.dma("sp", gbc[:], P.MOD[l, s, 2 * D:3 * D].partition_broadcast(128), reads=[P.b("MOD")], writes=[b_gbc])
        idf, b_idf = st.sb([128, 128], F32)
        idb, b_idb = st.sb([128, 128], BF16)
        S.dma("sp", idf[:], P.c_ident, writes=[b_idf])
        S.op("dve", I("tensor_copy", out=idb[:], in_=idf[:]), reads=[b_idf], writes=[b_idb])
        atts = [st.sb([128, AW], BF16) for _ in range(2)]
        attT, b_attT = st.sb([128, 8, W], BF16)
        sbT, b_sbT = st.sb([128, 4, W], BF16)
        scT, b_scT = st.sb([128, 4, W], BF16)
        gT, b_gT = st.sb([128, 48, W], BF16)
        zT, b_zT = st.sb([128, 16, W], BF16)
        xts = Rot([st.sb([128, D], F32) for _ in range(2)])
        f32t = Rot([st.sb([128, 512], F32) for _ in range(5)])
        pT, b_pT = st.ps([128, 8, 128], BF16)
        pst = Rot([st.ps([128, 512], F32) for _ in range(6)])
        def issue_loads(tt):
            t0 = tt * W
            S.dma("sp", sbT[:], P.SBT[s][:, t0:t0 + W].rearrange("(c p) t -> p c t", p=128), reads=[P.b("SBT%d" % s)],
                  writes=[b_sbT])
            S.dma("sp", scT[:], P.SCT[s][:, t0:t0 + W].rearrange("(c p) t -> p c t", p=128), reads=[P.b("SCT%d" % s)],
                  writes=[b_scT])
            S.dma("sp", gT[:], P.GT[s][:, t0:t0 + W].rearrange("(c p) t -> p c t", p=128), reads=[P.b("GT%d" % s)],
                  writes=[b_gT])
            for ti in range(W // 128):
                att, b_att = atts[ti % 2]
                S.dma("sp", att[:], P.ATT[s][t0 + ti * 128:t0 + (ti + 1) * 128, :], reads=[P.b("ATT%d" % s)],
                      writes=[b_att])

        def issue_transposes(tt):
            for ti in range(W // 128):
                att, b_att = atts[ti % 2]
                S.op("pe", [I("transpose", out=pT[:, kc, :], in_=att[:, kc * 128:(kc + 1) * 128], identity=idb[:])
                            for kc in range(8)], reads=[b_att, b_idb], writes=[b_pT])
                S.op("act", I("copy", out=attT[:, :, ti * 128:(ti + 1) * 128], in_=pT[:]), reads=[b_pT],
                     writes=[b_attT])

        ntile = n // W
        issue_loads(0)
        issue_transposes(0)
        for tt in range(ntile):
            t0 = tt * W
            for fo in range(16):
                fs = slice(fo * 128, (fo + 1) * 128)
                pA, b_pA = pst.next()
                pB, b_pB = pst.next()
                pC, b_pC = pst.next()
                S.op("pe", [I("matmul", out=pA[:, :W], lhsT=wao[:, kc, fs], rhs=attT[:, kc, :], start=(kc == 0),
                               stop=(kc == 7)) for kc in range(8)], reads=[b_wao, b_attT], writes=[b_pA])
                S.op("pe", [I("matmul", out=pB[:, :W], lhsT=wco[:, kc, fs], rhs=sbT[:, kc, :], start=(kc == 0),
                               stop=(kc == 3)) for kc in range(4)], reads=[b_wco, b_sbT], writes=[b_pB])
                S.op("pe", [I("matmul", out=pC[:, :W], lhsT=wso[:, kc, fs], rhs=scT[:, kc, :], start=(kc == 0),
                               stop=(kc == 3)) for kc in range(4)], reads=[b_wso, b_scT], writes=[b_pC])
                t1, b_t1 = f32t.next()
                t2, b_t2 = f32t.next()
                t3, b_t3 = f32t.next()
                S.op("dve", I("tensor_tensor", out=t1[:, :W], in0=pA[:, :W], in1=gT[:, fo, :], op=ALU.mult),
                     reads=[b_pA, b_gT], writes=[b_t1])
                S.op("dve", I("tensor_tensor", out=t2[:, :W], in0=pB[:, :W], in1=gT[:, 16 + fo, :], op=ALU.mult),
                     reads=[b_pB, b_gT], writes=[b_t2])
                S.op("dve", I("tensor_tensor", out=t3[:, :W], in0=pC[:, :W], in1=gT[:, 32 + fo, :], op=ALU.mult),
                     reads=[b_pC, b_gT], writes=[b_t3])
                S.op("pool", I("tensor_tensor", out=t1[:, :W], in0=t1[:, :W], in1=t2[:, :W], op=ALU.add),
                     reads=[b_t1, b_t2], writes=[b_t1])
                S.op("pool", I("tensor_tensor", out=zT[:, fo, :], in0=t1[:, :W], in1=t3[:, :W], op=ALU.add),
                     reads=[b_t1, b_t3], writes=[b_zT])
            if tt + 1 < ntile:
                issue_loads(tt + 1)
            for ti in range(W // 128):
                if ti == (W // 128) - 1 and tt + 1 < ntile:
                    issue_transposes(tt + 1)
                r0 = t0 + ti * 128
                xt, b_xt = xts.next()
                S.dma("sp", xt[:], xsrc[r0:r0 + 128, :], reads=[b_X], writes=[b_xt])
                for cg in range(4):
                    cs = slice(cg * 512, (cg + 1) * 512)
                    pm, b_pm = pst.next()
                    S.op("pe", [I("matmul", out=pm[:], lhsT=zT[:, kc, ti * 128:(ti + 1) * 128], rhs=wo[:, kc, cs],
                                   start=(kc == 0), stop=(kc == 15)) for kc in range(16)],
                         reads=[b_zT, b_wo], writes=[b_pm])
                    t1, b_t1 = f32t.next()
                    S.op("dve", I("tensor_tensor", out=t1[:], in0=pm[:], in1=gbc[:, cs], op=ALU.mult),
                         reads=[b_pm, b_gbc], writes=[b_t1])
                    S.op("pool", I("tensor_tensor", out=xt[:, cs], in0=xt[:, cs], in1=t1[:], op=ALU.add),
                         reads=[b_t1, b_xt], writes=[b_xt])
                S.dma("sp", P.X[s][r0:r0 + 128, :], xt[:], reads=[b_xt], writes=[b_X])


RW = 2068
CAPS = [512, 32]
SLOT0 = [0, 512]


def stage_moe_prep(P, l, s):
    S = P.S
    n = P.n[s]
    cap = CAPS[s]
    nt = n // 128
    b_X = P.b_X[s]
    X = P.X[s]
    with Stage(S, "mp") as st:
        A2, b_A2 = st.sb([128, D], F32)
        B2, b_B2 = st.sb([128, D], F32)
        g2, b_g2 = st.sb([128, D], F32)
        S.dma("sp", A2[:], P.MOD[l, s, 4 * D:5 * D].partition_broadcast(128), reads=[P.b("MOD")], writes=[b_A2])
        S.dma("sp", B2[:], P.MOD[l, s, 3 * D:4 * D].partition_broadcast(128), reads=[P.b("MOD")], writes=[b_B2])
        S.dma("sp", g2[:], P.g_norm2[l, :].partition_broadcast(128), writes=[b_g2])
        S.op("dve", I("scalar_tensor_tensor", out=A2[:], in0=A2[:], scalar=1.0, in1=g2[:], op0=ALU.add, op1=ALU.mult),
             reads=[b_A2, b_g2], writes=[b_A2])
        wr, b_wr = st.sb([128, 16, NE], BF16)
        S.dma("pool", wr[:], P.w_router[l].rearrange("(kc p) e -> p kc e", p=128), writes=[b_wr])
        idf, b_idf = st.sb([128, 128], F32)
        idb, b_idb = st.sb([128, 128], BF16)
        trib, b_trib = st.sb([128, 128], BF16)
        oneb, b_oneb = st.sb([128, 128], BF16)
        tokid, b_tokid = st.sb([128, 32], F32)
        S.dma("sp", idf[:], P.c_ident, writes=[b_idf])
        S.op("dve", I("tensor_copy", out=idb[:], in_=idf[:]), reads=[b_idf], writes=[b_idb])
        S.dma("pool", trib[:], P.c_tri, writes=[b_trib])
        S.dma("pool", oneb[:], P.c_ones, writes=[b_oneb])
        S.dma("sp", tokid[:], P.c_tokid, writes=[b_tokid])
        ebase, b_ebase = st.sb([128, NE], F32)
        S.dma("sp", ebase[:], P.c_ebase, writes=[b_ebase])
        dumpi, b_dumpi = st.sb([128, 1], F32)
        S.dma("sp", dumpi[:], P.c_dumpidx, writes=[b_dumpi])
        affT, b_affT = st.sb([NE, n], F32)
        maskT, b_maskT = st.sb([NE, n], BF16)
        junkT, b_junkT = st.sb([NE, n], BF16)
        affs, b_affs = st.sb([128, nt, NE], F32)
        xts = Rot([st.sb([128, D], F32) for _ in range(2)])
        h2xs = Rot([st.sb([128, RW], F32) for _ in range(2)])
        h2bs = Rot([st.sb([128, D], BF16) for _ in range(2)])
        h2Ts = Rot([st.sb([128, 16, 128], BF16) for _ in range(2)])
        junk, b_junk = st.sb([128, D], BF16)
        sm = Rot([st.sb([128, NE], F32) for _ in range(8)])
        smb = Rot([st.sb([128, NE], BF16) for _ in range(2)])
        smi = Rot([st.sb([128, NE], I32) for _ in range(2)])
        c1 = Rot([st.sb([128, 1], F32) for _ in range(6)])
        pT, b_pT = st.ps([128, 16, 128], BF16)
        pl = Rot([st.ps([128, NE], F32) for _ in range(2)])
        pA = Rot([st.ps([NE, 128], F32) for _ in range(2)])
        pm = Rot([st.ps([128, NE], BF16) for _ in range(1)])
        for ti in range(nt):
            r0 = ti * 128
            xt, b_xt = xts.next()
            h2x, b_h2x = h2xs.next()
            h2b, b_h2b = h2bs.next()
            h2T, b_h2T = h2Ts.next()
            ss, b_ss = c1.next()
            rs, b_rs = c1.next()
            se, b_se = c1.next()
            S.dma("sp", xt[:], X[r0:r0 + 128, :], reads=[b_X], writes=[b_xt])
            S.op("act", I("activation", out=junk[:], in_=xt[:], func=AF.Square, accum_out=ss[:]), reads=[b_xt],
                 writes=[b_junk, b_ss])
            S.op("act", I("activation", out=rs[:], in_=ss[:], func=AF.Sqrt, scale=1.0 / D, bias=EPS), reads=[b_ss],
                 writes=[b_rs])
            S.op("dve", I("reciprocal", out=rs[:], in_=rs[:]), reads=[b_rs], writes=[b_rs])
            S.op("dve", I("scalar_tensor_tensor", out=h2x[:, 0:D], in0=xt[:], scalar=rs[:, 0:1], in1=A2[:],
                          op0=ALU.mult, op1=ALU.mult), reads=[b_xt, b_rs, b_A2], writes=[b_h2x])
            S.op("pool", I("tensor_tensor", out=h2x[:, 0:D], in0=h2x[:, 0:D], in1=B2[:], op=ALU.add),
                 reads=[b_h2x, b_B2], writes=[b_h2x])
            S.op("act", I("copy", out=h2b[:], in_=h2x[:, 0:D]), reads=[b_h2x], writes=[b_h2b])
            S.op("pe", [I("transpose", out=pT[:, kc, :], in_=h2b[:, kc * 128:(kc + 1) * 128], identity=idb[:])
                        for kc in range(16)], reads=[b_h2b, b_idb], writes=[b_pT])
            S.op("dve", I("tensor_copy", out=h2T[:], in_=pT[:]), reads=[b_pT], writes=[b_h2T])
            S.dma("sp", P.H2[s][r0:r0 + 128, :], h2b[:], reads=[b_h2b], writes=[P.b("H2%d" % s)])
            plg, b_plg = pl.next()
            S.op("pe", [I("matmul", out=plg[:], lhsT=h2T[:, kc, :], rhs=wr[:, kc, :], start=(kc == 0), stop=(kc == 15))
                        for kc in range(16)], reads=[b_h2T, b_wr], writes=[b_plg])
            ex, b_ex = sm.next()
            S.op("act", I("activation", out=ex[:], in_=plg[:], func=AF.Exp, accum_out=se[:]), reads=[b_plg],
                 writes=[b_ex, b_se])
            S.op("dve", I("reciprocal", out=se[:], in_=se[:]), reads=[b_se], writes=[b_se])
            S.op("dve", I("tensor_scalar", out=affs[:, ti, :], in0=ex[:], scalar1=se[:, 0:1], scalar2=None,
                          op0=ALU.mult), reads=[b_ex, b_se], writes=[b_affs])
            pa, b_pa = pA.next()
            S.op("pe", I("transpose", out=pa[:], in_=affs[:, ti, :], identity=idf[:]), reads=[b_affs, b_idf],
                 writes=[b_pa])
            S.op("act", I("copy", out=affT[:, r0:r0 + 128], in_=pa[:]), reads=[b_pa], writes=[b_affT])
        lo, b_lo = st.sb([128, 1], F32)
        mid, b_mid = st.sb([128, 1], F32)
        cnt, b_cnt = st.sb([128, 1], F32)
        inc, b_inc = st.sb([128, 1], F32)
        S.op("dve", I("memset", ap=lo[0:NE, :], constant=0.0), writes=[b_lo])
        for it in range(26):
            step = 0.5 ** (it + 1)
            S.op("dve", I("tensor_scalar", out=mid[0:NE, :], in0=lo[0:NE, :], scalar1=step, scalar2=None,
                          op0=ALU.add), reads=[b_lo], writes=[b_mid])
            S.op("dve", I("tensor_scalar", out=junkT[:], in0=affT[:], scalar1=mid[0:NE, 0:1], scalar2=None,
                          op0=ALU.is_ge, op1=ALU.add, accum_out=cnt[0:NE, :]), reads=[b_affT, b_mid],
                 writes=[b_junkT, b_cnt])
            S.op("dve", I("tensor_scalar", out=inc[0:NE, :], in0=cnt[0:NE, :], scalar1=cap - 0.5, scalar2=step,
                          op0=ALU.is_ge, op1=ALU.mult), reads=[b_cnt], writes=[b_inc])
            S.op("dve", I("tensor_tensor", out=lo[0:NE, :], in0=lo[0:NE, :], in1=inc[0:NE, :], op=ALU.add),
                 reads=[b_lo, b_inc], writes=[b_lo])
        S.op("dve", I("tensor_scalar", out=maskT[:], in0=affT[:], scalar1=lo[0:NE, 0:1], scalar2=None, op0=ALU.is_ge),
             reads=[b_affT, b_lo], writes=[b_maskT])
        carry, b_carry = st.sb([128, NE], F32)
        S.op("dve", I("memset", ap=carry[:], constant=float(SLOT0[s])), writes=[b_carry])
        metas = Rot([st.sb([128, MW], F32) for _ in range(2)])
        bMETA = P.b("META")
        for ti in range(nt):
            r0 = ti * 128
            mt, b_mt = metas.next()
            pmk, b_pmk = pm.next()
            mk, b_mk = sm.next()
            mkb, b_mkb = smb.next()
            pos, b_pos = sm.next()
            t1, b_t1 = sm.next()
            t2, b_t2 = sm.next()
            idx, b_idx = smi.next()
            pp, b_pp = pl.next()
            pc, b_pc = pl.next()
            S.op("dve", I("memset", ap=mt[:], constant=0.0), writes=[b_mt])
            S.op("act", I("copy", out=mt[:, 0:1], in_=tokid[:, ti:ti + 1]), reads=[b_tokid], writes=[b_mt])
            S.op("act", I("copy", out=mt[:, 1:1 + NE], in_=affs[:, ti, :]), reads=[b_affs], writes=[b_mt])
            S.op("pe", I("transpose", out=pmk[:], in_=maskT[:, r0:r0 + 128], identity=idb[0:NE, 0:NE]),
                 reads=[b_maskT, b_idb], writes=[b_pmk])
            S.op("dve", I("tensor_copy", out=mk[:], in_=pmk[:]), reads=[b_pmk], writes=[b_mk])
            S.op("act", I("copy", out=mkb[:], in_=pmk[:]), reads=[b_pmk], writes=[b_mkb])
            S.op("pe", I("matmul", out=pp[:], lhsT=trib[:], rhs=mkb[:], start=True, stop=True), reads=[b_trib, b_mkb],
                 writes=[b_pp])
            S.op("pe", I("matmul", out=pc[:], lhsT=oneb[:], rhs=mkb[:], start=True, stop=True), reads=[b_oneb, b_mkb],
                 writes=[b_pc])
            S.op("dve", I("tensor_tensor", out=pos[:], in0=pp[:], in1=carry[:], op=ALU.add), reads=[b_pp, b_carry],
                 writes=[b_pos])
            S.op("dve", I("tensor_scalar", out=t1[:], in0=pos[:], scalar1=SLOT0[s] + cap - 0.5, scalar2=None,
                          op0=ALU.is_lt), reads=[b_pos], writes=[b_t1])
            S.op("dve", I("tensor_tensor", out=t1[:], in0=t1[:], in1=mk[:], op=ALU.mult), reads=[b_t1, b_mk],
                 writes=[b_t1])
            S.op("dve", I("tensor_tensor", out=pos[:], in0=pos[:], in1=ebase[:], op=ALU.add), reads=[b_pos, b_ebase],
                 writes=[b_pos])
            S.op("dve", I("tensor_scalar", out=pos[:], in0=pos[:], scalar1=dumpi[:, 0:1], scalar2=None,
                          op0=ALU.subtract), reads=[b_pos, b_dumpi], writes=[b_pos])
            S.op("dve", I("tensor_tensor", out=t2[:], in0=pos[:], in1=t1[:], op=ALU.mult), reads=[b_pos, b_t1],
                 writes=[b_t2])
            S.op("dve", I("tensor_scalar", out=t2[:], in0=t2[:], scalar1=dumpi[:, 0:1], scalar2=None, op0=ALU.add),
                 reads=[b_t2, b_dumpi], writes=[b_t2])
            S.op("dve", I("tensor_copy", out=idx[:], in_=t2[:]), reads=[b_t2], writes=[b_idx])
            S.op("dve", I("tensor_tensor", out=carry[:], in0=carry[:], in1=pc[:], op=ALU.add), reads=[b_carry, b_pc],
                 writes=[b_carry])
            for e in range(NE):
                S.dma_fn("pool", I("indirect_dma_start", out=P.META,
                                   out_offset=bass.IndirectOffsetOnAxis(ap=idx[:, e:e + 1], axis=0),
                                   in_=mt[:, :], in_offset=None, bounds_check=None),
                         reads=[b_mt, b_idx], writes=[bMETA])


def stage_experts_dense(P, l, streams):
    S = P.S
    with Stage(S, "ex") as st:
        w1, b_w1 = st.sb([128, 16, FF], BF16)
        w3, b_w3 = st.sb([128, 16, FF], BF16)
        w2, b_w2 = st.sb([128, 8, D], BF16)
        h2Ts = Rot([st.sb([128, 16, 512], BF16) for _ in range(2)])
        hT, b_hT = st.sb([128, 8, 512], BF16)
        m5 = {}
        gt = {}
        bxt = {}
        for s in streams:
            nt = P.n[s] // 128
            m5[s] = st.sb([128, D], F32)
            S.dma("sp", m5[s][0][:], P.MOD[l, s, 5 * D:6 * D].partition_broadcast(128), reads=[P.b("MOD")],
                  writes=[m5[s][1]])
            gt[s] = st.sb([128, nt, NE], F32)
            S.dma("sp", gt[s][0][:], P.GATE[s].rearrange("(t p) e -> p t e", p=128), reads=[P.b("GATE%d" % s)],
                  writes=[gt[s][1]])
            bxt[s] = [S.buf() for _ in range(nt)]
        yos = Rot([st.sb([128, D], F32) for _ in range(2)])
        xts = Rot([st.sb([128, D], F32) for _ in range(2)])
        f32t = Rot([st.sb([128, 512], F32) for _ in range(3)])
        pst = Rot([st.ps([128, 512], F32) for _ in range(6)])
        for e in range(NE):
            S.dma("pool", w1[:], P.w_e1[l, e].rearrange("(kc p) f -> p kc f", p=128), writes=[b_w1])
            S.dma("pool", w3[:], P.w_e3[l, e].rearrange("(kc p) f -> p kc f", p=128), writes=[b_w3])
            S.dma("pool", w2[:], P.w_e2[l, e].rearrange("(kc p) f -> p kc f", p=128), writes=[b_w2])
            for s in streams:
                n = P.n[s]
                W = min(512, n)
                for tt in range(n // W):
                    t0 = tt * W
                    h2T, b_h2T = h2Ts.next()
                    S.dma("sp", h2T[:, :, 0:W], P.H2T[s][:, t0:t0 + W].rearrange("(kc p) t -> p kc t", p=128),
                          reads=[P.b("H2T%d" % s)], writes=[b_h2T])
                    for ffc in range(8):
                        fs = slice(ffc * 128, (ffc + 1) * 128)
                        pa, b_pa = pst.next()
                        pb, b_pb = pst.next()
                        S.op("pe", [I("matmul", out=pa[:, 0:W], lhsT=w1[:, kc, fs], rhs=h2T[:, kc, 0:W],
                                       start=(kc == 0), stop=(kc == 15)) for kc in range(16)],
                             reads=[b_w1, b_h2T], writes=[b_pa])
                        S.op("pe", [I("matmul", out=pb[:, 0:W], lhsT=w3[:, kc, fs], rhs=h2T[:, kc, 0:W],
                                       start=(kc == 0), stop=(kc == 15)) for kc in range(16)],
                             reads=[b_w3, b_h2T], writes=[b_pb])
                        sa, b_sa = f32t.next()
                        S.op("act", I("activation", out=sa[:, 0:W], in_=pa[:, 0:W], func=AF.Silu), reads=[b_pa],
                             writes=[b_sa])
                        S.op("dve", I("tensor_tensor", out=hT[:, ffc, 0:W], in0=sa[:, 0:W], in1=pb[:, 0:W],
                                      op=ALU.mult), reads=[b_sa, b_pb], writes=[b_hT])
                    for ti in range(W // 128):
                        tix = (t0 // 128) + ti
                        r0 = t0 + ti * 128
                        yo, b_yo = yos.next()
                        xt, b_xt = xts.next()
                        S.dma("sp", xt[:], P.X[s][r0:r0 + 128, :], reads=[bxt[s][tix]], writes=[b_xt])
                        for cg in range(4):
                            cs = slice(cg * 512, (cg + 1) * 512)
                            py, b_py = pst.next()
                            S.op("pe", [I("matmul", out=py[:], lhsT=hT[:, ffc, ti * 128:(ti + 1) * 128],
                                           rhs=w2[:, ffc, cs], start=(ffc == 0), stop=(ffc == 7))
                                        for ffc in range(8)], reads=[b_hT, b_w2], writes=[b_py])
                            S.op("dve", I("scalar_tensor_tensor", out=yo[:, cs], in0=py[:],
                                          scalar=gt[s][0][:, tix, e:e + 1], in1=m5[s][0][:, cs], op0=ALU.mult,
                                          op1=ALU.mult), reads=[b_py, gt[s][1], m5[s][1]], writes=[b_yo])
                            S.op("pool", I("tensor_tensor", out=xt[:, cs], in0=xt[:, cs], in1=yo[:, cs], op=ALU.add),
                                 reads=[b_yo, b_xt], writes=[b_xt])
                        S.dma("sp", P.X[s][r0:r0 + 128, :], xt[:], reads=[b_xt], writes=[bxt[s][tix]])


def stage_experts(P, l, streams):
    S = P.S
    tiles = []
    for s in streams:
        cap = CAPS[s]
        for r in range(0, cap, 128):
            tiles.append((s, SLOT0[s] + r, min(128, cap - r)))
    groups = [(SLOT0[s], CAPS[s]) for s in streams]
    NS = 544
    bMETA = P.b("META")
    with Stage(S, "ex") as st:
        w1h = [st.sb([128, 16, FF // 2], BF16) for _ in range(2)]
        w3h = [st.sb([128, 16, FF // 2], BF16) for _ in range(2)]
        w2, b_w2 = st.sb([128, 8, D], BF16)
        xsT, b_xsT = st.sb([128, 16, NS], BF16)
        hT, b_hT = st.sb([128, 8, NS], BF16)
        m5 = {}
        for s in streams:
            m5[s] = st.sb([128, D], F32)
            S.dma("sp", m5[s][0][:], P.MOD[l, s, 5 * D:6 * D].partition_broadcast(128), reads=[P.b("MOD")],
                  writes=[m5[s][1]])
        idf, b_idf = st.sb([128, 128], F32)
        idb, b_idb = st.sb([128, 128], BF16)
        S.dma("sp", idf[:], P.c_ident, writes=[b_idf])
        S.op("dve", I("tensor_copy", out=idb[:], in_=idf[:]), reads=[b_idf], writes=[b_idb])
        mts = Rot([st.sb([128, MW], F32) for _ in range(16)])
        xbs = Rot([st.sb([128, D], BF16) for _ in range(6)])
        yos = Rot([st.sb([128, D], F32) for _ in range(4)])
        tks = Rot([st.sb([128, 1], I32) for _ in range(16)])
        f32t = Rot([st.sb([128, 512], F32) for _ in range(3)])
        pT, b_pT = st.ps([128, 16, 128], BF16)
        pst = Rot([st.ps([128, 512], F32) for _ in range(5)])

        def load13(e, hf):
            fsl = slice(hf * (FF // 2), (hf + 1) * (FF // 2))
            S.dma("pool", w1h[hf][0][:], P.w_e1[l, e][:, fsl].rearrange("(kc p) f -> p kc f", p=128),
                  writes=[w1h[hf][1]])
            S.dma("pool", w3h[hf][0][:], P.w_e3[l, e][:, fsl].rearrange("(kc p) f -> p kc f", p=128),
                  writes=[w3h[hf][1]])

        def load2(e):
            S.dma("pool", w2[:], P.w_e2[l, e].rearrange("(kc p) f -> p kc f", p=128), writes=[b_w2])

        def gather(e):
            meta = []
            for (s, r0, nr) in tiles:
                mt, b_mt = mts.next()
                xb, b_xb = xbs.next()
                tk, b_tk = tks.next()
                S.dma("sp", mt[0:nr, :], P.META[e * 544 + r0:e * 544 + r0 + nr, :], reads=[bMETA], writes=[b_mt])
                S.op("dve", I("tensor_copy", out=tk[0:nr, :], in_=mt[0:nr, 0:1]), reads=[b_mt], writes=[b_tk])
                S.dma_fn("pool", I("indirect_dma_start", out=xb[0:nr, :], out_offset=None, in_=P.H2[s],
                                   in_offset=bass.IndirectOffsetOnAxis(ap=tk[0:nr, 0:1], axis=0), bounds_check=None),
                         reads=[b_tk, P.b("H2%d" % s)], writes=[b_xb])
                meta.append((tk, b_tk, mt, b_mt, xb, b_xb))
            return meta

        load13(0, 0)
        load13(0, 1)
        load2(0)
        meta = gather(0)
        for e in range(NE):
            for ti_, (s, r0, nr) in enumerate(tiles):
                tk, b_tk, mt, b_mt, xb, b_xb = meta[ti_]
                S.op("pe", [I("transpose", out=pT[:, kc, 0:nr], in_=xb[0:nr, kc * 128:(kc + 1) * 128],
                               identity=idb[0:nr, 0:nr]) for kc in range(16)], reads=[b_xb, b_idb], writes=[b_pT])
                S.op("dve", I("tensor_copy", out=xsT[:, :, r0:r0 + nr], in_=pT[:, :, 0:nr]), reads=[b_pT],
                     writes=[b_xsT])
            for ffc in range(8):
                hf = ffc // 4
                fs = slice((ffc % 4) * 128, (ffc % 4 + 1) * 128)
                w1, b_w1 = w1h[hf]
                w3, b_w3 = w3h[hf]
                if ffc == 4 and e + 1 < NE:
                    load13(e + 1, 0)
                    meta_next = gather(e + 1)
                for (c0, cn) in groups:
                    pa, b_pa = pst.next()
                    pb, b_pb = pst.next()
                    S.op("pe", [I("matmul", out=pa[:, 0:cn], lhsT=w1[:, kc, fs], rhs=xsT[:, kc, c0:c0 + cn],
                                   start=(kc == 0), stop=(kc == 15)) for kc in range(16)],
                         reads=[b_w1, b_xsT], writes=[b_pa])
                    S.op("pe", [I("matmul", out=pb[:, 0:cn], lhsT=w3[:, kc, fs], rhs=xsT[:, kc, c0:c0 + cn],
                                   start=(kc == 0), stop=(kc == 15)) for kc in range(16)],
                         reads=[b_w3, b_xsT], writes=[b_pb])
                    sa, b_sa = f32t.next()
                    S.op("act", I("activation", out=sa[:, 0:cn], in_=pa[:, 0:cn], func=AF.Silu), reads=[b_pa],
                         writes=[b_sa])
                    S.op("dve", I("tensor_tensor", out=hT[:, ffc, c0:c0 + cn], in0=sa[:, 0:cn], in1=pb[:, 0:cn],
                                  op=ALU.mult), reads=[b_sa, b_pb], writes=[b_hT])
            if e + 1 < NE:
                load13(e + 1, 1)
            for ti_, (s, r0, nr) in enumerate(tiles):
                tk, b_tk, mt, b_mt, xb, b_xb = meta[ti_]
                yo, b_yo = yos.next()
                for cg in range(4):
                    cs = slice(cg * 512, (cg + 1) * 512)
                    py, b_py = pst.next()
                    S.op("pe", [I("matmul", out=py[0:nr, :], lhsT=hT[:, ffc, r0:r0 + nr], rhs=w2[:, ffc, cs],
                                   start=(ffc == 0), stop=(ffc == 7)) for ffc in range(8)],
                         reads=[b_hT, b_w2], writes=[b_py])
                    S.op("dve", I("scalar_tensor_tensor", out=yo[0:nr, cs], in0=py[0:nr, :],
                                  scalar=mt[0:nr, 1 + e:2 + e], in1=m5[s][0][0:nr, cs], op0=ALU.mult, op1=ALU.mult),
                         reads=[b_py, b_mt, m5[s][1]], writes=[b_yo])
                S.dma_fn("pool", I("indirect_dma_start", out=P.X[s],
                                   out_offset=bass.IndirectOffsetOnAxis(ap=tk[0:nr, 0:1], axis=0),
                                   in_=yo[0:nr, :], in_offset=None, bounds_check=None, compute_op=ALU.add),
                         reads=[b_yo, b_tk, P.b_X[s]], writes=[P.b_X[s]])
            if e + 1 < NE:
                load2(e + 1)
                meta = meta_next


def stage_meta_reset(P):
    S = P.S
    with Stage(S, "xr") as st:
        fill, b_fill = st.sb([128, 68 * MW], F32)
        S.dma("sp", fill[:], P.c_metafill, writes=[b_fill])
        S.dma("sp", P.META[0:NE * 544, :].rearrange("(p a) w -> p (a w)", p=128), fill[:], reads=[b_fill],
              writes=[P.b("META")])


def stage_init(P):
    S = P.S
    with Stage(S, "in") as st:
        z, b_z = st.sb([128, D], F32)
        zb, b_zb = st.sb([128, D], BF16)
        S.op("dve", I("memset", ap=z[:], constant=0.0), writes=[b_z])
        S.op("pool", I("memset", ap=zb[:], constant=0.0), writes=[b_zb])
        for s in range(2):
            n = P.n[s]
            S.dma("sp", P.X[s][n:n + 128, :], z[:], reads=[b_z], writes=[P.b_X[s]])
            S.dma("sp", P.H2[s][n:n + 128, :], zb[:], reads=[b_zb], writes=[P.b("H2%d" % s)])


def stage_final(P):
    S = P.S
    with Stage(S, "fi") as st:
        xts = Rot([st.sb([128, 4, D], F32) for _ in range(3)])
        bo = S.buf()
        for i in range(N // 512):
            xt, b_xt = xts.next()
            S.dma("sp", xt[:], P.X[0][i * 512:(i + 1) * 512, :].rearrange("(a p) f -> p a f", p=128),
                  reads=[P.b_X[0]], writes=[b_xt])
            S.dma("act", P.out[i * 512:(i + 1) * 512, :].rearrange("(a p) f -> p a f", p=128), xt[:], reads=[b_xt],
                  writes=[bo])


def build_program(dbg=()):
    P = Prog(dbg=dbg)
    stage_init(P)
    stage_mod(P)
    for l in range(2):
        last = (l == 1)
        xl = P.x if l == 0 else P.X[0]
        xc = P.ctx if l == 0 else P.X[1]
        stage_inproj(P, l, 1, xsrc=xc)
        stage_inproj(P, l, 0, xsrc=xl)
        if not last:
            stage_attn(P, l, 1)
        stage_attn(P, l, 0)
        if not last:
            stage_conv(P, l, 1)
        stage_conv(P, l, 0)
        if not last:
            stage_merge(P, l, 1, xc)
        stage_merge(P, l, 0, xl)
        stage_meta_reset(P)
        streams = [0] if last else [0, 1]
        for s in streams:
            stage_moe_prep(P, l, s)
        stage_experts(P, l, streams)
    stage_final(P)
    P.S.finish()
    return P


_PROG = {}


def kernel(**inputs):
    if "p" not in _PROG:
        _PROG["p"] = build_program()
    P = _PROG["p"]
    sh = prep_shared(inputs)
    in_maps = [prep_core(inputs, b % 4, sh) for b in range(8)]
    res = run_bass_kernel_spmd(P.nc, in_maps, core_ids=list(range(8)))
    out = np.stack([np.asarray(res.results[b]["out"], dtype=np.float32) for b in range(4)], axis=0)
    return out
```
